# Optimizing a Trainium2 kernel written in Bass

```python
import math
import jax, jax.numpy as jnp
from jax import lax
import numpy as np

D_MODEL = 1024
BATCH = 8
SEQ = 2048
DEPTH = 2

CHUNK = 64
N_META = 16
Q_BLOCK = 128
N_A = max(1, DEPTH // 2)
N_B = DEPTH - N_A
N_HEADS = 8
A_HEAD_DIM = D_MODEL // N_HEADS
A_KV_HEADS = 2
IDX_HEADS = 8
IDX_DIM = 64
TOPK_MAX = 256
A_Q = N_HEADS * A_HEAD_DIM
A_KV = A_KV_HEADS * A_HEAD_DIM
A_IDXQ = IDX_HEADS * IDX_DIM
A_SPLITS = [A_Q, A_Q + A_KV, A_Q + 2 * A_KV, A_Q + 2 * A_KV + A_IDXQ,
            A_Q + 2 * A_KV + A_IDXQ + IDX_DIM]
A_IN = A_Q + 2 * A_KV + A_IDXQ + IDX_DIM + IDX_HEADS
B_HEAD_DIM = D_MODEL // (2 * N_HEADS)
B_V_DIM = 2 * B_HEAD_DIM
B_Q = 2 * N_HEADS * B_HEAD_DIM
B_KV = 2 * N_HEADS * B_HEAD_DIM + N_HEADS * B_V_DIM
REL_BUCKETS = 32
REL_MAX_DIST = 128
D_FF = 2816
CONV_W = 3
EPS = 1e-6
NEG_INF = -1e30
FAR_CHUNK = 1 << 30

kernel_name = "yoco_dsa_diffattn_convffn_trunk"


def rms_norm(x, g):
    xf = x.astype(jnp.float32)
    y = xf * lax.rsqrt(jnp.mean(xf * xf, axis=-1, keepdims=True) + EPS)
    return (y * g.astype(jnp.float32)).astype(x.dtype)


def chunk_ids(t_pad, t_real):
    pos = jnp.arange(t_pad, dtype=jnp.int32)
    cid = jnp.where(pos < N_META, 0, 1 + (pos - N_META) // CHUNK)
    return jnp.where(pos < t_real, cid, FAR_CHUNK)


def rel_bucket(rel):
    nb = REL_BUCKETS // 2
    max_exact = nb // 2
    n = jnp.abs(rel)
    nf = jnp.maximum(n, 1).astype(jnp.float32)
    large = max_exact + (jnp.log(nf / max_exact) / math.log(REL_MAX_DIST / max_exact)
                         * (nb - max_exact)).astype(jnp.int32)
    large = jnp.minimum(large, nb - 1)
    return jnp.where(rel > 0, nb, 0) + jnp.where(n < max_exact, n, large)


def to_blocks(a, nblk):
    return a.reshape(a.shape[0], nblk, Q_BLOCK, *a.shape[2:]).swapaxes(0, 1)


def dsa_attention(h, w_in, w_o, gq, gk, rel_table, cid, pos, k_top):
    B, Tp, _ = h.shape
    nblk = Tp // Q_BLOCK
    rep = N_HEADS // A_KV_HEADS
    q, k, v, qi, ki, wi = jnp.split(h @ w_in, A_SPLITS, axis=-1)
    q = rms_norm(q.reshape(B, Tp, N_HEADS, A_HEAD_DIM), gq)
    k = rms_norm(k.reshape(B, Tp, A_KV_HEADS, A_HEAD_DIM), gk)
    v = v.reshape(B, Tp, A_KV_HEADS, A_HEAD_DIM)
    qi = qi.reshape(B, Tp, IDX_HEADS, IDX_DIM).astype(jnp.float32)
    ki = ki.astype(jnp.float32)
    wi = wi.astype(jnp.float32) * IDX_HEADS ** -0.5
    table = rel_table.astype(jnp.float32)
    scale = A_HEAD_DIM ** -0.5

    def block(args):
        qb, qib, wib, qpos, qcid = args
        s = jnp.einsum('bqhd,bsd->bqhs', qib, ki) * IDX_DIM ** -0.5
        score = jnp.einsum('bqh,bqhs->bqs', wib, jax.nn.relu(s))
        vis = cid[None, :] <= qcid[:, None]
        score = jnp.where(vis[None], score, NEG_INF)
        _, idx = lax.top_k(score, k_top)
        ksel = jax.vmap(lambda kb, ib: kb[ib])(k, idx)
        vsel = jax.vmap(lambda vb, ib: vb[ib])(v, idx)
        valid = cid[idx] <= qcid[None, :, None]
        bias = table[rel_bucket(idx - qpos[None, :, None])]
        bias = bias.reshape(B, Q_BLOCK, k_top, A_KV_HEADS, rep).transpose(0, 1, 3, 4, 2)
        qg = qb.reshape(B, Q_BLOCK, A_KV_HEADS, rep, A_HEAD_DIM)
        logits = jnp.einsum('bqgrd,bqkgd->bqgrk', qg, ksel).astype(jnp.float32) * scale + bias
        logits = jnp.where(valid[:, :, None, None, :], logits, NEG_INF)
        p = jax.nn.softmax(logits, axis=-1).astype(v.dtype)
        o = jnp.einsum('bqgrk,bqkgd->bqgrd', p, vsel)
        return o.reshape(B, Q_BLOCK, A_Q)

    out = lax.map(block, (to_blocks(q, nblk), to_blocks(qi, nblk), to_blocks(wi, nblk),
                          pos.reshape(nblk, Q_BLOCK), cid.reshape(nblk, Q_BLOCK)))
    out = out.swapaxes(0, 1).reshape(B, Tp, A_Q)
    return out @ w_o


def diff_attention(h, w_q, w_o, gq, sub_g, k1, k2, vb, lam, lam_init, rel_table, cid, pos):
    B, Tp, _ = h.shape
    nblk = Tp // Q_BLOCK
    q = h @ w_q
    q1, q2 = jnp.split(q, 2, axis=-1)
    q1 = rms_norm(q1.reshape(B, Tp, N_HEADS, B_HEAD_DIM), gq)
    q2 = rms_norm(q2.reshape(B, Tp, N_HEADS, B_HEAD_DIM), gq)
    table = rel_table.astype(jnp.float32)
    scale = B_HEAD_DIM ** -0.5

    def block(args):
        q1b, q2b, qpos, qcid = args
        bias = table[rel_bucket(pos[None, :] - qpos[:, None])].transpose(2, 0, 1)
        mask = cid[None, :] <= qcid[:, None]

        def attn_map(qb, kk):
            lg = jnp.einsum('bqhd,bshd->bhqs', qb, kk).astype(jnp.float32) * scale + bias
            return jax.nn.softmax(jnp.where(mask, lg, NEG_INF), axis=-1)

        a = attn_map(q1b, k1) - lam * attn_map(q2b, k2)
        return jnp.einsum('bhqs,bshd->bqhd', a.astype(vb.dtype), vb)

    o = lax.map(block, (to_blocks(q1, nblk), to_blocks(q2, nblk),
                        pos.reshape(nblk, Q_BLOCK), cid.reshape(nblk, Q_BLOCK)))
    o = o.swapaxes(0, 1).reshape(B, Tp, N_HEADS, B_V_DIM)
    o = rms_norm(o, sub_g) * (1.0 - lam_init)
    return o.reshape(B, Tp, N_HEADS * B_V_DIM) @ w_o


def conv_ffn(h, w_up, conv_w, conv_b, w_down):
    Tp = h.shape[1]
    u = h @ w_up
    up = jnp.pad(u, ((0, 0), (CONV_W - 1, 0), (0, 0)))
    c = conv_b + sum(up[:, j:j + Tp] * conv_w[j] for j in range(CONV_W))
    gate, val = jnp.split(c, 2, axis=-1)
    return (jax.nn.silu(gate) * val) @ w_down


def setup_inputs(seed: int = 0) -> dict:
    key = jax.random.key(seed)
    ks = jax.random.split(key, 26)
    f32 = jnp.float32

    def nrm(k, shape, scale):
        return jax.random.normal(k, shape, f32) * scale

    def gain(k, shape):
        return 1.0 + 0.05 * jax.random.normal(k, shape, f32)

    return {
        "x": nrm(ks[0], (BATCH, SEQ, D_MODEL), 1.0),
        "meta_tokens": nrm(ks[1], (N_META, D_MODEL), 1.0),
        "rel_table": nrm(ks[2], (REL_BUCKETS, N_HEADS), 0.5),
        "ln_attn_g": gain(ks[3], (DEPTH, D_MODEL)),
        "ln_ffn_g": gain(ks[4], (DEPTH, D_MODEL)),
        "w_in_a": nrm(ks[5], (N_A, D_MODEL, A_IN), D_MODEL ** -0.5),
        "w_o_a": nrm(ks[6], (N_A, A_Q, D_MODEL), A_Q ** -0.5),
        "qn_a": gain(ks[7], (N_A, A_HEAD_DIM)),
        "kn_a": gain(ks[8], (N_A, A_HEAD_DIM)),
        "kv_norm_g": gain(ks[9], (D_MODEL,)),
        "w_kv_b": nrm(ks[10], (D_MODEL, B_KV), D_MODEL ** -0.5),
        "kn_b": gain(ks[11], (B_HEAD_DIM,)),
        "w_q_b": nrm(ks[12], (N_B, D_MODEL, B_Q), D_MODEL ** -0.5),
        "qn_b": gain(ks[13], (N_B, B_HEAD_DIM)),
        "lam_q1": nrm(ks[14], (N_B, B_HEAD_DIM), 0.1),
        "lam_k1": nrm(ks[15], (N_B, B_HEAD_DIM), 0.1),
        "lam_q2": nrm(ks[16], (N_B, B_HEAD_DIM), 0.1),
        "lam_k2": nrm(ks[17], (N_B, B_HEAD_DIM), 0.1),
        "subln_b": gain(ks[18], (N_B, B_V_DIM)),
        "w_o_b": nrm(ks[19], (N_B, N_HEADS * B_V_DIM, D_MODEL), (N_HEADS * B_V_DIM) ** -0.5),
        "w_up": nrm(ks[20], (DEPTH, D_MODEL, 2 * D_FF), D_MODEL ** -0.5),
        "conv_w": nrm(ks[21], (DEPTH, CONV_W, 2 * D_FF), CONV_W ** -0.5),
        "conv_b": nrm(ks[22], (DEPTH, 2 * D_FF), 0.01),
        "w_down": nrm(ks[23], (DEPTH, D_FF, D_MODEL), D_FF ** -0.5),
    }


def reference(x, meta_tokens, rel_table, ln_attn_g, ln_ffn_g, w_in_a, w_o_a, qn_a, kn_a,
              kv_norm_g, w_kv_b, kn_b, w_q_b, qn_b, lam_q1, lam_k1, lam_q2, lam_k2,
              subln_b, w_o_b, w_up, conv_w, conv_b, w_down):
    B, S, D = x.shape
    T = N_META + S
    Tp = -(-T // Q_BLOCK) * Q_BLOCK
    k_top = min(TOPK_MAX, S // 4)
    h = jnp.concatenate([jnp.broadcast_to(meta_tokens.astype(x.dtype), (B, N_META, D)), x], axis=1)
    h = jnp.pad(h, ((0, 0), (0, Tp - T), (0, 0)))
    cid = chunk_ids(Tp, T)
    pos = jnp.arange(Tp, dtype=jnp.int32)

    k1 = k2 = vb = None
    for i in range(DEPTH):
        if i < N_A:
            h = h + dsa_attention(rms_norm(h, ln_attn_g[i]), w_in_a[i], w_o_a[i], qn_a[i], kn_a[i],
                                  rel_table, cid, pos, k_top)
        else:
            j = i - N_A
            if j == 0:
                kv = rms_norm(h, kv_norm_g) @ w_kv_b
                k1, k2, vb = jnp.split(kv, [N_HEADS * B_HEAD_DIM, 2 * N_HEADS * B_HEAD_DIM], axis=-1)
                k1 = rms_norm(k1.reshape(B, Tp, N_HEADS, B_HEAD_DIM), kn_b)
                k2 = rms_norm(k2.reshape(B, Tp, N_HEADS, B_HEAD_DIM), kn_b)
                vb = vb.reshape(B, Tp, N_HEADS, B_V_DIM)
            lam_init = 0.8 - 0.6 * math.exp(-0.3 * i)
            lam = (jnp.exp(jnp.sum(lam_q1[j].astype(jnp.float32) * lam_k1[j].astype(jnp.float32)))
                   - jnp.exp(jnp.sum(lam_q2[j].astype(jnp.float32) * lam_k2[j].astype(jnp.float32)))
                   + lam_init)
            h = h + diff_attention(rms_norm(h, ln_attn_g[i]), w_q_b[j], w_o_b[j], qn_b[j], subln_b[j],
                                   k1, k2, vb, lam, lam_init, rel_table, cid, pos)
        h = h + conv_ffn(rms_norm(h, ln_ffn_g[i]), w_up[i], conv_w[i], conv_b[i], w_down[i])
    return h[:, N_META:T]
```

```python
import math
import contextlib
import numpy as np
import concourse.bass as bass
import concourse.mybir as mybir
from concourse.bass_utils import run_bass_kernel_spmd

F32 = mybir.dt.float32
BF16 = mybir.dt.bfloat16
AF = mybir.ActivationFunctionType
ALU = mybir.AluOpType
AX = mybir.AxisListType

D = 1024
S = 2048
NMETA = 16
T = S + NMETA
NT = 17
TP = NT * 128
KC = 8
NH = 8
A_IN = 2120
DFF = 2816
NFC = DFF // 128
EPS = 1e-6
NEG = -30000.0
NEGBIG = -1.0e30
TOPK = 256


class Res:
    __slots__ = ("name", "writers", "readers", "excl")

    def __init__(self, name="", excl=False):
        self.name = name
        self.writers = {}
        self.readers = {}
        self.excl = excl


class Op:
    __slots__ = ("eng", "fn", "dma", "deps", "signal", "sem", "idx")


class Prog:
    ENGS = ("pe", "act", "dve", "pool", "sp")
    NDSEM = 8

    def __init__(self, nc):
        self.nc = nc
        self.ops = []
        self.pending = {e: [] for e in self.ENGS}
        self.last_op = {e: None for e in self.ENGS}
        self.open_dma = []
        import os
        self.cut = int(os.environ.get("KCUT", "100000000"))

    def add(self, eng, fn, reads=(), writes=(), dma=0):
        if len(self.ops) >= self.cut:
            return None
        op = Op()
        op.eng = eng
        op.fn = fn
        op.dma = dma
        op.signal = bool(dma)
        op.sem = None
        op.idx = len(self.ops)
        key = ("dma", op.idx) if dma else eng
        deps = set(self.pending[eng])
        self.pending[eng] = []
        for r in reads:
            for k, w in r.writers.items():
                if k == key and eng == "pe":
                    continue
                deps.add(w)
            if r.excl:
                for k, w in r.readers.items():
                    if k != key:
                        deps.add(w)
        for r in writes:
            for k, w in r.writers.items():
                if k == key:
                    continue
                deps.add(w)
            for k, w in r.readers.items():
                if k == key:
                    continue
                deps.add(w)
        for r in reads:
            r.readers[key] = op.idx
        for r in writes:
            r.writers = {key: op.idx}
            r.readers = {}
        op.deps = sorted(deps)
        for d in op.deps:
            self.ops[d].signal = True
        self.ops.append(op)
        if dma:
            self.open_dma.append(op.idx)
        else:
            self.last_op[eng] = op.idx
        return op

    def barrier(self):
        deps = [v for v in self.last_op.values() if v is not None] + list(self.open_dma)
        for d in deps:
            self.ops[d].signal = True
        for e in self.ENGS:
            self.pending[e] = sorted(set(self.pending[e]) | set(deps))
        self.open_dma = []

    def emit(self, es):
        nc = self.nc
        engs = {"pe": nc.tensor, "act": nc.scalar, "dve": nc.vector, "pool": nc.gpsimd, "sp": nc.sync}
        esem = {e: es.enter_context(nc.semaphore("s_" + e)) for e in self.ENGS}
        ecnt = {e: 0 for e in self.ENGS}
        dsem = {e: [es.enter_context(nc.semaphore("d_%s%d" % (e, i))) for i in range(self.NDSEM)]
                for e in ("sp", "pool", "act")}
        dval = {e: [0] * self.NDSEM for e in dsem}
        dcnt = {e: 0 for e in dsem}
        seen = {e: {} for e in self.ENGS}
        semid = {}

        def wait(e, sem, val):
            k = id(sem)
            semid[k] = sem
            if seen[e].get(k, 0) < val:
                engs[e].wait_ge(sem, val)
                seen[e][k] = val

        for op in self.ops:
            e = op.eng
            E = engs[e]
            for d in op.deps:
                sem, val = self.ops[d].sem
                wait(e, sem, val)
            if op.dma:
                n = dcnt[e]
                dcnt[e] += 1
                slot = n % self.NDSEM
                sem = dsem[e][slot]
                wait(e, sem, dval[e][slot])
                inss = op.fn(E)
                if not isinstance(inss, (list, tuple)):
                    inss = [inss]
                for ins in inss:
                    ins.then_inc(sem, 16)
                dval[e][slot] += 16 * len(inss)
                op.sem = (sem, dval[e][slot])
            else:
                ins = op.fn(E)
                if op.signal:
                    ecnt[e] += 1
                    ins.then_inc(esem[e], 1)
                    op.sem = (esem[e], ecnt[e])
        for d in self.pending["sp"]:
            sem, val = self.ops[d].sem
            wait("sp", sem, val)


def _rel_bucket_np(rel):
    nb = 16
    max_exact = 8
    n = np.abs(rel)
    nf = np.maximum(n, 1).astype(np.float32)
    large = max_exact + (np.log(nf / np.float32(max_exact)) / np.float32(math.log(128 / max_exact))
                         * np.float32(nb - max_exact)).astype(np.int32)
    large = np.minimum(large, nb - 1)
    return np.where(rel > 0, nb, 0) + np.where(n < max_exact, n, large)


def _static_tables():
    s_l = np.arange(128)[:, None]
    t_l = np.arange(128)[None, :]
    lim = np.where(t_l < 16, 16, np.where(t_l < 80, 80, 128))
    idx = np.zeros((3, 128, 128), np.int64)
    for o, off in enumerate((-1, 0, 1)):
        rel = 128 * off + s_l - t_l
        b = _rel_bucket_np(rel)
        if off == -1:
            vis = np.ones((128, 128), bool)
        elif off == 0:
            vis = s_l < lim
        else:
            vis = (t_l >= 80) & (s_l < 16)
        idx[o] = np.where(vis, b, 32)
    tt = np.arange(128)[:, None]
    ss = np.arange(256)[None, :]
    limt = np.where(tt < 16, 16, np.where(tt < 80, 80, 144))
    negvis = np.where(ss < limt, 0.0, NEGBIG).astype(np.float32)
    return idx, negvis


def build_nc(dbg_stage=None, dbg_tiles=NT):
    nc = bass.Bass("TRN2", target_bir_lowering=False)

    def din(name, shape):
        return nc.dram_tensor(name, list(shape), F32, kind="ExternalInput").ap()

    x_d = din("x", (S, D))
    meta_d = din("meta_tokens", (NMETA, D))
    gnorm_d = din("gnorm", (5, D))
    w_in_d = din("w_in_a", (D, A_IN))
    w_o_a_d = din("w_o_a", (D, D))
    w_kv_d = din("w_kv_b", (D, 2048))
    w_q_d = din("w_q_b", (D, D))
    w_o_b_d = din("w_o_b", (D, D))
    w_up_d = din("w_up", (2, D, 2 * DFF))
    w_down_d = din("w_down", (2, DFF, D))
    convw_d = din("conv_wT", (2, 128, 2 * NFC * 3))
    convb_d = din("conv_bT", (2, 128, 2 * NFC))
    vecs_d = din("vecs", (8, 128))
    lam_d = din("lamv", (4, 64))
    addm_d = din("addmask", (128, 3 * NH * 128))
    cfar_d = din("cfar", (1, NH))
    negvis_d = din("negvis", (128, 256))
    ident_d = din("ident", (128, 128))
    out_d = nc.dram_tensor("out", [S, D], F32, kind="ExternalOutput").ap()

    es = contextlib.ExitStack()
    P = Prog(nc)
    lam_init = 0.8 - 0.6 * math.exp(-0.3 * 1)

    def sb(scope, name, shape, dt):
        return scope.enter_context(nc.sbuf_tensor("sb_" + name, list(shape), dt))

    h = sb(es, "h", (128, NT, D), F32)
    h_res = [Res("h%d" % i) for i in range(NT)]
    ident = sb(es, "ident", (128, 128), BF16)
    ident_r = Res("ident")
    gb = sb(es, "gb", (128, D), F32)
    gb_r = Res("gb")
    vecs = sb(es, "vecs", (128, 8), F32)
    vecs_r = Res("vecs")
    addm = sb(es, "addm", (128, 3, NH, 128), BF16)
    addm_r = Res("addm")
    cfar = sb(es, "cfar", (128, NH), F32)
    cfar_r = Res("cfar")
    negvis = sb(es, "negvis", (128, 256), F32)
    negvis_r = Res("negvis")
    small = sb(es, "small", (128, 64), F32)
    init_sc = contextlib.ExitStack()
    addm_f = sb(init_sc, "addm_f", (128, 3, NH, 128), F32)
    pp = [es.enter_context(nc.psum_tensor("pp%d" % i, [128, 512], F32)) for i in range(2)]
    pp_r = [Res("pp%d" % i, True) for i in range(2)]
    psc = [es.enter_context(nc.psum_tensor("psc%d" % i, [128, 512], F32)) for i in range(2)]
    psc_r = [Res("psc%d" % i, True) for i in range(2)]
    po = [es.enter_context(nc.psum_tensor("po%d" % i, [128, 512], F32)) for i in range(2)]
    po_r = [Res("po%d" % i, True) for i in range(2)]
    ptr = es.enter_context(nc.psum_tensor("ptr", [128, 1024], BF16))
    ptr_r = Res("ptr", True)
    py = es.enter_context(nc.psum_tensor("py", [128, 512], F32))
    py_r = Res("py", True)

    cnt = {"pp": 0, "psc": 0}

    def next_pp():
        i = cnt["pp"] % 2
        cnt["pp"] += 1
        return pp[i], pp_r[i]

    def next_psc():
        i = cnt["psc"] % 2
        cnt["psc"] += 1
        return psc[i], psc_r[i]

    P.add("sp", lambda E: [
        E.dma_start(out=h[0:16, 0, :], in_=meta_d),
        E.dma_start(out=h[16:128, 0, :], in_=x_d[0:112, :]),
    ], writes=[h_res[0]], dma=1)
    for i in range(1, 16):
        P.add("sp", (lambda i: lambda E: E.dma_start(out=h[:, i, :], in_=x_d[128 * i - 16:128 * i + 112, :]))(i),
              writes=[h_res[i]], dma=1)
    P.add("dve", lambda E: E.memset(h[:, 16, :], 0.0), writes=[h_res[16]])
    P.add("sp", lambda E: E.dma_start(out=h[0:16, 16, :], in_=x_d[2032:2048, :]), writes=[h_res[16]], dma=1)
    P.add("pool", lambda E: E.dma_start(out=ident[:], in_=ident_d), writes=[ident_r], dma=1)
    P.add("sp", lambda E: E.dma_start(out=negvis[:], in_=negvis_d), writes=[negvis_r], dma=1)
    P.add("sp", lambda E: E.dma_start(out=addm_f[:].rearrange("p a h t -> p (a h t)"), in_=addm_d),
          writes=[addm_r], dma=1)
    P.add("sp", lambda E: E.dma_start(out=cfar[:], in_=cfar_d.partition_broadcast(128)), writes=[cfar_r], dma=1)
    for hh in range(NH):
        P.add("dve", (lambda hh: lambda E: E.tensor_scalar(
            out=addm[:, :, hh, :], in0=addm_f[:, :, hh, :], scalar1=cfar[:, hh:hh + 1], scalar2=None,
            op0=ALU.subtract))(hh), reads=[addm_r, cfar_r], writes=[Res()])
    addm_ready = Res("addm_ready")
    P.add("dve", lambda E: E.memset(small[:, 0:1], 0.0), reads=[], writes=[addm_ready])
    P.barrier()
    init_sc.close()

    def load_gain(idx):
        P.add("sp", lambda E: E.dma_start(out=gb[:], in_=gnorm_d[idx:idx + 1, :].partition_broadcast(128)),
              writes=[gb_r], dma=1)

    def rms_to_xnT(i, xn_tok, xn_tok_r, dst_fn, dst_res, scr, scr_r, st, st_r):
        P.add("act", lambda E: E.activation(out=scr[:], in_=h[:, i, :], func=AF.Square, accum_out=st[:, 0:1]),
              reads=[h_res[i]], writes=[scr_r, st_r])
        P.add("dve", lambda E: E.tensor_scalar(out=st[:, 1:2], in0=st[:, 0:1], scalar1=1.0 / D, scalar2=EPS,
                                               op0=ALU.mult, op1=ALU.add), reads=[st_r], writes=[st_r])
        P.add("act", lambda E: E.activation(out=st[:, 2:3], in_=st[:, 1:2], func=AF.Sqrt), reads=[st_r], writes=[st_r])
        P.add("dve", lambda E: E.reciprocal(out=st[:, 3:4], in_=st[:, 2:3]), reads=[st_r], writes=[st_r])
        P.add("dve", lambda E: E.scalar_tensor_tensor(out=xn_tok[:], in0=h[:, i, :], scalar=st[:, 3:4], in1=gb[:],
                                                      op0=ALU.mult, op1=ALU.mult),
              reads=[h_res[i], st_r, gb_r], writes=[xn_tok_r])

        def tr(E):
            ins = None
            for kc in range(KC):
                ins = E.transpose(out=ptr[:, kc * 128:(kc + 1) * 128], in_=xn_tok[:, kc * 128:(kc + 1) * 128],
                                  identity=ident[:])
            return ins
        P.add("pe", tr, reads=[xn_tok_r, ident_r], writes=[ptr_r])
        P.add("act", lambda E: E.copy(out=dst_fn(), in_=ptr[:].rearrange("p (k t) -> p k t", k=KC)),
              reads=[ptr_r], writes=[dst_res])

    def layer0_attention():
        sc = contextlib.ExitStack()
        w_in = sb(sc, "w_in", (128, KC, A_IN), BF16)
        w_in_r = [Res("w_in%d" % k) for k in range(KC)]
        w_o = sb(sc, "w_o", (128, KC, D), BF16)
        w_o_r = Res("w_o")
        kT = sb(sc, "kT", (128, 2, TP), BF16)
        kT_r = [Res("kT%d" % i) for i in range(NT)]
        vaug = sb(sc, "vaug", (128, NT, 2, 129), BF16)
        v_r = [Res("v%d" % i) for i in range(NT)]
        kiT = sb(sc, "kiT", (128, TP), BF16)
        kiT_r = [Res("kiT%d" % i) for i in range(NT)]
        NB = 2
        xn_tok = [sb(sc, "xn_tok%d" % b, (128, D), BF16) for b in range(1)] * NB
        xn_tok_r = [Res() for b in range(1)] * NB
        xnT = [sb(sc, "xnT%d" % b, (128, KC, 128), BF16) for b in range(1)] * NB
        xnT_r = [Res() for b in range(1)] * NB
        scr = sb(sc, "scr", (128, D), BF16)
        scr_r = Res("scr")
        st = [sb(sc, "st%d" % b, (128, 64), F32) for b in range(NB)]
        st_r = [Res() for b in range(NB)]
        qs = [sb(sc, "qs%d" % b, (128, NH, 128), BF16) for b in range(1)] * NB
        qs_r = [Res() for b in range(1)] * NB
        ks = [sb(sc, "ks%d" % b, (128, 2, 128), BF16) for b in range(1)] * NB
        ks_r = [Res() for b in range(1)] * NB
        qT = [sb(sc, "qT%d" % b, (128, NH, 128), BF16) for b in range(NB)]
        qT_r = [Res() for b in range(NB)]
        qis = [sb(sc, "qis%d" % b, (128, NH * 64), BF16) for b in range(1)] * NB
        qis_r = [Res() for b in range(1)] * NB
        qiT = [sb(sc, "qiT%d" % b, (128, 4, 128), BF16) for b in range(NB)]
        qiT_r = [Res() for b in range(NB)]
        kis = [sb(sc, "kis%d" % b, (128, 128), BF16) for b in range(1)] * NB
        kis_r = [Res() for b in range(1)] * NB
        wst = [sb(sc, "wst%d" % b, (128, 32), F32) for b in range(NB)]
        wst_r = [Res() for b in range(NB)]
        acc = sb(sc, "acc", (128, TP), F32)
        acc_r = Res("acc")
        work = sb(sc, "work", (128, TP), F32)
        work_r = Res("work")
        rbuf = [sb(sc, "rbuf%d" % b, (128, 512), F32) for b in range(2)]
        rbuf_r = [Res() for b in range(2)]
        m8 = sb(sc, "m8", (128, 8), F32)
        m8_r = Res("m8")
        tb = sb(sc, "tb", (128, 32), F32)
        tb_r = Res("tb")
        iota8 = sb(sc, "iota8", (128, 8), F32)
        iota_r = Res("iota8")
        for q8 in range(8):
            P.add("pool", (lambda q8=q8: lambda E: E.memset(iota8[:, q8:q8 + 1], float(q8)))(), writes=[iota_r])
        sel = work[:].bitcast(BF16)
        sel_r = work_r
        selT = sb(sc, "selT", (128, NT, 128), BF16)
        selT_r = Res("selT")
        pT = [sb(sc, "pT%d" % b, (128, 512), BF16) for b in range(3)]
        pT_r = [Res() for b in range(3)]
        otok = sb(sc, "otok", (128, D), BF16)
        otok_r = Res("otok")
        oT = sb(sc, "oT", (128, KC, 128), BF16)
        oT_r = Res("oT")
        rz = sb(sc, "rz", (128, 8), F32)
        rz_r = Res("rz")
        gq = sb(sc, "gq", (128, 2), F32)
        gq_r = Res("gq")

        for kc in range(KC):
            P.add("pool", (lambda kc: lambda E: E.dma_start(out=w_in[:, kc, :], in_=w_in_d[kc * 128:(kc + 1) * 128, :]))(kc),
                  writes=[w_in_r[kc]], dma=1)
        P.add("pool", lambda E: [E.dma_start(out=w_o[:, kc, :], in_=w_o_a_d[kc * 128:(kc + 1) * 128, :]) for kc in range(KC)],
              writes=[w_o_r], dma=1)
        load_gain(0)
        P.add("sp", lambda E: E.dma_start(out=gq[:], in_=vecs_d[0:2, :].rearrange("v p -> p v"),
                                          allow_slow_non_contiguous=True), writes=[gq_r], dma=1)
        P.add("dve", lambda E: E.tensor_scalar(out=gq[:, 0:1], in0=gq[:, 0:1], scalar1=128.0 ** -0.5, scalar2=None,
                                               op0=ALU.mult), reads=[gq_r], writes=[gq_r])
        P.add("pool", lambda E: E.memset(vaug[:, :, :, 128:129], 1.0), writes=v_r)

        def proj_tile(i):
            b = i % NB
            rms_to_xnT(i, xn_tok[b], xn_tok_r[b], lambda: xnT[b][:], xnT_r[b], scr, scr_r, st[b], st_r[b])
            for c, (c0, cw) in enumerate(((0, 512), (512, 512), (1024, 512), (1536, 512), (2048, 72))):
                pt, pr = next_pp()

                def mm(E, c0=c0, cw=cw, pt=pt):
                    ins = None
                    for kc in range(KC):
                        ins = E.matmul(pt[:, 0:cw], xnT[b][:, kc, :], w_in[:, kc, c0:c0 + cw],
                                       start=(kc == 0), stop=(kc == KC - 1))
                    return ins
                P.add("pe", mm, reads=[xnT_r[b]] + w_in_r, writes=[pr])
                if c in (0, 1, 2):
                    nh = 4 if c < 2 else 2
                    so = 8 + c * 4
                    for hh in range(nh):
                        P.add("act", (lambda hh, pt=pt, so=so: lambda E: E.activation(
                            out=scr[:, hh * 128:(hh + 1) * 128], in_=pt[:, hh * 128:(hh + 1) * 128], func=AF.Square,
                            accum_out=st[b][:, so + hh:so + hh + 1]))(hh),
                            reads=[pr], writes=[scr_r, st_r[b]])
                    P.add("dve", (lambda so=so, nh=nh: lambda E: E.tensor_scalar(
                        out=st[b][:, 24 + so:24 + so + nh], in0=st[b][:, so:so + nh], scalar1=1.0 / 128, scalar2=EPS,
                        op0=ALU.mult, op1=ALU.add))(), reads=[st_r[b]], writes=[st_r[b]])
                    P.add("act", (lambda so=so, nh=nh: lambda E: E.activation(
                        out=st[b][:, so:so + nh], in_=st[b][:, 24 + so:24 + so + nh], func=AF.Sqrt))(),
                        reads=[st_r[b]], writes=[st_r[b]])
                    P.add("dve", (lambda so=so, nh=nh: lambda E: E.reciprocal(
                        out=st[b][:, 24 + so:24 + so + nh], in_=st[b][:, so:so + nh]))(),
                        reads=[st_r[b]], writes=[st_r[b]])
                    for hh in range(nh):
                        if c < 2:
                            dst = qs[b][:, c * 4 + hh, :]
                            dr = qs_r[b]
                        else:
                            dst = ks[b][:, hh, :]
                            dr = ks_r[b]
                        P.add("dve", (lambda hh, dst=dst, pt=pt, so=so: lambda E: E.tensor_scalar(
                            out=dst, in0=pt[:, hh * 128:(hh + 1) * 128], scalar1=st[b][:, 24 + so + hh:24 + so + hh + 1],
                            scalar2=None, op0=ALU.mult))(hh), reads=[pr, st_r[b]], writes=[dr])
                    if c == 2:
                        P.add("act", (lambda pt=pt: lambda E: E.copy(
                            out=vaug[:, i, :, 0:128], in_=pt[:, 256:512].rearrange("p (g d) -> p g d", g=2)))(),
                            reads=[pr], writes=[v_r[i]])
                elif c == 3:
                    pass
                    qi_pt, qi_pr = pt, pr
                else:
                    P.add("dve", (lambda pt=pt: lambda E: E.tensor_scalar(
                        out=wst[b][:, 0:8], in0=pt[:, 64:72], scalar1=0.0, scalar2=2.0, op0=ALU.is_gt, op1=ALU.mult))(),
                        reads=[pr], writes=[wst_r[b]])
                    P.add("dve", lambda E: E.tensor_scalar(
                        out=wst[b][:, 0:8], in0=wst[b][:, 0:8], scalar1=-1.0, scalar2=None, op0=ALU.add),
                        reads=[wst_r[b]], writes=[wst_r[b]])
                    P.add("dve", (lambda pt=pt: lambda E: E.scalar_tensor_tensor(
                        out=wst[b][:, 8:16], in0=pt[:, 64:72], scalar=(8.0 ** -0.5) * (64.0 ** -0.5), in1=wst[b][:, 0:8],
                        op0=ALU.mult, op1=ALU.mult))(), reads=[pr, wst_r[b]], writes=[wst_r[b]])
                    P.add("act", (lambda pt=pt: lambda E: E.copy(out=kis[b][:, 0:64], in_=pt[:, 0:64]))(),
                          reads=[pr], writes=[kis_r[b]])
                    P.add("act", (lambda pt=pt: lambda E: E.copy(out=kis[b][:, 64:128], in_=pt[:, 0:64]))(),
                          reads=[pr], writes=[kis_r[b]])
                    for hh in range(NH):
                        P.add("dve", (lambda hh, qi_pt=qi_pt: lambda E: E.tensor_scalar(
                            out=qis[b][:, hh * 64:(hh + 1) * 64], in0=qi_pt[:, hh * 64:(hh + 1) * 64],
                            scalar1=wst[b][:, 8 + hh:9 + hh], scalar2=None, op0=ALU.mult))(hh),
                            reads=[qi_pr, wst_r[b]], writes=[qis_r[b]])
            def trq(E):
                ins = None
                for hh in range(NH):
                    ins = E.transpose(out=ptr[:, hh * 128:(hh + 1) * 128], in_=qs[b][:, hh, :], identity=ident[:])
                return ins
            P.add("pe", trq, reads=[qs_r[b], ident_r], writes=[ptr_r])
            P.add("act", lambda E: E.activation(out=qT[b][:], in_=ptr[:].rearrange("p (k t) -> p k t", k=NH),
                                                func=AF.Copy, scale=gq[:, 0:1]),
                  reads=[ptr_r, gq_r], writes=[qT_r[b]])

            def trk(E):
                ins = None
                for g in range(2):
                    ins = E.transpose(out=ptr[:, g * 128:(g + 1) * 128], in_=ks[b][:, g, :], identity=ident[:])
                for hp in range(4):
                    ins = E.transpose(out=ptr[:, 256 + hp * 128:256 + (hp + 1) * 128],
                                      in_=qis[b][:, hp * 128:(hp + 1) * 128], identity=ident[:])
                ins = E.transpose(out=ptr[:, 768:896], in_=kis[b][:], identity=ident[:])
                return ins
            P.add("pe", trk, reads=[ks_r[b], qis_r[b], kis_r[b], ident_r], writes=[ptr_r])
            P.add("act", lambda E: E.activation(out=kT[:, :, i * 128:(i + 1) * 128],
                                                in_=ptr[:, 0:256].rearrange("p (g t) -> p g t", g=2),
                                                func=AF.Copy, scale=gq[:, 1:2]),
                  reads=[ptr_r, gq_r], writes=[kT_r[i]])
            P.add("dve", lambda E: E.tensor_copy(out=qiT[b][:], in_=ptr[:, 256:768].rearrange("p (k t) -> p k t", k=4)),
                  reads=[ptr_r], writes=[qiT_r[b]])
            P.add("dve", lambda E: E.tensor_copy(out=kiT[:, i * 128:(i + 1) * 128], in_=ptr[:, 768:896]),
                  reads=[ptr_r], writes=[kiT_r[i]])

        def attn_tile(i):
            b = i % NB
            jmax = min(i + 1, NT - 1)
            nk = 128 * (jmax + 1)
            if i > 0:
                P.add("pool", lambda E: E.memset(acc[:, 0:128 * i], 0.0), writes=[acc_r])
            wv = nk - 128 * i
            P.add("pool", lambda E: E.tensor_copy(out=acc[:, 128 * i:nk], in_=negvis[:, 0:wv]),
                  reads=[negvis_r], writes=[acc_r])
            nchunk = (nk + 511) // 512
            ri = 0
            for hh in range(NH):
                for cch in range(nchunk):
                    c0 = cch * 512
                    cw = min(512, nk - c0)
                    pt, pr = next_psc()
                    po_ = (hh % 2) * 64
                    P.add("pe", (lambda pt=pt, c0=c0, cw=cw, hh=hh, po_=po_: lambda E: E.matmul(
                        pt[:, 0:cw], qiT[b][po_:po_ + 64, hh // 2, :], kiT[po_:po_ + 64, c0:c0 + cw],
                        start=True, stop=True))(),
                        reads=[qiT_r[b]] + kiT_r[0:jmax + 1], writes=[pr])
                    rb, rr = rbuf[ri % 2], rbuf_r[ri % 2]
                    ri += 1
                    P.add("act", (lambda pt=pt, cw=cw, rb=rb: lambda E: E.activation(
                        out=rb[:, 0:cw], in_=pt[:, 0:cw], func=AF.Relu))(), reads=[pr], writes=[rr])
                    P.add("dve", (lambda c0=c0, cw=cw, rb=rb, hh=hh: lambda E: E.scalar_tensor_tensor(
                        out=acc[:, c0:c0 + cw], in0=rb[:, 0:cw], scalar=wst[b][:, hh:hh + 1], in1=acc[:, c0:c0 + cw],
                        op0=ALU.mult, op1=ALU.add))(), reads=[rr, wst_r[b], acc_r], writes=[acc_r])
            if i < 2:
                src = acc
                src_r = acc_r
                for it in range(TOPK // 8):
                    P.add("dve", (lambda src=src: lambda E: E.max(out=m8[:], in_=src[:, 0:nk]))(),
                          reads=[src_r], writes=[m8_r])
                    if it < TOPK // 8 - 1:
                        P.add("dve", (lambda src=src: lambda E: E.match_replace(
                            out=work[:, 0:nk], in_to_replace=m8[:], in_values=src[:, 0:nk], imm_value=-3.0e38))(),
                            reads=[src_r, m8_r], writes=[work_r])
                        src = work
                        src_r = work_r
                thr_ap = m8[:, 7:8]
                thr_res = m8_r
            else:
                NB_IT = 12
                P.add("dve", lambda E: E.tensor_reduce(out=tb[:, 0:1], in_=acc[:, 0:nk], axis=AX.X, op=ALU.max),
                      reads=[acc_r], writes=[tb_r])
                P.add("dve", lambda E: E.tensor_reduce(out=tb[:, 1:2], in_=acc[:, 0:128 * i], axis=AX.X, op=ALU.min),
                      reads=[acc_r], writes=[tb_r])
                P.add("dve", lambda E: E.tensor_tensor(out=tb[:, 2:3], in0=tb[:, 0:1], in1=tb[:, 1:2], op=ALU.subtract),
                      reads=[tb_r], writes=[tb_r])
                for it in range(NB_IT):
                    cc = 0.5 ** (it + 1)
                    P.add("dve", (lambda cc=cc: lambda E: E.scalar_tensor_tensor(
                        out=tb[:, 3:4], in0=tb[:, 2:3], scalar=cc, in1=tb[:, 1:2], op0=ALU.mult, op1=ALU.add))(),
                        reads=[tb_r], writes=[tb_r])
                    P.add("dve", lambda E: E.tensor_scalar(
                        out=sel[:, 0:nk], in0=acc[:, 0:nk], scalar1=tb[:, 3:4], scalar2=None, op0=ALU.is_ge,
                        op1=ALU.add, accum_out=tb[:, 4:5]), reads=[acc_r, tb_r], writes=[work_r, tb_r])
                    P.add("dve", (lambda cc=cc: lambda E: E.tensor_scalar(
                        out=tb[:, 5:6], in0=tb[:, 4:5], scalar1=TOPK - 0.5, scalar2=cc, op0=ALU.is_ge, op1=ALU.mult))(),
                        reads=[tb_r], writes=[tb_r])
                    P.add("dve", lambda E: E.scalar_tensor_tensor(
                        out=tb[:, 1:2], in0=tb[:, 2:3], scalar=tb[:, 5:6], in1=tb[:, 1:2], op0=ALU.mult, op1=ALU.add),
                        reads=[tb_r], writes=[tb_r])
                P.add("dve", lambda E: E.scalar_tensor_tensor(
                    out=tb[:, 6:7], in0=tb[:, 2:3], scalar=0.5 ** NB_IT, in1=tb[:, 1:2], op0=ALU.mult, op1=ALU.add),
                    reads=[tb_r], writes=[tb_r])
                P.add("dve", lambda E: E.tensor_scalar(
                    out=sel[:, 0:nk], in0=acc[:, 0:nk], scalar1=tb[:, 6:7], scalar2=None, op0=ALU.is_ge,
                    op1=ALU.add, accum_out=tb[:, 7:8]), reads=[acc_r, tb_r], writes=[work_r, tb_r])
                P.add("dve", lambda E: E.tensor_scalar(
                    out=work[:, 0:nk], in0=acc[:, 0:nk], scalar1=tb[:, 6:7], scalar2=-1.0, op0=ALU.is_lt, op1=ALU.add),
                    reads=[acc_r, tb_r], writes=[work_r])
                P.add("dve", lambda E: E.scalar_tensor_tensor(
                    out=work[:, 0:nk], in0=work[:, 0:nk], scalar=3.0e38, in1=acc[:, 0:nk], op0=ALU.mult, op1=ALU.add),
                    reads=[acc_r, work_r], writes=[work_r])
                P.add("dve", lambda E: E.max(out=m8[:], in_=work[:, 0:nk]), reads=[work_r], writes=[m8_r])
                P.add("dve", lambda E: E.tensor_scalar(
                    out=tb[:, 8:9], in0=tb[:, 7:8], scalar1=-1.0, scalar2=float(TOPK - 1), op0=ALU.mult, op1=ALU.add),
                    reads=[tb_r], writes=[tb_r])
                P.add("dve", lambda E: E.tensor_scalar(
                    out=tb[:, 8:9], in0=tb[:, 8:9], scalar1=0.0, scalar2=7.0, op0=ALU.max, op1=ALU.min),
                    reads=[tb_r], writes=[tb_r])
                P.add("dve", lambda E: E.tensor_scalar(
                    out=tb[:, 16:24], in0=iota8[:], scalar1=tb[:, 8:9], scalar2=None, op0=ALU.is_equal),
                    reads=[tb_r, iota_r], writes=[tb_r])
                P.add("dve", lambda E: E.tensor_tensor(out=tb[:, 16:24], in0=tb[:, 16:24], in1=m8[:], op=ALU.mult),
                      reads=[tb_r, m8_r], writes=[tb_r])
                P.add("dve", lambda E: E.tensor_reduce(out=tb[:, 9:10], in_=tb[:, 16:24], axis=AX.X, op=ALU.add),
                      reads=[tb_r], writes=[tb_r])
                thr_ap = tb[:, 9:10]
                thr_res = tb_r
            P.add("dve", (lambda thr_ap=thr_ap: lambda E: E.tensor_scalar(
                out=sel[:, 0:nk], in0=acc[:, 0:nk], scalar1=thr_ap, scalar2=None, op0=ALU.is_ge))(),
                reads=[acc_r, thr_res], writes=[sel_r])
            for j0 in range(0, jmax + 1, 8):
                j1 = min(jmax + 1, j0 + 8)

                def trs(E, j0=j0, j1=j1):
                    ins = None
                    for j in range(j0, j1):
                        ins = E.transpose(out=ptr[:, (j - j0) * 128:(j - j0 + 1) * 128], in_=sel[:, j * 128:(j + 1) * 128],
                                          identity=ident[:])
                    return ins
                P.add("pe", trs, reads=[sel_r, ident_r], writes=[ptr_r])
                P.add("act", (lambda j0=j0, j1=j1: lambda E: E.copy(
                    out=selT[:, j0:j1, :], in_=ptr[:, 0:(j1 - j0) * 128].rearrange("p (k t) -> p k t", k=j1 - j0)))(),
                    reads=[ptr_r], writes=[selT_r])
            pi = 0
            for g in range(2):
                for j in range(jmax + 1):
                    pt, pr = next_psc()
                    off = j - i
                    near = off >= -1

                    def mm(E, pt=pt, j=j, g=g, off=off, near=near):
                        ins = E.matmul(pt[:].rearrange("p (k t) -> p k t", k=4), kT[:, g, j * 128:(j + 1) * 128],
                                       qT[b][:, g * 4:(g + 1) * 4, :], start=True, stop=not near)
                        if near:
                            ins = E.matmul(pt[:].rearrange("p (k t) -> p k t", k=4), ident[:],
                                           addm[:, off + 1, g * 4:(g + 1) * 4, :], start=False, stop=True)
                        return ins
                    P.add("pe", mm, reads=[kT_r[j], qT_r[b], ident_r, addm_ready], writes=[pr])
                    pb, pbr = pT[pi % 3], pT_r[pi % 3]
                    pi += 1
                    P.add("act", (lambda pt=pt, pb=pb: lambda E: E.activation(out=pb[:], in_=pt[:], func=AF.Exp))(),
                          reads=[pr], writes=[pbr])
                    P.add("dve", (lambda pb=pb, j=j: lambda E: E.tensor_tensor(
                        out=pb[:].rearrange("p (k t) -> p k t", k=4), in0=pb[:].rearrange("p (k t) -> p k t", k=4),
                        in1=selT[:, j:j + 1, :].to_broadcast([128, 4, 128]), op=ALU.mult))(),
                        reads=[pbr, selT_r], writes=[pbr])

                    def pv(E, pb=pb, j=j, g=g):
                        ins = None
                        for hh in range(4):
                            ins = E.matmul(po[hh // 2][:, (hh % 2) * 256:(hh % 2) * 256 + 129],
                                           pb[:, hh * 128:(hh + 1) * 128], vaug[:, j, g, :],
                                           start=(j == 0 and hh % 2 == 0), stop=(j == jmax),
                                           skip_group_check=True)
                        return ins
                    P.add("pe", pv, reads=[pbr, v_r[j]], writes=po_r)
                for hb in range(2):
                    P.add("dve", (lambda hb=hb, g=g: lambda E: E.reciprocal(
                        out=rz[:, g * 4 + hb * 2:g * 4 + hb * 2 + 2],
                        in_=po[hb][:].rearrange("p (k c) -> p k c", k=2)[:, :, 128]))(),
                        reads=[po_r[hb]], writes=[rz_r])
                for hh in range(4):
                    P.add("dve", (lambda hh=hh, g=g: lambda E: E.tensor_scalar(
                        out=otok[:, (g * 4 + hh) * 128:(g * 4 + hh + 1) * 128],
                        in0=po[hh // 2][:, (hh % 2) * 256:(hh % 2) * 256 + 128],
                        scalar1=rz[:, g * 4 + hh:g * 4 + hh + 1], scalar2=None, op0=ALU.mult))(),
                        reads=[po_r[hh // 2], rz_r], writes=[otok_r])
            def tro(E):
                ins = None
                for kc in range(KC):
                    ins = E.transpose(out=ptr[:, kc * 128:(kc + 1) * 128], in_=otok[:, kc * 128:(kc + 1) * 128],
                                      identity=ident[:])
                return ins
            P.add("pe", tro, reads=[otok_r, ident_r], writes=[ptr_r])
            P.add("act", lambda E: E.copy(out=oT[:], in_=ptr[:].rearrange("p (k t) -> p k t", k=KC)),
                  reads=[ptr_r], writes=[oT_r])
            for nh_ in range(2):
                def mmo(E, nh_=nh_):
                    ins = None
                    for kc in range(KC):
                        ins = E.matmul(py[:], oT[:, kc, :], w_o[:, kc, nh_ * 512:(nh_ + 1) * 512],
                                       start=(kc == 0), stop=(kc == KC - 1))
                    return ins
                P.add("pe", mmo, reads=[oT_r, w_o_r], writes=[py_r])
                P.add("dve", (lambda nh_=nh_: lambda E: E.tensor_tensor(
                    out=h[:, i, nh_ * 512:(nh_ + 1) * 512], in0=py[:], in1=h[:, i, nh_ * 512:(nh_ + 1) * 512],
                    op=ALU.add))(), reads=[py_r, h_res[i]], writes=[h_res[i]])

        proj_tile(0)
        for i in range(dbg_tiles):
            if i + 1 < NT:
                proj_tile(i + 1)
            attn_tile(i)
        P.barrier()
        sc.close()

    def conv_ffn(l):
        sc = contextlib.ExitStack()
        sbl = lambda scope, name, shape, dt: sb(scope, "L%d_%s" % (l, name), shape, dt)
        G = 6
        groups = []
        c = 0
        while c < NFC:
            groups.append(list(range(c, min(NFC, c + G))))
            c += G
        xnT = sbl(sc, "f_xnT", (128, KC, TP), BF16)
        xnT_r = [Res() for i in range(NT)]
        xn_tok = sbl(sc, "f_xn_tok", (128, D), BF16)
        xn_tok_r = Res()
        scr = sbl(sc, "f_scr", (128, D), BF16)
        scr_r = Res()
        st = sbl(sc, "f_st", (128, 64), F32)
        st_r = Res()
        NWU = 3
        wup = [sbl(sc, "wup%d" % k, (128, KC, 256), BF16) for k in range(NWU)]
        wup_r = [Res() for k in range(NWU)]
        NGT = G + 2
        gT = [sbl(sc, "gT%d" % k, (128, TP), BF16) for k in range(NGT)]
        gT_r = [Res() for k in range(NGT)]
        wdn = [sbl(sc, "wdn%d" % k, (128, D), BF16) for k in range(NGT)]
        wdn_r = [Res() for k in range(NGT)]
        ub = [sbl(sc, "ub%d" % k, (128, TP + 2), F32) for k in range(2)]
        TCH = [(0, 512), (512, 512), (1024, 512), (1536, 512), (2048, 128)]
        ub_r = [[Res() for t in TCH] for k in range(2)]
        halo_r = [Res() for k in range(2)]
        cb = [[sbl(sc, "cb%d_%d" % (k, q), (128, 512), F32) for q in range(2)] for k in range(2)]
        cb_r = [[Res() for q in range(2)] for k in range(2)]
        cw_sb = sbl(sc, "cw_sb", (128, 2 * NFC * 3), F32)
        cb_sb = sbl(sc, "cb_sb", (128, 2 * NFC), F32)
        cws_r = Res()

        load_gain(1 if l == 0 else 4)
        P.add("sp", lambda E: [E.dma_start(out=cw_sb[:], in_=convw_d[l]), E.dma_start(out=cb_sb[:], in_=convb_d[l])],
              writes=[cws_r], dma=1)
        for k in range(2):
            P.add("pool", (lambda k: lambda E: E.memset(ub[k][:, 0:2], 0.0))(k), writes=[halo_r[k]])
        for i in range(NT):
            rms_to_xnT(i, xn_tok, xn_tok_r, (lambda i=i: xnT[:, :, i * 128:(i + 1) * 128]), xnT_r[i], scr, scr_r, st, st_r)

        slot = 0
        qq = 0
        for grp in groups:
            gslots = []
            for c in grp:
                wu, wur = wup[c % NWU], wup_r[c % NWU]
                sl = slot % NGT
                slot += 1
                gslots.append(sl)
                P.add("pool", (lambda c=c, wu=wu: lambda E: [
                    E.dma_start(out=wu[:, :, 0:128],
                                in_=w_up_d[l, :, c * 128:(c + 1) * 128].rearrange("(kc k) n -> k kc n", k=128)),
                    E.dma_start(out=wu[:, :, 128:256],
                                in_=w_up_d[l, :, DFF + c * 128:DFF + (c + 1) * 128].rearrange("(kc k) n -> k kc n", k=128)),
                ])(), writes=[wur], dma=1)
                P.add("pool", (lambda c=c, sl=sl: lambda E: E.dma_start(
                    out=wdn[sl][:], in_=w_down_d[l, c * 128:(c + 1) * 128, :]))(), writes=[wdn_r[sl]], dma=1)
                for ti, (t0, tw) in enumerate(TCH):
                    for k in range(2):
                        pt, pr = next_pp()

                        def mm(E, pt=pt, k=k, t0=t0, tw=tw, wu=wu):
                            ins = None
                            for kc in range(KC):
                                ins = E.matmul(pt[:, 0:tw], wu[:, kc, k * 128:(k + 1) * 128], xnT[:, kc, t0:t0 + tw],
                                               start=(kc == 0), stop=(kc == KC - 1))
                            return ins
                        P.add("pe", mm, reads=[wur] + xnT_r[t0 // 128:(t0 + tw) // 128], writes=[pr])
                        ci = k * NFC + c
                        cbuf, cbr = cb[k][qq % 2], cb_r[k][qq % 2]
                        P.add("act", (lambda pt=pt, k=k, t0=t0, tw=tw: lambda E: E.copy(
                            out=ub[k][:, 2 + t0:2 + t0 + tw], in_=pt[:, 0:tw]))(), reads=[pr], writes=[ub_r[k][ti]])
                        P.add("act", (lambda pt=pt, tw=tw, ci=ci, cbuf=cbuf: lambda E: E.activation(
                            out=cbuf[:, 0:tw], in_=pt[:, 0:tw], func=AF.Identity,
                            scale=cw_sb[:, ci * 3 + 2:ci * 3 + 3], bias=cb_sb[:, ci:ci + 1]))(),
                            reads=[pr, cws_r], writes=[cbr])
                        prev = [ub_r[k][ti - 1]] if ti > 0 else [halo_r[k]]
                        P.add("dve", (lambda k=k, t0=t0, tw=tw, ci=ci, cbuf=cbuf: lambda E: E.scalar_tensor_tensor(
                            out=cbuf[:, 0:tw], in0=ub[k][:, 1 + t0:1 + t0 + tw], scalar=cw_sb[:, ci * 3 + 1:ci * 3 + 2],
                            in1=cbuf[:, 0:tw], op0=ALU.mult, op1=ALU.add))(),
                            reads=[ub_r[k][ti], cbr, cws_r] + prev, writes=[cbr])
                        P.add("dve", (lambda k=k, t0=t0, tw=tw, ci=ci, cbuf=cbuf: lambda E: E.scalar_tensor_tensor(
                            out=cbuf[:, 0:tw], in0=ub[k][:, t0:t0 + tw], scalar=cw_sb[:, ci * 3:ci * 3 + 1],
                            in1=cbuf[:, 0:tw], op0=ALU.mult, op1=ALU.add))(),
                            reads=[ub_r[k][ti], cbr, cws_r] + prev, writes=[cbr])
                    cg, cgr = cb[0][qq % 2], cb_r[0][qq % 2]
                    cv, cvr = cb[1][qq % 2], cb_r[1][qq % 2]
                    qq += 1
                    P.add("act", (lambda cg=cg, tw=tw: lambda E: E.activation(out=cg[:, 0:tw], in_=cg[:, 0:tw], func=AF.Silu))(),
                          reads=[cgr], writes=[cgr])
                    P.add("dve", (lambda cg=cg, cv=cv, t0=t0, tw=tw, sl=sl: lambda E: E.tensor_tensor(
                        out=gT[sl][:, t0:t0 + tw], in0=cg[:, 0:tw], in1=cv[:, 0:tw], op=ALU.mult))(),
                        reads=[cgr, cvr], writes=[gT_r[sl]])
            for i in range(NT):
                for nh_ in range(2):
                    def mmd(E, i=i, nh_=nh_, gslots=gslots):
                        ins = None
                        for q, sl in enumerate(gslots):
                            ins = E.matmul(py[:], gT[sl][:, i * 128:(i + 1) * 128], wdn[sl][:, nh_ * 512:(nh_ + 1) * 512],
                                           start=(q == 0), stop=(q == len(gslots) - 1))
                        return ins
                    P.add("pe", mmd, reads=[gT_r[s_] for s_ in gslots] + [wdn_r[s_] for s_ in gslots], writes=[py_r])
                    P.add("dve", (lambda i=i, nh_=nh_: lambda E: E.tensor_tensor(
                        out=h[:, i, nh_ * 512:(nh_ + 1) * 512], in0=py[:], in1=h[:, i, nh_ * 512:(nh_ + 1) * 512],
                        op=ALU.add))(), reads=[py_r, h_res[i]], writes=[h_res[i]])
        P.barrier()
        sc.close()

    def layer1_attention():
        sc = contextlib.ExitStack()
        kT12 = sb(sc, "kT12", (128, NH, TP), BF16)
        kT12_r = [Res() for i in range(NT)]
        vb = sb(sc, "vb", (128, NT, NH, 129), BF16)
        vb_r = [Res() for i in range(NT)]
        gk = sb(sc, "gk1", (128, 4), F32)
        gk_r = Res()
        lamb = sb(sc, "lamb", (128, 4, 64), F32)
        lamt = sb(sc, "lamt", (128, 8), F32)
        lam_r = Res()
        xn_tok = sb(sc, "b_xn_tok", (128, D), BF16)
        xn_tok_r = Res()
        xnT = sb(sc, "b_xnT", (128, KC, 128), BF16)
        xnT_r = Res()
        scr = sb(sc, "b_scr", (128, D), BF16)
        scr_r = Res()
        st = sb(sc, "b_st", (128, 64), F32)
        st_r = Res()
        ks12 = sb(sc, "ks12", (128, NH, 128), BF16)
        ks12_r = Res()

        P.add("sp", lambda E: E.dma_start(out=gk[:, 0:3], in_=vecs_d[2:5, :].rearrange("v p -> p v"),
                                          allow_slow_non_contiguous=True), writes=[gk_r], dma=1)
        P.add("dve", lambda E: E.tensor_scalar(out=gk[:, 0:1], in0=gk[:, 0:1], scalar1=64.0 ** -0.5, scalar2=None,
                                               op0=ALU.mult), reads=[gk_r], writes=[gk_r])
        P.add("dve", lambda E: E.tensor_scalar(out=gk[:, 2:3], in0=gk[:, 2:3], scalar1=1.0 - lam_init, scalar2=None,
                                               op0=ALU.mult), reads=[gk_r], writes=[gk_r])
        P.add("sp", lambda E: E.dma_start(out=lamb[:].rearrange("p a d -> p (a d)"),
                                          in_=lam_d.rearrange("a d -> (a d)").partition_broadcast(128)),
              writes=[lam_r], dma=1)
        P.add("dve", lambda E: E.tensor_tensor(out=lamb[:, 0, :], in0=lamb[:, 0, :], in1=lamb[:, 1, :], op=ALU.mult),
              reads=[lam_r], writes=[lam_r])
        P.add("dve", lambda E: E.tensor_tensor(out=lamb[:, 2, :], in0=lamb[:, 2, :], in1=lamb[:, 3, :], op=ALU.mult),
              reads=[lam_r], writes=[lam_r])
        P.add("dve", lambda E: E.tensor_reduce(out=lamt[:, 0:1], in_=lamb[:, 0, :], axis=AX.X, op=ALU.add),
              reads=[lam_r], writes=[lam_r])
        P.add("dve", lambda E: E.tensor_reduce(out=lamt[:, 1:2], in_=lamb[:, 2, :], axis=AX.X, op=ALU.add),
              reads=[lam_r], writes=[lam_r])
        P.add("act", lambda E: E.activation(out=lamt[:, 2:4], in_=lamt[:, 0:2], func=AF.Exp), reads=[lam_r], writes=[lam_r])
        P.add("dve", lambda E: E.tensor_tensor(out=lamt[:, 4:5], in0=lamt[:, 3:4], in1=lamt[:, 2:3], op=ALU.subtract),
              reads=[lam_r], writes=[lam_r])
        P.add("dve", lambda E: E.tensor_scalar(out=lamt[:, 5:6], in0=lamt[:, 4:5], scalar1=-lam_init, scalar2=None,
                                               op0=ALU.add), reads=[lam_r], writes=[lam_r])
        P.add("pool", lambda E: E.memset(vb[:, :, :, 128:129], 1.0), writes=vb_r)

        kv_sc = contextlib.ExitStack()
        w_kv = sb(kv_sc, "w_kv", (128, KC, 2048), BF16)
        w_kv_r = [Res() for k in range(KC)]
        for kc in range(KC):
            P.add("pool", (lambda kc: lambda E: E.dma_start(out=w_kv[:, kc, :], in_=w_kv_d[kc * 128:(kc + 1) * 128, :]))(kc),
                  writes=[w_kv_r[kc]], dma=1)
        load_gain(2)

        def kv_tile(i):
            rms_to_xnT(i, xn_tok, xn_tok_r, lambda: xnT[:], xnT_r, scr, scr_r, st, st_r)
            for c in range(4):
                pt, pr = next_pp()

                def mm(E, c=c, pt=pt):
                    ins = None
                    for kc in range(KC):
                        ins = E.matmul(pt[:], xnT[:, kc, :], w_kv[:, kc, c * 512:(c + 1) * 512],
                                       start=(kc == 0), stop=(kc == KC - 1))
                    return ins
                P.add("pe", mm, reads=[xnT_r] + w_kv_r, writes=[pr])
                if c < 2:
                    so = 8 + c * 8
                    for hh in range(NH):
                        P.add("act", (lambda hh, pt=pt, so=so: lambda E: E.activation(
                            out=scr[:, hh * 64:(hh + 1) * 64], in_=pt[:, hh * 64:(hh + 1) * 64], func=AF.Square,
                            accum_out=st[:, so + hh:so + hh + 1]))(hh), reads=[pr], writes=[scr_r, st_r])
                    P.add("dve", (lambda so=so: lambda E: E.tensor_scalar(
                        out=st[:, 24 + so:32 + so], in0=st[:, so:so + 8], scalar1=1.0 / 64, scalar2=EPS,
                        op0=ALU.mult, op1=ALU.add))(), reads=[st_r], writes=[st_r])
                    P.add("act", (lambda so=so: lambda E: E.activation(
                        out=st[:, so:so + 8], in_=st[:, 24 + so:32 + so], func=AF.Sqrt))(), reads=[st_r], writes=[st_r])
                    P.add("dve", (lambda so=so: lambda E: E.reciprocal(
                        out=st[:, 24 + so:32 + so], in_=st[:, so:so + 8]))(), reads=[st_r], writes=[st_r])
                    for hh in range(NH):
                        P.add("dve", (lambda hh, pt=pt, so=so, c=c: lambda E: E.tensor_scalar(
                            out=ks12[:, hh, c * 64:(c + 1) * 64], in0=pt[:, hh * 64:(hh + 1) * 64],
                            scalar1=st[:, 24 + so + hh:25 + so + hh], scalar2=None, op0=ALU.mult))(hh),
                            reads=[pr, st_r], writes=[ks12_r])
                else:
                    P.add("act", (lambda pt=pt, c=c: lambda E: E.copy(
                        out=vb[:, i, (c - 2) * 4:(c - 1) * 4, 0:128], in_=pt[:].rearrange("p (g d) -> p g d", g=4)))(),
                        reads=[pr], writes=[vb_r[i]])

            def trk(E):
                ins = None
                for hh in range(NH):
                    ins = E.transpose(out=ptr[:, hh * 128:(hh + 1) * 128], in_=ks12[:, hh, :], identity=ident[:])
                return ins
            P.add("pe", trk, reads=[ks12_r, ident_r], writes=[ptr_r])
            P.add("act", (lambda i=i: lambda E: E.activation(
                out=kT12[:, :, i * 128:(i + 1) * 128], in_=ptr[:].rearrange("p (k t) -> p k t", k=NH),
                func=AF.Copy, scale=gk[:, 1:2]))(), reads=[ptr_r, gk_r], writes=[kT12_r[i]])
        for i_ in range(NT):
            kv_tile(i_)
        P.barrier()
        kv_sc.close()

        w_q = sb(sc, "w_q", (128, KC, D), BF16)
        w_q_r = Res()
        w_o = sb(sc, "w_ob", (128, KC, D), BF16)
        w_o_r = Res()
        qT12 = sb(sc, "qT12", (128, NH, 2, 128), BF16)
        qT12_r = Res()
        P.add("pool", lambda E: E.memset(qT12[:], 0.0), writes=[qT12_r])
        pT = [sb(sc, "b_pT%d" % b, (128, 512), BF16) for b in range(3)]
        pT_r = [Res() for b in range(3)]
        otok = sb(sc, "b_otok", (128, D), BF16)
        otok_r = Res()
        oT = sb(sc, "b_oT", (128, KC, 128), BF16)
        oT_r = Res()
        rz = sb(sc, "b_rz", (128, 8), F32)
        rz_r = Res()
        otmp = [sb(sc, "otmp%d" % b, (128, 128), F32) for b in range(2)]
        otmp_r = [Res() for b in range(2)]
        ost = sb(sc, "ost", (128, 32), F32)
        ost_r = Res()
        P.add("pool", lambda E: [E.dma_start(out=w_q[:, kc, :], in_=w_q_d[kc * 128:(kc + 1) * 128, :]) for kc in range(KC)],
              writes=[w_q_r], dma=1)
        P.add("pool", lambda E: [E.dma_start(out=w_o[:, kc, :], in_=w_o_b_d[kc * 128:(kc + 1) * 128, :]) for kc in range(KC)],
              writes=[w_o_r], dma=1)
        load_gain(3)

        def b_tile(i):
            jmax = min(i + 1, NT - 1)
            rms_to_xnT(i, xn_tok, xn_tok_r, lambda: xnT[:], xnT_r, scr, scr_r, st, st_r)
            for c in range(2):
                pt, pr = next_pp()

                def mm(E, c=c, pt=pt):
                    ins = None
                    for kc in range(KC):
                        ins = E.matmul(pt[:], xnT[:, kc, :], w_q[:, kc, c * 512:(c + 1) * 512],
                                       start=(kc == 0), stop=(kc == KC - 1))
                    return ins
                P.add("pe", mm, reads=[xnT_r, w_q_r], writes=[pr])
                so = 8 + c * 8
                for hh in range(NH):
                    P.add("act", (lambda hh, pt=pt, so=so: lambda E: E.activation(
                        out=scr[:, hh * 64:(hh + 1) * 64], in_=pt[:, hh * 64:(hh + 1) * 64], func=AF.Square,
                        accum_out=st[:, so + hh:so + hh + 1]))(hh), reads=[pr], writes=[scr_r, st_r])
                P.add("dve", (lambda so=so: lambda E: E.tensor_scalar(
                    out=st[:, 24 + so:32 + so], in0=st[:, so:so + 8], scalar1=1.0 / 64, scalar2=EPS,
                    op0=ALU.mult, op1=ALU.add))(), reads=[st_r], writes=[st_r])
                P.add("act", (lambda so=so: lambda E: E.activation(
                    out=st[:, so:so + 8], in_=st[:, 24 + so:32 + so], func=AF.Sqrt))(), reads=[st_r], writes=[st_r])
                P.add("dve", (lambda so=so: lambda E: E.reciprocal(
                    out=st[:, 24 + so:32 + so], in_=st[:, so:so + 8]))(), reads=[st_r], writes=[st_r])
                for hh in range(NH):
                    P.add("dve", (lambda hh, pt=pt, so=so, c=c: lambda E: E.tensor_scalar(
                        out=ks12[:, hh, c * 64:(c + 1) * 64], in0=pt[:, hh * 64:(hh + 1) * 64],
                        scalar1=st[:, 24 + so + hh:25 + so + hh], scalar2=None, op0=ALU.mult))(hh),
                        reads=[pr, st_r], writes=[ks12_r])

            def trq(E):
                ins = None
                for hh in range(NH):
                    ins = E.transpose(out=ptr[:, hh * 128:(hh + 1) * 128], in_=ks12[:, hh, :], identity=ident[:])
                return ins
            P.add("pe", trq, reads=[ks12_r, ident_r], writes=[ptr_r])
            P.add("act", lambda E: E.activation(out=qT12[0:64, :, 0, :],
                                                in_=ptr[0:64, :].rearrange("p (k t) -> p k t", k=NH),
                                                func=AF.Copy, scale=gk[0:64, 0:1]),
                  reads=[ptr_r, gk_r], writes=[qT12_r])
            P.add("act", lambda E: E.activation(out=qT12[64:128, :, 1, :],
                                                in_=ptr[64:128, :].rearrange("p (k t) -> p k t", k=NH),
                                                func=AF.Copy, scale=gk[64:128, 0:1]),
                  reads=[ptr_r, gk_r], writes=[qT12_r])
            pi = 0
            for hp in range(4):
                for j in range(jmax + 1):
                    pt, pr = next_psc()
                    off = j - i
                    near = off >= -1

                    def mm(E, pt=pt, j=j, hp=hp, off=off, near=near):
                        ins = None
                        for hh in range(2):
                            h_ = hp * 2 + hh
                            ins = E.matmul(pt[:, hh * 256:(hh + 1) * 256].rearrange("p (b t) -> p b t", b=2),
                                           kT12[:, h_, j * 128:(j + 1) * 128], qT12[:, h_, :, :],
                                           start=(hh == 0), stop=not near, skip_group_check=True)
                        if near:
                            for hh in range(2):
                                h_ = hp * 2 + hh
                                for br in range(2):
                                    o_ = (hh * 2 + br) * 128
                                    ins = E.matmul(pt[:, o_:o_ + 128], ident[:], addm[:, off + 1, h_, :],
                                                   start=False, stop=True, skip_group_check=True)
                        return ins
                    P.add("pe", mm, reads=[kT12_r[j], qT12_r, ident_r, addm_ready], writes=[pr])
                    pb, pbr = pT[pi % 3], pT_r[pi % 3]
                    pi += 1
                    P.add("act", (lambda pt=pt, pb=pb: lambda E: E.activation(out=pb[:], in_=pt[:], func=AF.Exp))(),
                          reads=[pr], writes=[pbr])

                    def pv(E, pb=pb, j=j, hp=hp):
                        ins = None
                        for q in range(4):
                            h_ = hp * 2 + q // 2
                            ins = E.matmul(po[q // 2][:, (q % 2) * 256:(q % 2) * 256 + 129],
                                           pb[:, q * 128:(q + 1) * 128], vb[:, j, h_, :],
                                           start=(j == 0 and q % 2 == 0), stop=(j == jmax), skip_group_check=True)
                        return ins
                    P.add("pe", pv, reads=[pbr, vb_r[j]], writes=po_r)
                for hh in range(2):
                    h_ = hp * 2 + hh
                    P.add("dve", (lambda hh=hh: lambda E: E.reciprocal(
                        out=rz[:, hh * 2:hh * 2 + 2], in_=po[hh][:].rearrange("p (k c) -> p k c", k=2)[:, :, 128]))(),
                        reads=[po_r[hh]], writes=[rz_r])
                    P.add("dve", (lambda hh=hh: lambda E: E.tensor_scalar(
                        out=rz[:, hh * 2 + 1:hh * 2 + 2], in0=rz[:, hh * 2 + 1:hh * 2 + 2], scalar1=lamt[:, 5:6],
                        scalar2=None, op0=ALU.mult))(), reads=[rz_r, lam_r], writes=[rz_r])
                    ot, otr = otmp[hh], otmp_r[hh]
                    P.add("dve", (lambda hh=hh, ot=ot: lambda E: E.tensor_scalar(
                        out=ot[:], in0=po[hh][:, 0:128], scalar1=rz[:, hh * 2:hh * 2 + 1], scalar2=None, op0=ALU.mult))(),
                        reads=[po_r[hh], rz_r], writes=[otr])
                    P.add("dve", (lambda hh=hh, ot=ot: lambda E: E.scalar_tensor_tensor(
                        out=ot[:], in0=po[hh][:, 256:384], scalar=rz[:, hh * 2 + 1:hh * 2 + 2], in1=ot[:],
                        op0=ALU.mult, op1=ALU.add))(), reads=[po_r[hh], rz_r, otr], writes=[otr])
                    P.add("act", (lambda h_=h_, ot=ot: lambda E: E.activation(
                        out=scr[:, 0:128], in_=ot[:], func=AF.Square, accum_out=ost[:, h_:h_ + 1]))(),
                        reads=[otr], writes=[scr_r, ost_r])
                    P.add("dve", (lambda h_=h_: lambda E: E.tensor_scalar(
                        out=ost[:, 8 + h_:9 + h_], in0=ost[:, h_:h_ + 1], scalar1=1.0 / 128, scalar2=EPS,
                        op0=ALU.mult, op1=ALU.add))(), reads=[ost_r], writes=[ost_r])
                    P.add("act", (lambda h_=h_: lambda E: E.activation(
                        out=ost[:, 16 + h_:17 + h_], in_=ost[:, 8 + h_:9 + h_], func=AF.Sqrt))(), reads=[ost_r], writes=[ost_r])
                    P.add("dve", (lambda h_=h_: lambda E: E.reciprocal(
                        out=ost[:, 24 + h_:25 + h_], in_=ost[:, 16 + h_:17 + h_]))(), reads=[ost_r], writes=[ost_r])
                    P.add("dve", (lambda h_=h_, ot=ot: lambda E: E.tensor_scalar(
                        out=otok[:, h_ * 128:(h_ + 1) * 128], in0=ot[:], scalar1=ost[:, 24 + h_:25 + h_], scalar2=None,
                        op0=ALU.mult))(), reads=[otr, ost_r], writes=[otok_r])

            def tro(E):
                ins = None
                for kc in range(KC):
                    ins = E.transpose(out=ptr[:, kc * 128:(kc + 1) * 128], in_=otok[:, kc * 128:(kc + 1) * 128],
                                      identity=ident[:])
                return ins
            P.add("pe", tro, reads=[otok_r, ident_r], writes=[ptr_r])
            P.add("act", lambda E: E.activation(out=oT[:], in_=ptr[:].rearrange("p (k t) -> p k t", k=KC),
                                                func=AF.Copy, scale=gk[:, 2:3]),
                  reads=[ptr_r, gk_r], writes=[oT_r])
            for nh_ in range(2):
                def mmo(E, nh_=nh_):
                    ins = None
                    for kc in range(KC):
                        ins = E.matmul(py[:], oT[:, kc, :], w_o[:, kc, nh_ * 512:(nh_ + 1) * 512],
                                       start=(kc == 0), stop=(kc == KC - 1))
                    return ins
                P.add("pe", mmo, reads=[oT_r, w_o_r], writes=[py_r])
                P.add("dve", (lambda nh_=nh_, i=i: lambda E: E.tensor_tensor(
                    out=h[:, i, nh_ * 512:(nh_ + 1) * 512], in0=py[:], in1=h[:, i, nh_ * 512:(nh_ + 1) * 512],
                    op=ALU.add))(), reads=[py_r, h_res[i]], writes=[h_res[i]])
        for i_ in range(dbg_tiles):
            b_tile(i_)
        P.barrier()
        sc.close()

    stage = dbg_stage if dbg_stage is not None else 99
    import os
    skip01 = os.environ.get("KSKIP01") == "1"
    if not skip01:
        layer0_attention()
    if stage >= 2 and not skip01:
        conv_ffn(0)
    if stage >= 3:
        layer1_attention()
    if stage >= 4:
        conv_ffn(1)

    out_r = Res("out")
    P.add("sp", lambda E: E.dma_start(out=out_d[0:112, :], in_=h[16:128, 0, :]), reads=[h_res[0]], writes=[out_r], dma=1)
    for i in range(1, 16):
        P.add("sp", (lambda i: lambda E: E.dma_start(out=out_d[128 * i - 16:128 * i + 112, :], in_=h[:, i, :]))(i),
              reads=[h_res[i]], writes=[Res()], dma=1)
    P.add("sp", lambda E: E.dma_start(out=out_d[2032:2048, :], in_=h[0:16, 16, :]), reads=[h_res[16]], writes=[Res()], dma=1)
    P.barrier()
    if dbg_stage is not None:
        print("n_ops", len(P.ops))
    P.emit(es)
    es.close()
    return nc


def _host_inputs(inputs):
    f = lambda a: np.ascontiguousarray(np.asarray(a, dtype=np.float32))
    idx, negvis = _static_tables()
    rel = f(inputs["rel_table"])
    table_ext = np.concatenate([rel, np.full((1, NH), NEG, np.float32)], axis=0)
    am = table_ext[idx]
    am = np.ascontiguousarray(am.transpose(1, 0, 3, 2)).reshape(128, 3 * NH * 128)
    gnorm = np.stack([f(inputs["ln_attn_g"])[0], f(inputs["ln_ffn_g"])[0], f(inputs["kv_norm_g"]),
                      f(inputs["ln_attn_g"])[1], f(inputs["ln_ffn_g"])[1]], axis=0)
    vecs = np.zeros((8, 128), np.float32)
    vecs[0] = f(inputs["qn_a"])[0]
    vecs[1] = f(inputs["kn_a"])[0]
    vecs[2] = np.concatenate([f(inputs["qn_b"])[0]] * 2)
    vecs[3] = np.concatenate([f(inputs["kn_b"])] * 2)
    vecs[4] = f(inputs["subln_b"])[0]
    lamv = np.stack([f(inputs["lam_q1"])[0], f(inputs["lam_k1"])[0], f(inputs["lam_q2"])[0], f(inputs["lam_k2"])[0]], 0)
    cw = f(inputs["conv_w"])
    convwT = np.ascontiguousarray(cw.reshape(2, 3, 2 * NFC, 128).transpose(0, 3, 2, 1)).reshape(2, 128, 2 * NFC * 3)
    cbias = f(inputs["conv_b"])
    convbT = np.ascontiguousarray(cbias.reshape(2, 2 * NFC, 128).transpose(0, 2, 1))
    shared = {
        "meta_tokens": f(inputs["meta_tokens"]),
        "gnorm": gnorm,
        "w_in_a": f(inputs["w_in_a"])[0],
        "w_o_a": f(inputs["w_o_a"])[0],
        "w_kv_b": f(inputs["w_kv_b"]),
        "w_q_b": f(inputs["w_q_b"])[0],
        "w_o_b": f(inputs["w_o_b"])[0],
        "w_up": f(inputs["w_up"]),
        "w_down": f(inputs["w_down"]),
        "conv_wT": convwT,
        "conv_bT": convbT,
        "vecs": vecs,
        "lamv": lamv,
        "addmask": am,
        "cfar": np.ascontiguousarray(rel[15:16, :]),
        "negvis": negvis,
        "ident": np.eye(128, dtype=np.float32),
    }
    return shared


def kernel(**inputs):
    shared = _host_inputs(inputs)
    x = np.asarray(inputs["x"], dtype=np.float32)
    nc = build_nc()
    in_maps = []
    for b in range(8):
        m = dict(shared)
        m["x"] = np.ascontiguousarray(x[b])
        in_maps.append(m)
    res = run_bass_kernel_spmd(nc, in_maps, core_ids=list(range(8)))
    out = np.stack([np.asarray(r["out"], dtype=np.float32) for r in res.results], axis=0)
    return out
```

```python
import math
import contextlib
import numpy as np
import concourse.bass as bass
import concourse.mybir as mybir
from concourse.bass_utils import run_bass_kernel_spmd

F32 = mybir.dt.float32
BF16 = mybir.dt.bfloat16
AF = mybir.ActivationFunctionType
ALU = mybir.AluOpType
AX = mybir.AxisListType

D = 1024
S = 2048
NMETA = 16
T = S + NMETA
NT = 17
TP = NT * 128
KC = 8
NH = 8
A_IN = 2120
DFF = 2816
NFC = DFF // 128
EPS = 1e-6
NEG = -30000.0
NEGBIG = -1.0e30
TOPK = 256


class Res:
    __slots__ = ("name", "writers", "readers", "excl")

    def __init__(self, name="", excl=False):
        self.name = name
        self.writers = {}
        self.readers = {}
        self.excl = excl


class Op:
    __slots__ = ("eng", "fn", "dma", "deps", "signal", "sem", "idx")


class Prog:
    ENGS = ("pe", "act", "dve", "pool", "sp")
    NDSEM = 8

    def __init__(self, nc):
        self.nc = nc
        self.ops = []
        self.pending = {e: [] for e in self.ENGS}
        self.last_op = {e: None for e in self.ENGS}
        self.open_dma = []
        import os
        self.cut = int(os.environ.get("KCUT", "100000000"))

    def add(self, eng, fn, reads=(), writes=(), dma=0):
        if len(self.ops) >= self.cut:
            return None
        op = Op()
        op.eng = eng
        op.fn = fn
        op.dma = dma
        op.signal = bool(dma)
        op.sem = None
        op.idx = len(self.ops)
        key = ("dma", op.idx) if dma else eng
        deps = set(self.pending[eng])
        self.pending[eng] = []
        for r in reads:
            for k, w in r.writers.items():
                if k == key and eng == "pe":
                    continue
                deps.add(w)
            if r.excl:
                for k, w in r.readers.items():
                    if k != key:
                        deps.add(w)
        for r in writes:
            for k, w in r.writers.items():
                if k == key:
                    continue
                deps.add(w)
            for k, w in r.readers.items():
                if k == key:
                    continue
                deps.add(w)
        for r in reads:
            r.readers[key] = op.idx
        for r in writes:
            r.writers = {key: op.idx}
            r.readers = {}
        op.deps = sorted(deps)
        for d in op.deps:
            self.ops[d].signal = True
        self.ops.append(op)
        if dma:
            self.open_dma.append(op.idx)
        else:
            self.last_op[eng] = op.idx
        return op

    def barrier(self):
        deps = [v for v in self.last_op.values() if v is not None] + list(self.open_dma)
        for d in deps:
            self.ops[d].signal = True
        for e in self.ENGS:
            self.pending[e] = sorted(set(self.pending[e]) | set(deps))
        self.open_dma = []

    def emit(self, es):
        nc = self.nc
        engs = {"pe": nc.tensor, "act": nc.scalar, "dve": nc.vector, "pool": nc.gpsimd, "sp": nc.sync}
        esem = {e: es.enter_context(nc.semaphore("s_" + e)) for e in self.ENGS}
        ecnt = {e: 0 for e in self.ENGS}
        dsem = {e: [es.enter_context(nc.semaphore("d_%s%d" % (e, i))) for i in range(self.NDSEM)]
                for e in ("sp", "pool", "act")}
        dval = {e: [0] * self.NDSEM for e in dsem}
        dcnt = {e: 0 for e in dsem}
        seen = {e: {} for e in self.ENGS}
        semid = {}

        def wait(e, sem, val):
            k = id(sem)
            semid[k] = sem
            if seen[e].get(k, 0) < val:
                engs[e].wait_ge(sem, val)
                seen[e][k] = val

        for op in self.ops:
            e = op.eng
            E = engs[e]
            for d in op.deps:
                sem, val = self.ops[d].sem
                wait(e, sem, val)
            if op.dma:
                n = dcnt[e]
                dcnt[e] += 1
                slot = n % self.NDSEM
                sem = dsem[e][slot]
                wait(e, sem, dval[e][slot])
                inss = op.fn(E)
                if not isinstance(inss, (list, tuple)):
                    inss = [inss]
                for ins in inss:
                    ins.then_inc(sem, 16)
                dval[e][slot] += 16 * len(inss)
                op.sem = (sem, dval[e][slot])
            else:
                ins = op.fn(E)
                if op.signal:
                    ecnt[e] += 1
                    ins.then_inc(esem[e], 1)
                    op.sem = (esem[e], ecnt[e])
        for d in self.pending["sp"]:
            sem, val = self.ops[d].sem
            wait("sp", sem, val)


def _rel_bucket_np(rel):
    nb = 16
    max_exact = 8
    n = np.abs(rel)
    nf = np.maximum(n, 1).astype(np.float32)
    large = max_exact + (np.log(nf / np.float32(max_exact)) / np.float32(math.log(128 / max_exact))
                         * np.float32(nb - max_exact)).astype(np.int32)
    large = np.minimum(large, nb - 1)
    return np.where(rel > 0, nb, 0) + np.where(n < max_exact, n, large)


def _static_tables():
    s_l = np.arange(128)[:, None]
    t_l = np.arange(128)[None, :]
    lim = np.where(t_l < 16, 16, np.where(t_l < 80, 80, 128))
    idx = np.zeros((3, 128, 128), np.int64)
    for o, off in enumerate((-1, 0, 1)):
        rel = 128 * off + s_l - t_l
        b = _rel_bucket_np(rel)
        if off == -1:
            vis = np.ones((128, 128), bool)
        elif off == 0:
            vis = s_l < lim
        else:
            vis = (t_l >= 80) & (s_l < 16)
        idx[o] = np.where(vis, b, 32)
    tt = np.arange(128)[:, None]
    ss = np.arange(256)[None, :]
    limt = np.where(tt < 16, 16, np.where(tt < 80, 80, 144))
    negvis = np.where(ss < limt, 0.0, NEGBIG).astype(np.float32)
    return idx, negvis


def build_nc(dbg_stage=None, dbg_tiles=NT):
    nc = bass.Bass("TRN2", target_bir_lowering=False)

    def din(name, shape):
        return nc.dram_tensor(name, list(shape), F32, kind="ExternalInput").ap()

    x_d = din("x", (S, D))
    meta_d = din("meta_tokens", (NMETA, D))
    gnorm_d = din("gnorm", (5, D))
    w_in_d = din("w_in_a", (D, A_IN))
    w_o_a_d = din("w_o_a", (D, D))
    w_kv_d = din("w_kv_b", (D, 2048))
    w_q_d = din("w_q_b", (D, D))
    w_o_b_d = din("w_o_b", (D, D))
    w_up_d = din("w_up", (2, D, 2 * DFF))
    w_down_d = din("w_down", (2, DFF, D))
    convw_d = din("conv_wT", (2, 128, 2 * NFC * 3))
    convb_d = din("conv_bT", (2, 128, 2 * NFC))
    vecs_d = din("vecs", (8, 128))
    lam_d = din("lamv", (4, 64))
    addm_d = din("addmask", (128, 3 * NH * 128))
    cfar_d = din("cfar", (1, NH))
    negvis_d = din("negvis", (128, 256))
    ident_d = din("ident", (128, 128))
    out_d = nc.dram_tensor("out", [S, D], F32, kind="ExternalOutput").ap()

    es = contextlib.ExitStack()
    P = Prog(nc)
    lam_init = 0.8 - 0.6 * math.exp(-0.3 * 1)

    def sb(scope, name, shape, dt):
        return scope.enter_context(nc.sbuf_tensor("sb_" + name, list(shape), dt))

    h = sb(es, "h", (128, NT, D), F32)
    h_res = [Res("h%d" % i) for i in range(NT)]
    ident = sb(es, "ident", (128, 128), BF16)
    ident_r = Res("ident")
    gb = sb(es, "gb", (128, D), F32)
    gb_r = Res("gb")
    vecs = sb(es, "vecs", (128, 8), F32)
    vecs_r = Res("vecs")
    addm = sb(es, "addm", (128, 3, NH, 128), BF16)
    addm_r = Res("addm")
    cfar = sb(es, "cfar", (128, NH), F32)
    cfar_r = Res("cfar")
    negvis = sb(es, "negvis", (128, 256), F32)
    negvis_r = Res("negvis")
    small = sb(es, "small", (128, 64), F32)
    init_sc = contextlib.ExitStack()
    addm_f = sb(init_sc, "addm_f", (128, 3, NH, 128), F32)
    pp = [es.enter_context(nc.psum_tensor("pp%d" % i, [128, 512], F32)) for i in range(2)]
    pp_r = [Res("pp%d" % i, True) for i in range(2)]
    psc = [es.enter_context(nc.psum_tensor("psc%d" % i, [128, 512], F32)) for i in range(2)]
    psc_r = [Res("psc%d" % i, True) for i in range(2)]
    po = [es.enter_context(nc.psum_tensor("po%d" % i, [128, 512], F32)) for i in range(2)]
    po_r = [Res("po%d" % i, True) for i in range(2)]
    ptr = es.enter_context(nc.psum_tensor("ptr", [128, 1024], BF16))
    ptr_r = Res("ptr", True)
    py = es.enter_context(nc.psum_tensor("py", [128, 512], F32))
    py_r = Res("py", True)

    cnt = {"pp": 0, "psc": 0}

    def next_pp():
        i = cnt["pp"] % 2
        cnt["pp"] += 1
        return pp[i], pp_r[i]

    def next_psc():
        i = cnt["psc"] % 2
        cnt["psc"] += 1
        return psc[i], psc_r[i]

    P.add("sp", lambda E: [
        E.dma_start(out=h[0:16, 0, :], in_=meta_d),
        E.dma_start(out=h[16:128, 0, :], in_=x_d[0:112, :]),
    ], writes=[h_res[0]], dma=1)
    for i in range(1, 16):
        P.add("sp", (lambda i: lambda E: E.dma_start(out=h[:, i, :], in_=x_d[128 * i - 16:128 * i + 112, :]))(i),
              writes=[h_res[i]], dma=1)
    P.add("dve", lambda E: E.memset(h[:, 16, :], 0.0), writes=[h_res[16]])
    P.add("sp", lambda E: E.dma_start(out=h[0:16, 16, :], in_=x_d[2032:2048, :]), writes=[h_res[16]], dma=1)
    P.add("pool", lambda E: E.dma_start(out=ident[:], in_=ident_d), writes=[ident_r], dma=1)
    P.add("sp", lambda E: E.dma_start(out=negvis[:], in_=negvis_d), writes=[negvis_r], dma=1)
    P.add("sp", lambda E: E.dma_start(out=addm_f[:].rearrange("p a h t -> p (a h t)"), in_=addm_d),
          writes=[addm_r], dma=1)
    P.add("sp", lambda E: E.dma_start(out=cfar[:], in_=cfar_d.partition_broadcast(128)), writes=[cfar_r], dma=1)
    for hh in range(NH):
        P.add("dve", (lambda hh: lambda E: E.tensor_scalar(
            out=addm[:, :, hh, :], in0=addm_f[:, :, hh, :], scalar1=cfar[:, hh:hh + 1], scalar2=None,
            op0=ALU.subtract))(hh), reads=[addm_r, cfar_r], writes=[Res()])
    addm_ready = Res("addm_ready")
    P.add("dve", lambda E: E.memset(small[:, 0:1], 0.0), reads=[], writes=[addm_ready])
    P.barrier()
    init_sc.close()

    def load_gain(idx):
        P.add("sp", lambda E: E.dma_start(out=gb[:], in_=gnorm_d[idx:idx + 1, :].partition_broadcast(128)),
              writes=[gb_r], dma=1)

    def rms_to_xnT(i, xn_tok, xn_tok_r, dst_fn, dst_res, scr, scr_r, st, st_r):
        P.add("act", lambda E: E.activation(out=scr[:], in_=h[:, i, :], func=AF.Square, accum_out=st[:, 0:1]),
              reads=[h_res[i]], writes=[scr_r, st_r])
        P.add("dve", lambda E: E.tensor_scalar(out=st[:, 1:2], in0=st[:, 0:1], scalar1=1.0 / D, scalar2=EPS,
                                               op0=ALU.mult, op1=ALU.add), reads=[st_r], writes=[st_r])
        P.add("act", lambda E: E.activation(out=st[:, 2:3], in_=st[:, 1:2], func=AF.Sqrt), reads=[st_r], writes=[st_r])
        P.add("dve", lambda E: E.reciprocal(out=st[:, 3:4], in_=st[:, 2:3]), reads=[st_r], writes=[st_r])
        P.add("dve", lambda E: E.scalar_tensor_tensor(out=xn_tok[:], in0=h[:, i, :], scalar=st[:, 3:4], in1=gb[:],
                                                      op0=ALU.mult, op1=ALU.mult),
              reads=[h_res[i], st_r, gb_r], writes=[xn_tok_r])

        def tr(E):
            ins = None
            for kc in range(KC):
                ins = E.transpose(out=ptr[:, kc * 128:(kc + 1) * 128], in_=xn_tok[:, kc * 128:(kc + 1) * 128],
                                  identity=ident[:])
            return ins
        P.add("pe", tr, reads=[xn_tok_r, ident_r], writes=[ptr_r])
        P.add("act", lambda E: E.copy(out=dst_fn(), in_=ptr[:].rearrange("p (k t) -> p k t", k=KC)),
              reads=[ptr_r], writes=[dst_res])

    def layer0_attention():
        sc = contextlib.ExitStack()
        w_in = sb(sc, "w_in", (128, KC, A_IN), BF16)
        w_in_r = [Res("w_in%d" % k) for k in range(KC)]
        w_o = sb(sc, "w_o", (128, KC, D), BF16)
        w_o_r = Res("w_o")
        kT = sb(sc, "kT", (128, 2, TP), BF16)
        kT_r = [Res("kT%d" % i) for i in range(NT)]
        vaug = sb(sc, "vaug", (128, NT, 2, 129), BF16)
        v_r = [Res("v%d" % i) for i in range(NT)]
        kiT = sb(sc, "kiT", (128, TP), BF16)
        kiT_r = [Res("kiT%d" % i) for i in range(NT)]
        NB = 2
        xn_tok = [sb(sc, "xn_tok%d" % b, (128, D), BF16) for b in range(1)] * NB
        xn_tok_r = [Res() for b in range(1)] * NB
        xnT = [sb(sc, "xnT%d" % b, (128, KC, 128), BF16) for b in range(1)] * NB
        xnT_r = [Res() for b in range(1)] * NB
        scr = sb(sc, "scr", (128, D), BF16)
        scr_r = Res("scr")
        st = [sb(sc, "st%d" % b, (128, 64), F32) for b in range(NB)]
        st_r = [Res() for b in range(NB)]
        qs = [sb(sc, "qs%d" % b, (128, NH, 128), BF16) for b in range(1)] * NB
        qs_r = [Res() for b in range(1)] * NB
        ks = [sb(sc, "ks%d" % b, (128, 2, 128), BF16) for b in range(1)] * NB
        ks_r = [Res() for b in range(1)] * NB
        qT = [sb(sc, "qT%d" % b, (128, NH, 128), BF16) for b in range(NB)]
        qT_r = [Res() for b in range(NB)]
        qis = [sb(sc, "qis%d" % b, (128, NH * 64), BF16) for b in range(1)] * NB
        qis_r = [Res() for b in range(1)] * NB
        qiT = [sb(sc, "qiT%d" % b, (128, 4, 128), BF16) for b in range(NB)]
        qiT_r = [Res() for b in range(NB)]
        kis = [sb(sc, "kis%d" % b, (128, 128), BF16) for b in range(1)] * NB
        kis_r = [Res() for b in range(1)] * NB
        wst = [sb(sc, "wst%d" % b, (128, 32), F32) for b in range(NB)]
        wst_r = [Res() for b in range(NB)]
        acc = sb(sc, "acc", (128, TP), F32)
        acc_r = Res("acc")
        work = sb(sc, "work", (128, TP), F32)
        work_r = Res("work")
        rbuf = [sb(sc, "rbuf%d" % b, (128, 512), F32) for b in range(2)]
        rbuf_r = [Res() for b in range(2)]
        m8 = sb(sc, "m8", (128, 8), F32)
        m8_r = Res("m8")
        tb = sb(sc, "tb", (128, 32), F32)
        tb_r = Res("tb")
        iota8 = sb(sc, "iota8", (128, 8), F32)
        iota_r = Res("iota8")
        for q8 in range(8):
            P.add("pool", (lambda q8=q8: lambda E: E.memset(iota8[:, q8:q8 + 1], float(q8)))(), writes=[iota_r])
        sel = work[:].bitcast(BF16)
        sel_r = work_r
        selT = sb(sc, "selT", (128, NT, 128), BF16)
        selT_r = Res("selT")
        pT = [sb(sc, "pT%d" % b, (128, 512), BF16) for b in range(3)]
        pT_r = [Res() for b in range(3)]
        otok = sb(sc, "otok", (128, D), BF16)
        otok_r = Res("otok")
        oT = sb(sc, "oT", (128, KC, 128), BF16)
        oT_r = Res("oT")
        rz = sb(sc, "rz", (128, 8), F32)
        rz_r = Res("rz")
        gq = sb(sc, "gq", (128, 2), F32)
        gq_r = Res("gq")

        for kc in range(KC):
            P.add("pool", (lambda kc: lambda E: E.dma_start(out=w_in[:, kc, :], in_=w_in_d[kc * 128:(kc + 1) * 128, :]))(kc),
                  writes=[w_in_r[kc]], dma=1)
        P.add("pool", lambda E: [E.dma_start(out=w_o[:, kc, :], in_=w_o_a_d[kc * 128:(kc + 1) * 128, :]) for kc in range(KC)],
              writes=[w_o_r], dma=1)
        load_gain(0)
        P.add("sp", lambda E: E.dma_start(out=gq[:], in_=vecs_d[0:2, :].rearrange("v p -> p v"),
                                          allow_slow_non_contiguous=True), writes=[gq_r], dma=1)
        P.add("dve", lambda E: E.tensor_scalar(out=gq[:, 0:1], in0=gq[:, 0:1], scalar1=128.0 ** -0.5, scalar2=None,
                                               op0=ALU.mult), reads=[gq_r], writes=[gq_r])
        P.add("pool", lambda E: E.memset(vaug[:, :, :, 128:129], 1.0), writes=v_r)

        def proj_tile(i):
            b = i % NB
            rms_to_xnT(i, xn_tok[b], xn_tok_r[b], lambda: xnT[b][:], xnT_r[b], scr, scr_r, st[b], st_r[b])
            for c, (c0, cw) in enumerate(((0, 512), (512, 512), (1024, 512), (1536, 512), (2048, 72))):
                pt, pr = next_pp()

                def mm(E, c0=c0, cw=cw, pt=pt):
                    ins = None
                    for kc in range(KC):
                        ins = E.matmul(pt[:, 0:cw], xnT[b][:, kc, :], w_in[:, kc, c0:c0 + cw],
                                       start=(kc == 0), stop=(kc == KC - 1))
                    return ins
                P.add("pe", mm, reads=[xnT_r[b]] + w_in_r, writes=[pr])
                if c in (0, 1, 2):
                    nh = 4 if c < 2 else 2
                    so = 8 + c * 4
                    for hh in range(nh):
                        P.add("act", (lambda hh, pt=pt, so=so: lambda E: E.activation(
                            out=scr[:, hh * 128:(hh + 1) * 128], in_=pt[:, hh * 128:(hh + 1) * 128], func=AF.Square,
                            accum_out=st[b][:, so + hh:so + hh + 1]))(hh),
                            reads=[pr], writes=[scr_r, st_r[b]])
                    P.add("dve", (lambda so=so, nh=nh: lambda E: E.tensor_scalar(
                        out=st[b][:, 24 + so:24 + so + nh], in0=st[b][:, so:so + nh], scalar1=1.0 / 128, scalar2=EPS,
                        op0=ALU.mult, op1=ALU.add))(), reads=[st_r[b]], writes=[st_r[b]])
                    P.add("act", (lambda so=so, nh=nh: lambda E: E.activation(
                        out=st[b][:, so:so + nh], in_=st[b][:, 24 + so:24 + so + nh], func=AF.Sqrt))(),
                        reads=[st_r[b]], writes=[st_r[b]])
                    P.add("dve", (lambda so=so, nh=nh: lambda E: E.reciprocal(
                        out=st[b][:, 24 + so:24 + so + nh], in_=st[b][:, so:so + nh]))(),
                        reads=[st_r[b]], writes=[st_r[b]])
                    for hh in range(nh):
                        if c < 2:
                            dst = qs[b][:, c * 4 + hh, :]
                            dr = qs_r[b]
                        else:
                            dst = ks[b][:, hh, :]
                            dr = ks_r[b]
                        P.add("dve", (lambda hh, dst=dst, pt=pt, so=so: lambda E: E.tensor_scalar(
                            out=dst, in0=pt[:, hh * 128:(hh + 1) * 128], scalar1=st[b][:, 24 + so + hh:24 + so + hh + 1],
                            scalar2=None, op0=ALU.mult))(hh), reads=[pr, st_r[b]], writes=[dr])
                    if c == 2:
                        P.add("act", (lambda pt=pt: lambda E: E.copy(
                            out=vaug[:, i, :, 0:128], in_=pt[:, 256:512].rearrange("p (g d) -> p g d", g=2)))(),
                            reads=[pr], writes=[v_r[i]])
                elif c == 3:
                    pass
                    qi_pt, qi_pr = pt, pr
                else:
                    P.add("dve", (lambda pt=pt: lambda E: E.tensor_scalar(
                        out=wst[b][:, 0:8], in0=pt[:, 64:72], scalar1=0.0, scalar2=2.0, op0=ALU.is_gt, op1=ALU.mult))(),
                        reads=[pr], writes=[wst_r[b]])
                    P.add("dve", lambda E: E.tensor_scalar(
                        out=wst[b][:, 0:8], in0=wst[b][:, 0:8], scalar1=-1.0, scalar2=None, op0=ALU.add),
                        reads=[wst_r[b]], writes=[wst_r[b]])
                    P.add("dve", (lambda pt=pt: lambda E: E.scalar_tensor_tensor(
                        out=wst[b][:, 8:16], in0=pt[:, 64:72], scalar=(8.0 ** -0.5) * (64.0 ** -0.5), in1=wst[b][:, 0:8],
                        op0=ALU.mult, op1=ALU.mult))(), reads=[pr, wst_r[b]], writes=[wst_r[b]])
                    P.add("act", (lambda pt=pt: lambda E: E.copy(out=kis[b][:, 0:64], in_=pt[:, 0:64]))(),
                          reads=[pr], writes=[kis_r[b]])
                    P.add("act", (lambda pt=pt: lambda E: E.copy(out=kis[b][:, 64:128], in_=pt[:, 0:64]))(),
                          reads=[pr], writes=[kis_r[b]])
                    for hh in range(NH):
                        P.add("dve", (lambda hh, qi_pt=qi_pt: lambda E: E.tensor_scalar(
                            out=qis[b][:, hh * 64:(hh + 1) * 64], in0=qi_pt[:, hh * 64:(hh + 1) * 64],
                            scalar1=wst[b][:, 8 + hh:9 + hh], scalar2=None, op0=ALU.mult))(hh),
                            reads=[qi_pr, wst_r[b]], writes=[qis_r[b]])
            def trq(E):
                ins = None
                for hh in range(NH):
                    ins = E.transpose(out=ptr[:, hh * 128:(hh + 1) * 128], in_=qs[b][:, hh, :], identity=ident[:])
                return ins
            P.add("pe", trq, reads=[qs_r[b], ident_r], writes=[ptr_r])
            P.add("act", lambda E: E.activation(out=qT[b][:], in_=ptr[:].rearrange("p (k t) -> p k t", k=NH),
                                                func=AF.Copy, scale=gq[:, 0:1]),
                  reads=[ptr_r, gq_r], writes=[qT_r[b]])

            def trk(E):
                ins = None
                for g in range(2):
                    ins = E.transpose(out=ptr[:, g * 128:(g + 1) * 128], in_=ks[b][:, g, :], identity=ident[:])
                for hp in range(4):
                    ins = E.transpose(out=ptr[:, 256 + hp * 128:256 + (hp + 1) * 128],
                                      in_=qis[b][:, hp * 128:(hp + 1) * 128], identity=ident[:])
                ins = E.transpose(out=ptr[:, 768:896], in_=kis[b][:], identity=ident[:])
                return ins
            P.add("pe", trk, reads=[ks_r[b], qis_r[b], kis_r[b], ident_r], writes=[ptr_r])
            P.add("act", lambda E: E.activation(out=kT[:, :, i * 128:(i + 1) * 128],
                                                in_=ptr[:, 0:256].rearrange("p (g t) -> p g t", g=2),
                                                func=AF.Copy, scale=gq[:, 1:2]),
                  reads=[ptr_r, gq_r], writes=[kT_r[i]])
            P.add("dve", lambda E: E.tensor_copy(out=qiT[b][:], in_=ptr[:, 256:768].rearrange("p (k t) -> p k t", k=4)),
                  reads=[ptr_r], writes=[qiT_r[b]])
            P.add("dve", lambda E: E.tensor_copy(out=kiT[:, i * 128:(i + 1) * 128], in_=ptr[:, 768:896]),
                  reads=[ptr_r], writes=[kiT_r[i]])

        def attn_tile(i):
            b = i % NB
            jmax = min(i + 1, NT - 1)
            nk = 128 * (jmax + 1)
            if i > 0:
                P.add("pool", lambda E: E.memset(acc[:, 0:128 * i], 0.0), writes=[acc_r])
            wv = nk - 128 * i
            P.add("pool", lambda E: E.tensor_copy(out=acc[:, 128 * i:nk], in_=negvis[:, 0:wv]),
                  reads=[negvis_r], writes=[acc_r])
            nchunk = (nk + 511) // 512
            ri = 0
            for hh in range(NH):
                for cch in range(nchunk):
                    c0 = cch * 512
                    cw = min(512, nk - c0)
                    pt, pr = next_psc()
                    po_ = (hh % 2) * 64
                    P.add("pe", (lambda pt=pt, c0=c0, cw=cw, hh=hh, po_=po_: lambda E: E.matmul(
                        pt[:, 0:cw], qiT[b][po_:po_ + 64, hh // 2, :], kiT[po_:po_ + 64, c0:c0 + cw],
                        start=True, stop=True))(),
                        reads=[qiT_r[b]] + kiT_r[0:jmax + 1], writes=[pr])
                    rb, rr = rbuf[ri % 2], rbuf_r[ri % 2]
                    ri += 1
                    P.add("act", (lambda pt=pt, cw=cw, rb=rb: lambda E: E.activation(
                        out=rb[:, 0:cw], in_=pt[:, 0:cw], func=AF.Relu))(), reads=[pr], writes=[rr])
                    P.add("dve", (lambda c0=c0, cw=cw, rb=rb, hh=hh: lambda E: E.scalar_tensor_tensor(
                        out=acc[:, c0:c0 + cw], in0=rb[:, 0:cw], scalar=wst[b][:, hh:hh + 1], in1=acc[:, c0:c0 + cw],
                        op0=ALU.mult, op1=ALU.add))(), reads=[rr, wst_r[b], acc_r], writes=[acc_r])
            if i < 2:
                src = acc
                src_r = acc_r
                for it in range(TOPK // 8):
                    P.add("dve", (lambda src=src: lambda E: E.max(out=m8[:], in_=src[:, 0:nk]))(),
                          reads=[src_r], writes=[m8_r])
                    if it < TOPK // 8 - 1:
                        P.add("dve", (lambda src=src: lambda E: E.match_replace(
                            out=work[:, 0:nk], in_to_replace=m8[:], in_values=src[:, 0:nk], imm_value=-3.0e38))(),
                            reads=[src_r, m8_r], writes=[work_r])
                        src = work
                        src_r = work_r
                thr_ap = m8[:, 7:8]
                thr_res = m8_r
            else:
                NB_IT = 12
                P.add("dve", lambda E: E.tensor_reduce(out=tb[:, 0:1], in_=acc[:, 0:nk], axis=AX.X, op=ALU.max),
                      reads=[acc_r], writes=[tb_r])
                P.add("dve", lambda E: E.tensor_reduce(out=tb[:, 1:2], in_=acc[:, 0:128 * i], axis=AX.X, op=ALU.min),
                      reads=[acc_r], writes=[tb_r])
                P.add("dve", lambda E: E.tensor_tensor(out=tb[:, 2:3], in0=tb[:, 0:1], in1=tb[:, 1:2], op=ALU.subtract),
                      reads=[tb_r], writes=[tb_r])
                for it in range(NB_IT):
                    cc = 0.5 ** (it + 1)
                    P.add("dve", (lambda cc=cc: lambda E: E.scalar_tensor_tensor(
                        out=tb[:, 3:4], in0=tb[:, 2:3], scalar=cc, in1=tb[:, 1:2], op0=ALU.mult, op1=ALU.add))(),
                        reads=[tb_r], writes=[tb_r])
                    P.add("dve", lambda E: E.tensor_scalar(
                        out=sel[:, 0:nk], in0=acc[:, 0:nk], scalar1=tb[:, 3:4], scalar2=None, op0=ALU.is_ge,
                        op1=ALU.add, accum_out=tb[:, 4:5]), reads=[acc_r, tb_r], writes=[work_r, tb_r])
                    P.add("dve", (lambda cc=cc: lambda E: E.tensor_scalar(
                        out=tb[:, 5:6], in0=tb[:, 4:5], scalar1=TOPK - 0.5, scalar2=cc, op0=ALU.is_ge, op1=ALU.mult))(),
                        reads=[tb_r], writes=[tb_r])
                    P.add("dve", lambda E: E.scalar_tensor_tensor(
                        out=tb[:, 1:2], in0=tb[:, 2:3], scalar=tb[:, 5:6], in1=tb[:, 1:2], op0=ALU.mult, op1=ALU.add),
                        reads=[tb_r], writes=[tb_r])
                P.add("dve", lambda E: E.scalar_tensor_tensor(
                    out=tb[:, 6:7], in0=tb[:, 2:3], scalar=0.5 ** NB_IT, in1=tb[:, 1:2], op0=ALU.mult, op1=ALU.add),
                    reads=[tb_r], writes=[tb_r])
                P.add("dve", lambda E: E.tensor_scalar(
                    out=sel[:, 0:nk], in0=acc[:, 0:nk], scalar1=tb[:, 6:7], scalar2=None, op0=ALU.is_ge,
                    op1=ALU.add, accum_out=tb[:, 7:8]), reads=[acc_r, tb_r], writes=[work_r, tb_r])
                P.add("dve", lambda E: E.tensor_scalar(
                    out=work[:, 0:nk], in0=acc[:, 0:nk], scalar1=tb[:, 6:7], scalar2=-1.0, op0=ALU.is_lt, op1=ALU.add),
                    reads=[acc_r, tb_r], writes=[work_r])
                P.add("dve", lambda E: E.scalar_tensor_tensor(
                    out=work[:, 0:nk], in0=work[:, 0:nk], scalar=3.0e38, in1=acc[:, 0:nk], op0=ALU.mult, op1=ALU.add),
                    reads=[acc_r, work_r], writes=[work_r])
                P.add("dve", lambda E: E.max(out=m8[:], in_=work[:, 0:nk]), reads=[work_r], writes=[m8_r])
                P.add("dve", lambda E: E.tensor_scalar(
                    out=tb[:, 8:9], in0=tb[:, 7:8], scalar1=-1.0, scalar2=float(TOPK - 1), op0=ALU.mult, op1=ALU.add),
                    reads=[tb_r], writes=[tb_r])
                P.add("dve", lambda E: E.tensor_scalar(
                    out=tb[:, 8:9], in0=tb[:, 8:9], scalar1=0.0, scalar2=7.0, op0=ALU.max, op1=ALU.min),
                    reads=[tb_r], writes=[tb_r])
                P.add("dve", lambda E: E.tensor_scalar(
                    out=tb[:, 16:24], in0=iota8[:], scalar1=tb[:, 8:9], scalar2=None, op0=ALU.is_equal),
                    reads=[tb_r, iota_r], writes=[tb_r])
                P.add("dve", lambda E: E.tensor_tensor(out=tb[:, 16:24], in0=tb[:, 16:24], in1=m8[:], op=ALU.mult),
                      reads=[tb_r, m8_r], writes=[tb_r])
                P.add("dve", lambda E: E.tensor_reduce(out=tb[:, 9:10], in_=tb[:, 16:24], axis=AX.X, op=ALU.add),
                      reads=[tb_r], writes=[tb_r])
                thr_ap = tb[:, 9:10]
                thr_res = tb_r
            P.add("dve", (lambda thr_ap=thr_ap: lambda E: E.tensor_scalar(
                out=sel[:, 0:nk], in0=acc[:, 0:nk], scalar1=thr_ap, scalar2=None, op0=ALU.is_ge))(),
                reads=[acc_r, thr_res], writes=[sel_r])
            for j0 in range(0, jmax + 1, 8):
                j1 = min(jmax + 1, j0 + 8)

                def trs(E, j0=j0, j1=j1):
                    ins = None
                    for j in range(j0, j1):
                        ins = E.transpose(out=ptr[:, (j - j0) * 128:(j - j0 + 1) * 128], in_=sel[:, j * 128:(j + 1) * 128],
                                          identity=ident[:])
                    return ins
                P.add("pe", trs, reads=[sel_r, ident_r], writes=[ptr_r])
                P.add("act", (lambda j0=j0, j1=j1: lambda E: E.copy(
                    out=selT[:, j0:j1, :], in_=ptr[:, 0:(j1 - j0) * 128].rearrange("p (k t) -> p k t", k=j1 - j0)))(),
                    reads=[ptr_r], writes=[selT_r])
            steps = [(g, j) for g in range(2) for j in range(jmax + 1)]
            pstate = {"pi": 0}

            def emit_qk(k):
                g, j = steps[k]
                pt, pr = next_psc()
                off = j - i
                near = off >= -1

                def mm(E, pt=pt, j=j, g=g, off=off, near=near):
                    ins = E.matmul(pt[:].rearrange("p (k t) -> p k t", k=4), kT[:, g, j * 128:(j + 1) * 128],
                                   qT[b][:, g * 4:(g + 1) * 4, :], start=True, stop=not near)
                    if near:
                        ins = E.matmul(pt[:].rearrange("p (k t) -> p k t", k=4), ident[:],
                                       addm[:, off + 1, g * 4:(g + 1) * 4, :], start=False, stop=True)
                    return ins
                P.add("pe", mm, reads=[kT_r[j], qT_r[b], ident_r, addm_ready], writes=[pr])
                return pt, pr

            def emit_rest(k, pt, pr):
                g, j = steps[k]
                pb, pbr = pT[pstate["pi"] % 3], pT_r[pstate["pi"] % 3]
                pstate["pi"] += 1
                P.add("act", (lambda pt=pt, pb=pb: lambda E: E.activation(out=pb[:], in_=pt[:], func=AF.Exp))(),
                      reads=[pr], writes=[pbr])
                P.add("dve", (lambda pb=pb, j=j: lambda E: E.tensor_tensor(
                    out=pb[:].rearrange("p (k t) -> p k t", k=4), in0=pb[:].rearrange("p (k t) -> p k t", k=4),
                    in1=selT[:, j:j + 1, :].to_broadcast([128, 4, 128]), op=ALU.mult))(),
                    reads=[pbr, selT_r], writes=[pbr])

                def pv(E, pb=pb, j=j, g=g):
                    ins = None
                    for hh in range(4):
                        ins = E.matmul(po[hh // 2][:, (hh % 2) * 256:(hh % 2) * 256 + 129],
                                       pb[:, hh * 128:(hh + 1) * 128], vaug[:, j, g, :],
                                       start=(j == 0 and hh % 2 == 0), stop=(j == jmax),
                                       skip_group_check=True)
                    return ins
                P.add("pe", pv, reads=[pbr, v_r[j]], writes=po_r)
                if j == jmax:
                    for hb in range(2):
                        P.add("dve", (lambda hb=hb, g=g: lambda E: E.reciprocal(
                            out=rz[:, g * 4 + hb * 2:g * 4 + hb * 2 + 2],
                            in_=po[hb][:].rearrange("p (k c) -> p k c", k=2)[:, :, 128]))(),
                            reads=[po_r[hb]], writes=[rz_r])
                    for hh in range(4):
                        P.add("dve", (lambda hh=hh, g=g: lambda E: E.tensor_scalar(
                            out=otok[:, (g * 4 + hh) * 128:(g * 4 + hh + 1) * 128],
                            in0=po[hh // 2][:, (hh % 2) * 256:(hh % 2) * 256 + 128],
                            scalar1=rz[:, g * 4 + hh:g * 4 + hh + 1], scalar2=None, op0=ALU.mult))(),
                            reads=[po_r[hh // 2], rz_r], writes=[otok_r])

            cur = emit_qk(0)
            for k in range(len(steps)):
                nxt = emit_qk(k + 1) if k + 1 < len(steps) else None
                emit_rest(k, *cur)
                cur = nxt
            def tro(E):
                ins = None
                for kc in range(KC):
                    ins = E.transpose(out=ptr[:, kc * 128:(kc + 1) * 128], in_=otok[:, kc * 128:(kc + 1) * 128],
                                      identity=ident[:])
                return ins
            P.add("pe", tro, reads=[otok_r, ident_r], writes=[ptr_r])
            P.add("act", lambda E: E.copy(out=oT[:], in_=ptr[:].rearrange("p (k t) -> p k t", k=KC)),
                  reads=[ptr_r], writes=[oT_r])
            for nh_ in range(2):
                def mmo(E, nh_=nh_):
                    ins = None
                    for kc in range(KC):
                        ins = E.matmul(py[:], oT[:, kc, :], w_o[:, kc, nh_ * 512:(nh_ + 1) * 512],
                                       start=(kc == 0), stop=(kc == KC - 1))
                    return ins
                P.add("pe", mmo, reads=[oT_r, w_o_r], writes=[py_r])
                P.add("dve", (lambda nh_=nh_: lambda E: E.tensor_tensor(
                    out=h[:, i, nh_ * 512:(nh_ + 1) * 512], in0=py[:], in1=h[:, i, nh_ * 512:(nh_ + 1) * 512],
                    op=ALU.add))(), reads=[py_r, h_res[i]], writes=[h_res[i]])

        proj_tile(0)
        for i in range(dbg_tiles):
            if i + 1 < NT:
                proj_tile(i + 1)
            attn_tile(i)
        P.barrier()
        sc.close()

    def conv_ffn(l):
        sc = contextlib.ExitStack()
        sbl = lambda scope, name, shape, dt: sb(scope, "L%d_%s" % (l, name), shape, dt)
        G = 6
        groups = []
        c = 0
        while c < NFC:
            groups.append(list(range(c, min(NFC, c + G))))
            c += G
        xnT = sbl(sc, "f_xnT", (128, KC, TP), BF16)
        xnT_r = [Res() for i in range(NT)]
        xn_tok = sbl(sc, "f_xn_tok", (128, D), BF16)
        xn_tok_r = Res()
        scr = sbl(sc, "f_scr", (128, D), BF16)
        scr_r = Res()
        st = sbl(sc, "f_st", (128, 64), F32)
        st_r = Res()
        NWU = 3
        wup = [sbl(sc, "wup%d" % k, (128, KC, 256), BF16) for k in range(NWU)]
        wup_r = [Res() for k in range(NWU)]
        NGT = G + 2
        gT = [sbl(sc, "gT%d" % k, (128, TP), BF16) for k in range(NGT)]
        gT_r = [Res() for k in range(NGT)]
        wdn = [sbl(sc, "wdn%d" % k, (128, D), BF16) for k in range(NGT)]
        wdn_r = [Res() for k in range(NGT)]
        ub = [sbl(sc, "ub%d" % k, (128, TP + 2), F32) for k in range(2)]
        TCH = [(0, 512), (512, 512), (1024, 512), (1536, 512), (2048, 128)]
        ub_r = [[Res() for t in TCH] for k in range(2)]
        halo_r = [Res() for k in range(2)]
        cb = [[sbl(sc, "cb%d_%d" % (k, q), (128, 512), F32) for q in range(2)] for k in range(2)]
        cb_r = [[Res() for q in range(2)] for k in range(2)]
        cw_sb = sbl(sc, "cw_sb", (128, 2 * NFC * 3), F32)
        cb_sb = sbl(sc, "cb_sb", (128, 2 * NFC), F32)
        cws_r = Res()

        load_gain(1 if l == 0 else 4)
        P.add("sp", lambda E: [E.dma_start(out=cw_sb[:], in_=convw_d[l]), E.dma_start(out=cb_sb[:], in_=convb_d[l])],
              writes=[cws_r], dma=1)
        for k in range(2):
            P.add("pool", (lambda k: lambda E: E.memset(ub[k][:, 0:2], 0.0))(k), writes=[halo_r[k]])
        for i in range(NT):
            rms_to_xnT(i, xn_tok, xn_tok_r, (lambda i=i: xnT[:, :, i * 128:(i + 1) * 128]), xnT_r[i], scr, scr_r, st, st_r)

        slot = 0
        qq = 0
        for grp in groups:
            gslots = []
            for c in grp:
                wu, wur = wup[c % NWU], wup_r[c % NWU]
                sl = slot % NGT
                slot += 1
                gslots.append(sl)
                P.add("pool", (lambda c=c, wu=wu: lambda E: [
                    E.dma_start(out=wu[:, :, 0:128],
                                in_=w_up_d[l, :, c * 128:(c + 1) * 128].rearrange("(kc k) n -> k kc n", k=128)),
                    E.dma_start(out=wu[:, :, 128:256],
                                in_=w_up_d[l, :, DFF + c * 128:DFF + (c + 1) * 128].rearrange("(kc k) n -> k kc n", k=128)),
                ])(), writes=[wur], dma=1)
                P.add("pool", (lambda c=c, sl=sl: lambda E: E.dma_start(
                    out=wdn[sl][:], in_=w_down_d[l, c * 128:(c + 1) * 128, :]))(), writes=[wdn_r[sl]], dma=1)
                for ti, (t0, tw) in enumerate(TCH):
                    for k in range(2):
                        pt, pr = next_pp()

                        def mm(E, pt=pt, k=k, t0=t0, tw=tw, wu=wu):
                            ins = None
                            for kc in range(KC):
                                ins = E.matmul(pt[:, 0:tw], wu[:, kc, k * 128:(k + 1) * 128], xnT[:, kc, t0:t0 + tw],
                                               start=(kc == 0), stop=(kc == KC - 1))
                            return ins
                        P.add("pe", mm, reads=[wur] + xnT_r[t0 // 128:(t0 + tw) // 128], writes=[pr])
                        ci = k * NFC + c
                        cbuf, cbr = cb[k][qq % 2], cb_r[k][qq % 2]
                        P.add("act", (lambda pt=pt, k=k, t0=t0, tw=tw: lambda E: E.copy(
                            out=ub[k][:, 2 + t0:2 + t0 + tw], in_=pt[:, 0:tw]))(), reads=[pr], writes=[ub_r[k][ti]])
                        P.add("act", (lambda pt=pt, tw=tw, ci=ci, cbuf=cbuf: lambda E: E.activation(
                            out=cbuf[:, 0:tw], in_=pt[:, 0:tw], func=AF.Identity,
                            scale=cw_sb[:, ci * 3 + 2:ci * 3 + 3], bias=cb_sb[:, ci:ci + 1]))(),
                            reads=[pr, cws_r], writes=[cbr])
                        prev = [ub_r[k][ti - 1]] if ti > 0 else [halo_r[k]]
                        P.add("dve", (lambda k=k, t0=t0, tw=tw, ci=ci, cbuf=cbuf: lambda E: E.scalar_tensor_tensor(
                            out=cbuf[:, 0:tw], in0=ub[k][:, 1 + t0:1 + t0 + tw], scalar=cw_sb[:, ci * 3 + 1:ci * 3 + 2],
                            in1=cbuf[:, 0:tw], op0=ALU.mult, op1=ALU.add))(),
                            reads=[ub_r[k][ti], cbr, cws_r] + prev, writes=[cbr])
                        P.add("dve", (lambda k=k, t0=t0, tw=tw, ci=ci, cbuf=cbuf: lambda E: E.scalar_tensor_tensor(
                            out=cbuf[:, 0:tw], in0=ub[k][:, t0:t0 + tw], scalar=cw_sb[:, ci * 3:ci * 3 + 1],
                            in1=cbuf[:, 0:tw], op0=ALU.mult, op1=ALU.add))(),
                            reads=[ub_r[k][ti], cbr, cws_r] + prev, writes=[cbr])
                    cg, cgr = cb[0][qq % 2], cb_r[0][qq % 2]
                    cv, cvr = cb[1][qq % 2], cb_r[1][qq % 2]
                    qq += 1
                    P.add("act", (lambda cg=cg, tw=tw: lambda E: E.activation(out=cg[:, 0:tw], in_=cg[:, 0:tw], func=AF.Silu))(),
                          reads=[cgr], writes=[cgr])
                    P.add("dve", (lambda cg=cg, cv=cv, t0=t0, tw=tw, sl=sl: lambda E: E.tensor_tensor(
                        out=gT[sl][:, t0:t0 + tw], in0=cg[:, 0:tw], in1=cv[:, 0:tw], op=ALU.mult))(),
                        reads=[cgr, cvr], writes=[gT_r[sl]])
            for i in range(NT):
                for nh_ in range(2):
                    def mmd(E, i=i, nh_=nh_, gslots=gslots):
                        ins = None
                        for q, sl in enumerate(gslots):
                            ins = E.matmul(py[:], gT[sl][:, i * 128:(i + 1) * 128], wdn[sl][:, nh_ * 512:(nh_ + 1) * 512],
                                           start=(q == 0), stop=(q == len(gslots) - 1))
                        return ins
                    P.add("pe", mmd, reads=[gT_r[s_] for s_ in gslots] + [wdn_r[s_] for s_ in gslots], writes=[py_r])
                    P.add("dve", (lambda i=i, nh_=nh_: lambda E: E.tensor_tensor(
                        out=h[:, i, nh_ * 512:(nh_ + 1) * 512], in0=py[:], in1=h[:, i, nh_ * 512:(nh_ + 1) * 512],
                        op=ALU.add))(), reads=[py_r, h_res[i]], writes=[h_res[i]])
        P.barrier()
        sc.close()

    def layer1_attention():
        sc = contextlib.ExitStack()
        kT12 = sb(sc, "kT12", (128, NH, TP), BF16)
        kT12_r = [Res() for i in range(NT)]
        vb = sb(sc, "vb", (128, NT, NH, 129), BF16)
        vb_r = [Res() for i in range(NT)]
        gk = sb(sc, "gk1", (128, 4), F32)
        gk_r = Res()
        lamb = sb(sc, "lamb", (128, 4, 64), F32)
        lamt = sb(sc, "lamt", (128, 8), F32)
        lam_r = Res()
        xn_tok = sb(sc, "b_xn_tok", (128, D), BF16)
        xn_tok_r = Res()
        xnT = sb(sc, "b_xnT", (128, KC, 128), BF16)
        xnT_r = Res()
        scr = sb(sc, "b_scr", (128, D), BF16)
        scr_r = Res()
        st = sb(sc, "b_st", (128, 64), F32)
        st_r = Res()
        ks12 = sb(sc, "ks12", (128, NH, 128), BF16)
        ks12_r = Res()

        P.add("sp", lambda E: E.dma_start(out=gk[:, 0:3], in_=vecs_d[2:5, :].rearrange("v p -> p v"),
                                          allow_slow_non_contiguous=True), writes=[gk_r], dma=1)
        P.add("dve", lambda E: E.tensor_scalar(out=gk[:, 0:1], in0=gk[:, 0:1], scalar1=64.0 ** -0.5, scalar2=None,
                                               op0=ALU.mult), reads=[gk_r], writes=[gk_r])
        P.add("dve", lambda E: E.tensor_scalar(out=gk[:, 2:3], in0=gk[:, 2:3], scalar1=1.0 - lam_init, scalar2=None,
                                               op0=ALU.mult), reads=[gk_r], writes=[gk_r])
        P.add("sp", lambda E: E.dma_start(out=lamb[:].rearrange("p a d -> p (a d)"),
                                          in_=lam_d.rearrange("a d -> (a d)").partition_broadcast(128)),
              writes=[lam_r], dma=1)
        P.add("dve", lambda E: E.tensor_tensor(out=lamb[:, 0, :], in0=lamb[:, 0, :], in1=lamb[:, 1, :], op=ALU.mult),
              reads=[lam_r], writes=[lam_r])
        P.add("dve", lambda E: E.tensor_tensor(out=lamb[:, 2, :], in0=lamb[:, 2, :], in1=lamb[:, 3, :], op=ALU.mult),
              reads=[lam_r], writes=[lam_r])
        P.add("dve", lambda E: E.tensor_reduce(out=lamt[:, 0:1], in_=lamb[:, 0, :], axis=AX.X, op=ALU.add),
              reads=[lam_r], writes=[lam_r])
        P.add("dve", lambda E: E.tensor_reduce(out=lamt[:, 1:2], in_=lamb[:, 2, :], axis=AX.X, op=ALU.add),
              reads=[lam_r], writes=[lam_r])
        P.add("act", lambda E: E.activation(out=lamt[:, 2:4], in_=lamt[:, 0:2], func=AF.Exp), reads=[lam_r], writes=[lam_r])
        P.add("dve", lambda E: E.tensor_tensor(out=lamt[:, 4:5], in0=lamt[:, 3:4], in1=lamt[:, 2:3], op=ALU.subtract),
              reads=[lam_r], writes=[lam_r])
        P.add("dve", lambda E: E.tensor_scalar(out=lamt[:, 5:6], in0=lamt[:, 4:5], scalar1=-lam_init, scalar2=None,
                                               op0=ALU.add), reads=[lam_r], writes=[lam_r])
        P.add("pool", lambda E: E.memset(vb[:, :, :, 128:129], 1.0), writes=vb_r)

        kv_sc = contextlib.ExitStack()
        w_kv = sb(kv_sc, "w_kv", (128, KC, 2048), BF16)
        w_kv_r = [Res() for k in range(KC)]
        for kc in range(KC):
            P.add("pool", (lambda kc: lambda E: E.dma_start(out=w_kv[:, kc, :], in_=w_kv_d[kc * 128:(kc + 1) * 128, :]))(kc),
                  writes=[w_kv_r[kc]], dma=1)
        load_gain(2)

        def kv_tile(i):
            rms_to_xnT(i, xn_tok, xn_tok_r, lambda: xnT[:], xnT_r, scr, scr_r, st, st_r)
            for c in range(4):
                pt, pr = next_pp()

                def mm(E, c=c, pt=pt):
                    ins = None
                    for kc in range(KC):
                        ins = E.matmul(pt[:], xnT[:, kc, :], w_kv[:, kc, c * 512:(c + 1) * 512],
                                       start=(kc == 0), stop=(kc == KC - 1))
                    return ins
                P.add("pe", mm, reads=[xnT_r] + w_kv_r, writes=[pr])
                if c < 2:
                    so = 8 + c * 8
                    for hh in range(NH):
                        P.add("act", (lambda hh, pt=pt, so=so: lambda E: E.activation(
                            out=scr[:, hh * 64:(hh + 1) * 64], in_=pt[:, hh * 64:(hh + 1) * 64], func=AF.Square,
                            accum_out=st[:, so + hh:so + hh + 1]))(hh), reads=[pr], writes=[scr_r, st_r])
                    P.add("dve", (lambda so=so: lambda E: E.tensor_scalar(
                        out=st[:, 24 + so:32 + so], in0=st[:, so:so + 8], scalar1=1.0 / 64, scalar2=EPS,
                        op0=ALU.mult, op1=ALU.add))(), reads=[st_r], writes=[st_r])
                    P.add("act", (lambda so=so: lambda E: E.activation(
                        out=st[:, so:so + 8], in_=st[:, 24 + so:32 + so], func=AF.Sqrt))(), reads=[st_r], writes=[st_r])
                    P.add("dve", (lambda so=so: lambda E: E.reciprocal(
                        out=st[:, 24 + so:32 + so], in_=st[:, so:so + 8]))(), reads=[st_r], writes=[st_r])
                    for hh in range(NH):
                        P.add("dve", (lambda hh, pt=pt, so=so, c=c: lambda E: E.tensor_scalar(
                            out=ks12[:, hh, c * 64:(c + 1) * 64], in0=pt[:, hh * 64:(hh + 1) * 64],
                            scalar1=st[:, 24 + so + hh:25 + so + hh], scalar2=None, op0=ALU.mult))(hh),
                            reads=[pr, st_r], writes=[ks12_r])
                else:
                    P.add("act", (lambda pt=pt, c=c: lambda E: E.copy(
                        out=vb[:, i, (c - 2) * 4:(c - 1) * 4, 0:128], in_=pt[:].rearrange("p (g d) -> p g d", g=4)))(),
                        reads=[pr], writes=[vb_r[i]])

            def trk(E):
                ins = None
                for hh in range(NH):
                    ins = E.transpose(out=ptr[:, hh * 128:(hh + 1) * 128], in_=ks12[:, hh, :], identity=ident[:])
                return ins
            P.add("pe", trk, reads=[ks12_r, ident_r], writes=[ptr_r])
            P.add("act", (lambda i=i: lambda E: E.activation(
                out=kT12[:, :, i * 128:(i + 1) * 128], in_=ptr[:].rearrange("p (k t) -> p k t", k=NH),
                func=AF.Copy, scale=gk[:, 1:2]))(), reads=[ptr_r, gk_r], writes=[kT12_r[i]])
        for i_ in range(NT):
            kv_tile(i_)
        P.barrier()
        kv_sc.close()

        w_q = sb(sc, "w_q", (128, KC, D), BF16)
        w_q_r = Res()
        w_o = sb(sc, "w_ob", (128, KC, D), BF16)
        w_o_r = Res()
        qT12 = sb(sc, "qT12", (128, NH, 2, 128), BF16)
        qT12_r = Res()
        P.add("pool", lambda E: E.memset(qT12[:], 0.0), writes=[qT12_r])
        pT = [sb(sc, "b_pT%d" % b, (128, 512), BF16) for b in range(3)]
        pT_r = [Res() for b in range(3)]
        otok = sb(sc, "b_otok", (128, D), BF16)
        otok_r = Res()
        oT = sb(sc, "b_oT", (128, KC, 128), BF16)
        oT_r = Res()
        rz = sb(sc, "b_rz", (128, 8), F32)
        rz_r = Res()
        otmp = [sb(sc, "otmp%d" % b, (128, 128), F32) for b in range(2)]
        otmp_r = [Res() for b in range(2)]
        ost = sb(sc, "ost", (128, 32), F32)
        ost_r = Res()
        P.add("pool", lambda E: [E.dma_start(out=w_q[:, kc, :], in_=w_q_d[kc * 128:(kc + 1) * 128, :]) for kc in range(KC)],
              writes=[w_q_r], dma=1)
        P.add("pool", lambda E: [E.dma_start(out=w_o[:, kc, :], in_=w_o_b_d[kc * 128:(kc + 1) * 128, :]) for kc in range(KC)],
              writes=[w_o_r], dma=1)
        load_gain(3)

        def b_tile(i):
            jmax = min(i + 1, NT - 1)
            rms_to_xnT(i, xn_tok, xn_tok_r, lambda: xnT[:], xnT_r, scr, scr_r, st, st_r)
            for c in range(2):
                pt, pr = next_pp()

                def mm(E, c=c, pt=pt):
                    ins = None
                    for kc in range(KC):
                        ins = E.matmul(pt[:], xnT[:, kc, :], w_q[:, kc, c * 512:(c + 1) * 512],
                                       start=(kc == 0), stop=(kc == KC - 1))
                    return ins
                P.add("pe", mm, reads=[xnT_r, w_q_r], writes=[pr])
                so = 8 + c * 8
                for hh in range(NH):
                    P.add("act", (lambda hh, pt=pt, so=so: lambda E: E.activation(
                        out=scr[:, hh * 64:(hh + 1) * 64], in_=pt[:, hh * 64:(hh + 1) * 64], func=AF.Square,
                        accum_out=st[:, so + hh:so + hh + 1]))(hh), reads=[pr], writes=[scr_r, st_r])
                P.add("dve", (lambda so=so: lambda E: E.tensor_scalar(
                    out=st[:, 24 + so:32 + so], in0=st[:, so:so + 8], scalar1=1.0 / 64, scalar2=EPS,
                    op0=ALU.mult, op1=ALU.add))(), reads=[st_r], writes=[st_r])
                P.add("act", (lambda so=so: lambda E: E.activation(
                    out=st[:, so:so + 8], in_=st[:, 24 + so:32 + so], func=AF.Sqrt))(), reads=[st_r], writes=[st_r])
                P.add("dve", (lambda so=so: lambda E: E.reciprocal(
                    out=st[:, 24 + so:32 + so], in_=st[:, so:so + 8]))(), reads=[st_r], writes=[st_r])
                for hh in range(NH):
                    P.add("dve", (lambda hh, pt=pt, so=so, c=c: lambda E: E.tensor_scalar(
                        out=ks12[:, hh, c * 64:(c + 1) * 64], in0=pt[:, hh * 64:(hh + 1) * 64],
                        scalar1=st[:, 24 + so + hh:25 + so + hh], scalar2=None, op0=ALU.mult))(hh),
                        reads=[pr, st_r], writes=[ks12_r])

            def trq(E):
                ins = None
                for hh in range(NH):
                    ins = E.transpose(out=ptr[:, hh * 128:(hh + 1) * 128], in_=ks12[:, hh, :], identity=ident[:])
                return ins
            P.add("pe", trq, reads=[ks12_r, ident_r], writes=[ptr_r])
            P.add("act", lambda E: E.activation(out=qT12[0:64, :, 0, :],
                                                in_=ptr[0:64, :].rearrange("p (k t) -> p k t", k=NH),
                                                func=AF.Copy, scale=gk[0:64, 0:1]),
                  reads=[ptr_r, gk_r], writes=[qT12_r])
            P.add("act", lambda E: E.activation(out=qT12[64:128, :, 1, :],
                                                in_=ptr[64:128, :].rearrange("p (k t) -> p k t", k=NH),
                                                func=AF.Copy, scale=gk[64:128, 0:1]),
                  reads=[ptr_r, gk_r], writes=[qT12_r])
            steps = [(hp, j) for hp in range(4) for j in range(jmax + 1)]
            pstate = {"pi": 0}

            def emit_qk(k):
                hp, j = steps[k]
                pt, pr = next_psc()
                off = j - i
                near = off >= -1

                def mm(E, pt=pt, j=j, hp=hp, off=off, near=near):
                    ins = None
                    for hh in range(2):
                        h_ = hp * 2 + hh
                        ins = E.matmul(pt[:, hh * 256:(hh + 1) * 256].rearrange("p (b t) -> p b t", b=2),
                                       kT12[:, h_, j * 128:(j + 1) * 128], qT12[:, h_, :, :],
                                       start=(hh == 0), stop=not near, skip_group_check=True)
                    if near:
                        for hh in range(2):
                            h_ = hp * 2 + hh
                            for br in range(2):
                                o_ = (hh * 2 + br) * 128
                                ins = E.matmul(pt[:, o_:o_ + 128], ident[:], addm[:, off + 1, h_, :],
                                               start=False, stop=True, skip_group_check=True)
                    return ins
                P.add("pe", mm, reads=[kT12_r[j], qT12_r, ident_r, addm_ready], writes=[pr])
                return pt, pr

            def emit_rest(k, pt, pr):
                hp, j = steps[k]
                pb, pbr = pT[pstate["pi"] % 3], pT_r[pstate["pi"] % 3]
                pstate["pi"] += 1
                P.add("act", (lambda pt=pt, pb=pb: lambda E: E.activation(out=pb[:], in_=pt[:], func=AF.Exp))(),
                      reads=[pr], writes=[pbr])

                def pv(E, pb=pb, j=j, hp=hp):
                    ins = None
                    for q in range(4):
                        h_ = hp * 2 + q // 2
                        ins = E.matmul(po[q // 2][:, (q % 2) * 256:(q % 2) * 256 + 129],
                                       pb[:, q * 128:(q + 1) * 128], vb[:, j, h_, :],
                                       start=(j == 0 and q % 2 == 0), stop=(j == jmax), skip_group_check=True)
                    return ins
                P.add("pe", pv, reads=[pbr, vb_r[j]], writes=po_r)
                if j != jmax:
                    return
                for hh in range(2):
                    h_ = hp * 2 + hh
                    P.add("dve", (lambda hh=hh: lambda E: E.reciprocal(
                        out=rz[:, hh * 2:hh * 2 + 2], in_=po[hh][:].rearrange("p (k c) -> p k c", k=2)[:, :, 128]))(),
                        reads=[po_r[hh]], writes=[rz_r])
                    P.add("dve", (lambda hh=hh: lambda E: E.tensor_scalar(
                        out=rz[:, hh * 2 + 1:hh * 2 + 2], in0=rz[:, hh * 2 + 1:hh * 2 + 2], scalar1=lamt[:, 5:6],
                        scalar2=None, op0=ALU.mult))(), reads=[rz_r, lam_r], writes=[rz_r])
                    ot, otr = otmp[hh], otmp_r[hh]
                    P.add("dve", (lambda hh=hh, ot=ot: lambda E: E.tensor_scalar(
                        out=ot[:], in0=po[hh][:, 0:128], scalar1=rz[:, hh * 2:hh * 2 + 1], scalar2=None, op0=ALU.mult))(),
                        reads=[po_r[hh], rz_r], writes=[otr])
                    P.add("dve", (lambda hh=hh, ot=ot: lambda E: E.scalar_tensor_tensor(
                        out=ot[:], in0=po[hh][:, 256:384], scalar=rz[:, hh * 2 + 1:hh * 2 + 2], in1=ot[:],
                        op0=ALU.mult, op1=ALU.add))(), reads=[po_r[hh], rz_r, otr], writes=[otr])
                    P.add("act", (lambda h_=h_, ot=ot: lambda E: E.activation(
                        out=scr[:, 0:128], in_=ot[:], func=AF.Square, accum_out=ost[:, h_:h_ + 1]))(),
                        reads=[otr], writes=[scr_r, ost_r])
                    P.add("dve", (lambda h_=h_: lambda E: E.tensor_scalar(
                        out=ost[:, 8 + h_:9 + h_], in0=ost[:, h_:h_ + 1], scalar1=1.0 / 128, scalar2=EPS,
                        op0=ALU.mult, op1=ALU.add))(), reads=[ost_r], writes=[ost_r])
                    P.add("act", (lambda h_=h_: lambda E: E.activation(
                        out=ost[:, 16 + h_:17 + h_], in_=ost[:, 8 + h_:9 + h_], func=AF.Sqrt))(), reads=[ost_r], writes=[ost_r])
                    P.add("dve", (lambda h_=h_: lambda E: E.reciprocal(
                        out=ost[:, 24 + h_:25 + h_], in_=ost[:, 16 + h_:17 + h_]))(), reads=[ost_r], writes=[ost_r])
                    P.add("dve", (lambda h_=h_, ot=ot: lambda E: E.tensor_scalar(
                        out=otok[:, h_ * 128:(h_ + 1) * 128], in0=ot[:], scalar1=ost[:, 24 + h_:25 + h_], scalar2=None,
                        op0=ALU.mult))(), reads=[otr, ost_r], writes=[otok_r])

            cur = emit_qk(0)
            for k in range(len(steps)):
                nxt = emit_qk(k + 1) if k + 1 < len(steps) else None
                emit_rest(k, *cur)
                cur = nxt

            def tro(E):
                ins = None
                for kc in range(KC):
                    ins = E.transpose(out=ptr[:, kc * 128:(kc + 1) * 128], in_=otok[:, kc * 128:(kc + 1) * 128],
                                      identity=ident[:])
                return ins
            P.add("pe", tro, reads=[otok_r, ident_r], writes=[ptr_r])
            P.add("act", lambda E: E.activation(out=oT[:], in_=ptr[:].rearrange("p (k t) -> p k t", k=KC),
                                                func=AF.Copy, scale=gk[:, 2:3]),
                  reads=[ptr_r, gk_r], writes=[oT_r])
            for nh_ in range(2):
                def mmo(E, nh_=nh_):
                    ins = None
                    for kc in range(KC):
                        ins = E.matmul(py[:], oT[:, kc, :], w_o[:, kc, nh_ * 512:(nh_ + 1) * 512],
                                       start=(kc == 0), stop=(kc == KC - 1))
                    return ins
                P.add("pe", mmo, reads=[oT_r, w_o_r], writes=[py_r])
                P.add("dve", (lambda nh_=nh_, i=i: lambda E: E.tensor_tensor(
                    out=h[:, i, nh_ * 512:(nh_ + 1) * 512], in0=py[:], in1=h[:, i, nh_ * 512:(nh_ + 1) * 512],
                    op=ALU.add))(), reads=[py_r, h_res[i]], writes=[h_res[i]])
        for i_ in range(dbg_tiles):
            b_tile(i_)
        P.barrier()
        sc.close()

    stage = dbg_stage if dbg_stage is not None else 99
    import os
    skip01 = os.environ.get("KSKIP01") == "1"
    if not skip01:
        layer0_attention()
    if stage >= 2 and not skip01:
        conv_ffn(0)
    if stage >= 3:
        layer1_attention()
    if stage >= 4:
        conv_ffn(1)

    out_r = Res("out")
    P.add("sp", lambda E: E.dma_start(out=out_d[0:112, :], in_=h[16:128, 0, :]), reads=[h_res[0]], writes=[out_r], dma=1)
    for i in range(1, 16):
        P.add("sp", (lambda i: lambda E: E.dma_start(out=out_d[128 * i - 16:128 * i + 112, :], in_=h[:, i, :]))(i),
              reads=[h_res[i]], writes=[Res()], dma=1)
    P.add("sp", lambda E: E.dma_start(out=out_d[2032:2048, :], in_=h[0:16, 16, :]), reads=[h_res[16]], writes=[Res()], dma=1)
    P.barrier()
    if dbg_stage is not None:
        print("n_ops", len(P.ops))
    P.emit(es)
    es.close()
    return nc


def _host_inputs(inputs):
    f = lambda a: np.ascontiguousarray(np.asarray(a, dtype=np.float32))
    idx, negvis = _static_tables()
    rel = f(inputs["rel_table"])
    table_ext = np.concatenate([rel, np.full((1, NH), NEG, np.float32)], axis=0)
    am = table_ext[idx]
    am = np.ascontiguousarray(am.transpose(1, 0, 3, 2)).reshape(128, 3 * NH * 128)
    gnorm = np.stack([f(inputs["ln_attn_g"])[0], f(inputs["ln_ffn_g"])[0], f(inputs["kv_norm_g"]),
                      f(inputs["ln_attn_g"])[1], f(inputs["ln_ffn_g"])[1]], axis=0)
    vecs = np.zeros((8, 128), np.float32)
    vecs[0] = f(inputs["qn_a"])[0]
    vecs[1] = f(inputs["kn_a"])[0]
    vecs[2] = np.concatenate([f(inputs["qn_b"])[0]] * 2)
    vecs[3] = np.concatenate([f(inputs["kn_b"])] * 2)
    vecs[4] = f(inputs["subln_b"])[0]
    lamv = np.stack([f(inputs["lam_q1"])[0], f(inputs["lam_k1"])[0], f(inputs["lam_q2"])[0], f(inputs["lam_k2"])[0]], 0)
    cw = f(inputs["conv_w"])
    convwT = np.ascontiguousarray(cw.reshape(2, 3, 2 * NFC, 128).transpose(0, 3, 2, 1)).reshape(2, 128, 2 * NFC * 3)
    cbias = f(inputs["conv_b"])
    convbT = np.ascontiguousarray(cbias.reshape(2, 2 * NFC, 128).transpose(0, 2, 1))
    shared = {
        "meta_tokens": f(inputs["meta_tokens"]),
        "gnorm": gnorm,
        "w_in_a": f(inputs["w_in_a"])[0],
        "w_o_a": f(inputs["w_o_a"])[0],
        "w_kv_b": f(inputs["w_kv_b"]),
        "w_q_b": f(inputs["w_q_b"])[0],
        "w_o_b": f(inputs["w_o_b"])[0],
        "w_up": f(inputs["w_up"]),
        "w_down": f(inputs["w_down"]),
        "conv_wT": convwT,
        "conv_bT": convbT,
        "vecs": vecs,
        "lamv": lamv,
        "addmask": am,
        "cfar": np.ascontiguousarray(rel[15:16, :]),
        "negvis": negvis,
        "ident": np.eye(128, dtype=np.float32),
    }
    return shared


def kernel(**inputs):
    shared = _host_inputs(inputs)
    x = np.asarray(inputs["x"], dtype=np.float32)
    nc = build_nc()
    in_maps = []
    for b in range(8):
        m = dict(shared)
        m["x"] = np.ascontiguousarray(x[b])
        in_maps.append(m)
    res = run_bass_kernel_spmd(nc, in_maps, core_ids=list(range(8)))
    out = np.stack([np.asarray(r["out"], dtype=np.float32) for r in res.results], axis=0)
    return out
```

```python
import math
import contextlib
import numpy as np
import concourse.bass as bass
import concourse.mybir as mybir
from concourse.bass_utils import run_bass_kernel_spmd

F32 = mybir.dt.float32
BF16 = mybir.dt.bfloat16
AF = mybir.ActivationFunctionType
ALU = mybir.AluOpType
AX = mybir.AxisListType

D = 1024
S = 2048
NMETA = 16
T = S + NMETA
NT = 17
TP = NT * 128
KC = 8
NH = 8
A_IN = 2120
DFF = 2816
NFC = DFF // 128
EPS = 1e-6
NEG = -30000.0
NEGBIG = -1.0e30
TOPK = 256


class Res:
    __slots__ = ("name", "writers", "readers", "excl")

    def __init__(self, name="", excl=False):
        self.name = name
        self.writers = {}
        self.readers = {}
        self.excl = excl


class Op:
    __slots__ = ("eng", "fn", "dma", "deps", "signal", "sem", "idx")


class Prog:
    ENGS = ("pe", "act", "dve", "pool", "sp")
    NDSEM = 8

    def __init__(self, nc):
        self.nc = nc
        self.ops = []
        self.pending = {e: [] for e in self.ENGS}
        self.last_op = {e: None for e in self.ENGS}
        self.open_dma = []
        import os
        self.cut = int(os.environ.get("KCUT", "100000000"))

    def add(self, eng, fn, reads=(), writes=(), dma=0):
        if len(self.ops) >= self.cut:
            return None
        op = Op()
        op.eng = eng
        op.fn = fn
        op.dma = dma
        op.signal = bool(dma)
        op.sem = None
        op.idx = len(self.ops)
        key = ("dma", op.idx) if dma else eng
        deps = set(self.pending[eng])
        self.pending[eng] = []
        for r in reads:
            for k, w in r.writers.items():
                if k == key and eng == "pe":
                    continue
                deps.add(w)
            if r.excl:
                for k, w in r.readers.items():
                    if k != key:
                        deps.add(w)
        for r in writes:
            for k, w in r.writers.items():
                if k == key:
                    continue
                deps.add(w)
            for k, w in r.readers.items():
                if k == key:
                    continue
                deps.add(w)
        for r in reads:
            r.readers[key] = op.idx
        for r in writes:
            r.writers = {key: op.idx}
            r.readers = {}
        op.deps = sorted(deps)
        for d in op.deps:
            self.ops[d].signal = True
        self.ops.append(op)
        if dma:
            self.open_dma.append(op.idx)
        else:
            self.last_op[eng] = op.idx
        return op

    def barrier(self):
        deps = [v for v in self.last_op.values() if v is not None] + list(self.open_dma)
        for d in deps:
            self.ops[d].signal = True
        for e in self.ENGS:
            self.pending[e] = sorted(set(self.pending[e]) | set(deps))
        self.open_dma = []

    def emit(self, es):
        nc = self.nc
        engs = {"pe": nc.tensor, "act": nc.scalar, "dve": nc.vector, "pool": nc.gpsimd, "sp": nc.sync}
        esem = {e: es.enter_context(nc.semaphore("s_" + e)) for e in self.ENGS}
        ecnt = {e: 0 for e in self.ENGS}
        dsem = {e: [es.enter_context(nc.semaphore("d_%s%d" % (e, i))) for i in range(self.NDSEM)]
                for e in ("sp", "pool", "act")}
        dval = {e: [0] * self.NDSEM for e in dsem}
        dcnt = {e: 0 for e in dsem}
        seen = {e: {} for e in self.ENGS}
        semid = {}

        def wait(e, sem, val):
            k = id(sem)
            semid[k] = sem
            if seen[e].get(k, 0) < val:
                engs[e].wait_ge(sem, val)
                seen[e][k] = val

        for op in self.ops:
            e = op.eng
            E = engs[e]
            for d in op.deps:
                sem, val = self.ops[d].sem
                wait(e, sem, val)
            if op.dma:
                n = dcnt[e]
                dcnt[e] += 1
                slot = n % self.NDSEM
                sem = dsem[e][slot]
                wait(e, sem, dval[e][slot])
                inss = op.fn(E)
                if not isinstance(inss, (list, tuple)):
                    inss = [inss]
                for ins in inss:
                    ins.then_inc(sem, 16)
                dval[e][slot] += 16 * len(inss)
                op.sem = (sem, dval[e][slot])
            else:
                ins = op.fn(E)
                if op.signal:
                    ecnt[e] += 1
                    ins.then_inc(esem[e], 1)
                    op.sem = (esem[e], ecnt[e])
        for d in self.pending["sp"]:
            sem, val = self.ops[d].sem
            wait("sp", sem, val)


def _rel_bucket_np(rel):
    nb = 16
    max_exact = 8
    n = np.abs(rel)
    nf = np.maximum(n, 1).astype(np.float32)
    large = max_exact + (np.log(nf / np.float32(max_exact)) / np.float32(math.log(128 / max_exact))
                         * np.float32(nb - max_exact)).astype(np.int32)
    large = np.minimum(large, nb - 1)
    return np.where(rel > 0, nb, 0) + np.where(n < max_exact, n, large)


def _static_tables():
    s_l = np.arange(128)[:, None]
    t_l = np.arange(128)[None, :]
    lim = np.where(t_l < 16, 16, np.where(t_l < 80, 80, 128))
    idx = np.zeros((3, 128, 128), np.int64)
    for o, off in enumerate((-1, 0, 1)):
        rel = 128 * off + s_l - t_l
        b = _rel_bucket_np(rel)
        if off == -1:
            vis = np.ones((128, 128), bool)
        elif off == 0:
            vis = s_l < lim
        else:
            vis = (t_l >= 80) & (s_l < 16)
        idx[o] = np.where(vis, b, 32)
    tt = np.arange(128)[:, None]
    ss = np.arange(256)[None, :]
    limt = np.where(tt < 16, 16, np.where(tt < 80, 80, 144))
    negvis = np.where(ss < limt, 0.0, NEGBIG).astype(np.float32)
    return idx, negvis


def build_nc(dbg_stage=None, dbg_tiles=NT):
    nc = bass.Bass("TRN2", target_bir_lowering=False)

    def din(name, shape):
        return nc.dram_tensor(name, list(shape), F32, kind="ExternalInput").ap()

    x_d = din("x", (S, D))
    meta_d = din("meta_tokens", (NMETA, D))
    gnorm_d = din("gnorm", (5, D))
    w_in_d = din("w_in_a", (D, A_IN))
    w_o_a_d = din("w_o_a", (D, D))
    w_kv_d = din("w_kv_b", (D, 2048))
    w_q_d = din("w_q_b", (D, D))
    w_o_b_d = din("w_o_b", (D, D))
    w_up_d = din("w_up", (2, D, 2 * DFF))
    w_down_d = din("w_down", (2, DFF, D))
    convw_d = din("conv_wT", (2, 128, 2 * NFC * 3))
    convb_d = din("conv_bT", (2, 128, 2 * NFC))
    vecs_d = din("vecs", (8, 128))
    lam_d = din("lamv", (4, 64))
    addm_d = din("addmask", (128, 3 * NH * 128))
    cfar_d = din("cfar", (1, NH))
    negvis_d = din("negvis", (128, 256))
    ident_d = din("ident", (128, 128))
    out_d = nc.dram_tensor("out", [S, D], F32, kind="ExternalOutput").ap()

    es = contextlib.ExitStack()
    P = Prog(nc)
    lam_init = 0.8 - 0.6 * math.exp(-0.3 * 1)

    def sb(scope, name, shape, dt):
        return scope.enter_context(nc.sbuf_tensor("sb_" + name, list(shape), dt))

    h = sb(es, "h", (128, NT, D), F32)
    h_res = [Res("h%d" % i) for i in range(NT)]
    ident = sb(es, "ident", (128, 128), BF16)
    ident_r = Res("ident")
    gb = sb(es, "gb", (128, D), F32)
    gb_r = Res("gb")
    vecs = sb(es, "vecs", (128, 8), F32)
    vecs_r = Res("vecs")
    addm = sb(es, "addm", (128, 3, NH, 128), BF16)
    addm_r = Res("addm")
    cfar = sb(es, "cfar", (128, NH), F32)
    cfar_r = Res("cfar")
    negvis = sb(es, "negvis", (128, 256), F32)
    negvis_r = Res("negvis")
    small = sb(es, "small", (128, 64), F32)
    nhalf = sb(es, "nhalf", (128, 8), F32)
    nhalf_r = Res("nhalf")
    init_sc = contextlib.ExitStack()
    addm_f = sb(init_sc, "addm_f", (128, 3, NH, 128), F32)
    pp = [es.enter_context(nc.psum_tensor("pp%d" % i, [128, 512], F32)) for i in range(2)]
    pp_r = [Res("pp%d" % i, True) for i in range(2)]
    psc = [es.enter_context(nc.psum_tensor("psc%d" % i, [128, 512], F32)) for i in range(2)]
    psc_r = [Res("psc%d" % i, True) for i in range(2)]
    po = [es.enter_context(nc.psum_tensor("po%d" % i, [128, 512], F32)) for i in range(2)]
    po_r = [Res("po%d" % i, True) for i in range(2)]
    ptr = es.enter_context(nc.psum_tensor("ptr", [128, 1024], BF16))
    ptr_r = Res("ptr", True)
    py = es.enter_context(nc.psum_tensor("py", [128, 512], F32))
    py_r = Res("py", True)

    cnt = {"pp": 0, "psc": 0}

    def next_pp():
        i = cnt["pp"] % 2
        cnt["pp"] += 1
        return pp[i], pp_r[i]

    def next_psc():
        i = cnt["psc"] % 2
        cnt["psc"] += 1
        return psc[i], psc_r[i]

    P.add("sp", lambda E: [
        E.dma_start(out=h[0:16, 0, :], in_=meta_d),
        E.dma_start(out=h[16:128, 0, :], in_=x_d[0:112, :]),
    ], writes=[h_res[0]], dma=1)
    for i in range(1, 16):
        P.add("sp", (lambda i: lambda E: E.dma_start(out=h[:, i, :], in_=x_d[128 * i - 16:128 * i + 112, :]))(i),
              writes=[h_res[i]], dma=1)
    P.add("dve", lambda E: E.memset(h[:, 16, :], 0.0), writes=[h_res[16]])
    P.add("pool", lambda E: E.memset(nhalf[:], -0.5), writes=[nhalf_r])
    P.add("sp", lambda E: E.dma_start(out=h[0:16, 16, :], in_=x_d[2032:2048, :]), writes=[h_res[16]], dma=1)
    P.add("pool", lambda E: E.dma_start(out=ident[:], in_=ident_d), writes=[ident_r], dma=1)
    P.add("sp", lambda E: E.dma_start(out=negvis[:], in_=negvis_d), writes=[negvis_r], dma=1)
    P.add("sp", lambda E: E.dma_start(out=addm_f[:].rearrange("p a h t -> p (a h t)"), in_=addm_d),
          writes=[addm_r], dma=1)
    P.add("sp", lambda E: E.dma_start(out=cfar[:], in_=cfar_d.partition_broadcast(128)), writes=[cfar_r], dma=1)
    for hh in range(NH):
        P.add("dve", (lambda hh: lambda E: E.tensor_scalar(
            out=addm[:, :, hh, :], in0=addm_f[:, :, hh, :], scalar1=cfar[:, hh:hh + 1], scalar2=None,
            op0=ALU.subtract))(hh), reads=[addm_r, cfar_r], writes=[Res()])
    addm_ready = Res("addm_ready")
    P.add("dve", lambda E: E.memset(small[:, 0:1], 0.0), reads=[], writes=[addm_ready])
    P.barrier()
    init_sc.close()

    def load_gain(idx):
        P.add("sp", lambda E: E.dma_start(out=gb[:], in_=gnorm_d[idx:idx + 1, :].partition_broadcast(128)),
              writes=[gb_r], dma=1)

    def rms_to_xnT(i, xn_tok, xn_tok_r, dst_fn, dst_res, scr, scr_r, st, st_r):
        P.add("act", lambda E: E.activation(out=scr[:], in_=h[:, i, :], func=AF.Square, accum_out=st[:, 0:1]),
              reads=[h_res[i]], writes=[scr_r, st_r])
        P.add("dve", lambda E: E.tensor_scalar(out=st[:, 1:2], in0=st[:, 0:1], scalar1=1.0 / D, scalar2=EPS,
                                               op0=ALU.mult, op1=ALU.add), reads=[st_r], writes=[st_r])
        P.add("pool", lambda E: E.tensor_tensor(out=st[:, 3:4], in0=st[:, 1:2], in1=nhalf[:, 0:1], op=ALU.pow),
              reads=[st_r, nhalf_r], writes=[st_r])
        P.add("dve", lambda E: E.scalar_tensor_tensor(out=xn_tok[:], in0=h[:, i, :], scalar=st[:, 3:4], in1=gb[:],
                                                      op0=ALU.mult, op1=ALU.mult),
              reads=[h_res[i], st_r, gb_r], writes=[xn_tok_r])

        def tr(E):
            ins = None
            for kc in range(KC):
                ins = E.transpose(out=ptr[:, kc * 128:(kc + 1) * 128], in_=xn_tok[:, kc * 128:(kc + 1) * 128],
                                  identity=ident[:])
            return ins
        P.add("pe", tr, reads=[xn_tok_r, ident_r], writes=[ptr_r])
        P.add("act", lambda E: E.copy(out=dst_fn(), in_=ptr[:].rearrange("p (k t) -> p k t", k=KC)),
              reads=[ptr_r], writes=[dst_res])

    def layer0_attention():
        sc = contextlib.ExitStack()
        w_in = sb(sc, "w_in", (128, KC, A_IN), BF16)
        w_in_r = [Res("w_in%d" % k) for k in range(KC)]
        w_o = sb(sc, "w_o", (128, KC, D), BF16)
        w_o_r = Res("w_o")
        kT = sb(sc, "kT", (128, 2, TP), BF16)
        kT_r = [Res("kT%d" % i) for i in range(NT)]
        vaug = sb(sc, "vaug", (128, NT, 2, 129), BF16)
        v_r = [Res("v%d" % i) for i in range(NT)]
        kiT = sb(sc, "kiT", (128, TP), BF16)
        kiT_r = [Res("kiT%d" % i) for i in range(NT)]
        NB = 2
        xn_tok = [sb(sc, "xn_tok%d" % b, (128, D), BF16) for b in range(1)] * NB
        xn_tok_r = [Res() for b in range(1)] * NB
        xnT = [sb(sc, "xnT%d" % b, (128, KC, 128), BF16) for b in range(1)] * NB
        xnT_r = [Res() for b in range(1)] * NB
        scr = sb(sc, "scr", (128, D), BF16)
        scr_r = Res("scr")
        st = [sb(sc, "st%d" % b, (128, 64), F32) for b in range(NB)]
        st_r = [Res() for b in range(NB)]
        qs = [sb(sc, "qs%d" % b, (128, NH, 128), BF16) for b in range(1)] * NB
        qs_r = [Res() for b in range(1)] * NB
        ks = [sb(sc, "ks%d" % b, (128, 2, 128), BF16) for b in range(1)] * NB
        ks_r = [Res() for b in range(1)] * NB
        qT = [sb(sc, "qT%d" % b, (128, NH, 128), BF16) for b in range(NB)]
        qT_r = [Res() for b in range(NB)]
        qis = [sb(sc, "qis%d" % b, (128, NH * 64), BF16) for b in range(1)] * NB
        qis_r = [Res() for b in range(1)] * NB
        qiT = [sb(sc, "qiT%d" % b, (128, 4, 128), BF16) for b in range(NB)]
        qiT_r = [Res() for b in range(NB)]
        kis = [sb(sc, "kis%d" % b, (128, 128), BF16) for b in range(1)] * NB
        kis_r = [Res() for b in range(1)] * NB
        wst = [sb(sc, "wst%d" % b, (128, 32), F32) for b in range(NB)]
        wst_r = [Res() for b in range(NB)]
        acc = sb(sc, "acc", (128, TP), F32)
        acc_r = Res("acc")
        work = sb(sc, "work", (128, TP), F32)
        work_r = Res("work")
        rbuf = [sb(sc, "rbuf%d" % b, (128, 512), F32) for b in range(2)]
        rbuf_r = [Res() for b in range(2)]
        m8 = sb(sc, "m8", (128, 8), F32)
        m8_r = Res("m8")
        tb = sb(sc, "tb", (128, 32), F32)
        tb_r = Res("tb")
        iota8 = sb(sc, "iota8", (128, 8), F32)
        iota_r = Res("iota8")
        for q8 in range(8):
            P.add("pool", (lambda q8=q8: lambda E: E.memset(iota8[:, q8:q8 + 1], float(q8)))(), writes=[iota_r])
        sel = work[:].bitcast(BF16)
        sel_r = work_r
        selT = sb(sc, "selT", (128, NT, 128), BF16)
        selT_r = Res("selT")
        pT = [sb(sc, "pT%d" % b, (128, 512), BF16) for b in range(3)]
        pT_r = [Res() for b in range(3)]
        otok = sb(sc, "otok", (128, D), BF16)
        otok_r = Res("otok")
        oT = sb(sc, "oT", (128, KC, 128), BF16)
        oT_r = Res("oT")
        rz = sb(sc, "rz", (128, 8), F32)
        rz_r = Res("rz")
        gq = sb(sc, "gq", (128, 2), F32)
        gq_r = Res("gq")

        for kc in range(KC):
            P.add("pool", (lambda kc: lambda E: E.dma_start(out=w_in[:, kc, :], in_=w_in_d[kc * 128:(kc + 1) * 128, :]))(kc),
                  writes=[w_in_r[kc]], dma=1)
        P.add("pool", lambda E: [E.dma_start(out=w_o[:, kc, :], in_=w_o_a_d[kc * 128:(kc + 1) * 128, :]) for kc in range(KC)],
              writes=[w_o_r], dma=1)
        load_gain(0)
        P.add("sp", lambda E: E.dma_start(out=gq[:], in_=vecs_d[0:2, :].rearrange("v p -> p v"),
                                          allow_slow_non_contiguous=True), writes=[gq_r], dma=1)
        P.add("dve", lambda E: E.tensor_scalar(out=gq[:, 0:1], in0=gq[:, 0:1], scalar1=128.0 ** -0.5, scalar2=None,
                                               op0=ALU.mult), reads=[gq_r], writes=[gq_r])
        P.add("pool", lambda E: E.memset(vaug[:, :, :, 128:129], 1.0), writes=v_r)

        def proj_tile(i):
            b = i % NB
            rms_to_xnT(i, xn_tok[b], xn_tok_r[b], lambda: xnT[b][:], xnT_r[b], scr, scr_r, st[b], st_r[b])
            for c, (c0, cw) in enumerate(((0, 512), (512, 512), (1024, 512), (1536, 512), (2048, 72))):
                pt, pr = next_pp()

                def mm(E, c0=c0, cw=cw, pt=pt):
                    ins = None
                    for kc in range(KC):
                        ins = E.matmul(pt[:, 0:cw], xnT[b][:, kc, :], w_in[:, kc, c0:c0 + cw],
                                       start=(kc == 0), stop=(kc == KC - 1))
                    return ins
                P.add("pe", mm, reads=[xnT_r[b]] + w_in_r, writes=[pr])
                if c in (0, 1, 2):
                    nh = 4 if c < 2 else 2
                    so = 8 + c * 4
                    for hh in range(nh):
                        P.add("act", (lambda hh, pt=pt, so=so: lambda E: E.activation(
                            out=scr[:, hh * 128:(hh + 1) * 128], in_=pt[:, hh * 128:(hh + 1) * 128], func=AF.Square,
                            accum_out=st[b][:, so + hh:so + hh + 1]))(hh),
                            reads=[pr], writes=[scr_r, st_r[b]])
                    P.add("dve", (lambda so=so, nh=nh: lambda E: E.tensor_scalar(
                        out=st[b][:, 24 + so:24 + so + nh], in0=st[b][:, so:so + nh], scalar1=1.0 / 128, scalar2=EPS,
                        op0=ALU.mult, op1=ALU.add))(), reads=[st_r[b]], writes=[st_r[b]])
                    P.add("pool", (lambda so=so, nh=nh: lambda E: E.tensor_tensor(
                        out=st[b][:, 24 + so:24 + so + nh], in0=st[b][:, 24 + so:24 + so + nh], in1=nhalf[:, 0:nh],
                        op=ALU.pow))(), reads=[st_r[b], nhalf_r], writes=[st_r[b]])
                    for hh in range(nh):
                        if c < 2:
                            dst = qs[b][:, c * 4 + hh, :]
                            dr = qs_r[b]
                        else:
                            dst = ks[b][:, hh, :]
                            dr = ks_r[b]
                        P.add("dve", (lambda hh, dst=dst, pt=pt, so=so: lambda E: E.tensor_scalar(
                            out=dst, in0=pt[:, hh * 128:(hh + 1) * 128], scalar1=st[b][:, 24 + so + hh:24 + so + hh + 1],
                            scalar2=None, op0=ALU.mult))(hh), reads=[pr, st_r[b]], writes=[dr])
                    if c == 2:
                        P.add("act", (lambda pt=pt: lambda E: E.copy(
                            out=vaug[:, i, :, 0:128], in_=pt[:, 256:512].rearrange("p (g d) -> p g d", g=2)))(),
                            reads=[pr], writes=[v_r[i]])
                elif c == 3:
                    pass
                    qi_pt, qi_pr = pt, pr
                else:
                    P.add("dve", (lambda pt=pt: lambda E: E.tensor_scalar(
                        out=wst[b][:, 0:8], in0=pt[:, 64:72], scalar1=0.0, scalar2=2.0, op0=ALU.is_gt, op1=ALU.mult))(),
                        reads=[pr], writes=[wst_r[b]])
                    P.add("dve", lambda E: E.tensor_scalar(
                        out=wst[b][:, 0:8], in0=wst[b][:, 0:8], scalar1=-1.0, scalar2=None, op0=ALU.add),
                        reads=[wst_r[b]], writes=[wst_r[b]])
                    P.add("dve", (lambda pt=pt: lambda E: E.scalar_tensor_tensor(
                        out=wst[b][:, 8:16], in0=pt[:, 64:72], scalar=(8.0 ** -0.5) * (64.0 ** -0.5), in1=wst[b][:, 0:8],
                        op0=ALU.mult, op1=ALU.mult))(), reads=[pr, wst_r[b]], writes=[wst_r[b]])
                    P.add("act", (lambda pt=pt: lambda E: E.copy(out=kis[b][:, 0:64], in_=pt[:, 0:64]))(),
                          reads=[pr], writes=[kis_r[b]])
                    P.add("act", (lambda pt=pt: lambda E: E.copy(out=kis[b][:, 64:128], in_=pt[:, 0:64]))(),
                          reads=[pr], writes=[kis_r[b]])
                    for hh in range(NH):
                        P.add("dve", (lambda hh, qi_pt=qi_pt: lambda E: E.tensor_scalar(
                            out=qis[b][:, hh * 64:(hh + 1) * 64], in0=qi_pt[:, hh * 64:(hh + 1) * 64],
                            scalar1=wst[b][:, 8 + hh:9 + hh], scalar2=None, op0=ALU.mult))(hh),
                            reads=[qi_pr, wst_r[b]], writes=[qis_r[b]])
            def trq(E):
                ins = None
                for hh in range(NH):
                    ins = E.transpose(out=ptr[:, hh * 128:(hh + 1) * 128], in_=qs[b][:, hh, :], identity=ident[:])
                return ins
            P.add("pe", trq, reads=[qs_r[b], ident_r], writes=[ptr_r])
            P.add("act", lambda E: E.activation(out=qT[b][:], in_=ptr[:].rearrange("p (k t) -> p k t", k=NH),
                                                func=AF.Copy, scale=gq[:, 0:1]),
                  reads=[ptr_r, gq_r], writes=[qT_r[b]])

            def trk(E):
                ins = None
                for g in range(2):
                    ins = E.transpose(out=ptr[:, g * 128:(g + 1) * 128], in_=ks[b][:, g, :], identity=ident[:])
                for hp in range(4):
                    ins = E.transpose(out=ptr[:, 256 + hp * 128:256 + (hp + 1) * 128],
                                      in_=qis[b][:, hp * 128:(hp + 1) * 128], identity=ident[:])
                ins = E.transpose(out=ptr[:, 768:896], in_=kis[b][:], identity=ident[:])
                return ins
            P.add("pe", trk, reads=[ks_r[b], qis_r[b], kis_r[b], ident_r], writes=[ptr_r])
            P.add("act", lambda E: E.activation(out=kT[:, :, i * 128:(i + 1) * 128],
                                                in_=ptr[:, 0:256].rearrange("p (g t) -> p g t", g=2),
                                                func=AF.Copy, scale=gq[:, 1:2]),
                  reads=[ptr_r, gq_r], writes=[kT_r[i]])
            P.add("dve", lambda E: E.tensor_copy(out=qiT[b][:], in_=ptr[:, 256:768].rearrange("p (k t) -> p k t", k=4)),
                  reads=[ptr_r], writes=[qiT_r[b]])
            P.add("dve", lambda E: E.tensor_copy(out=kiT[:, i * 128:(i + 1) * 128], in_=ptr[:, 768:896]),
                  reads=[ptr_r], writes=[kiT_r[i]])

        def attn_tile(i):
            b = i % NB
            jmax = min(i + 1, NT - 1)
            nk = 128 * (jmax + 1)
            if i > 0:
                P.add("pool", lambda E: E.memset(acc[:, 0:128 * i], 0.0), writes=[acc_r])
            wv = nk - 128 * i
            P.add("pool", lambda E: E.tensor_copy(out=acc[:, 128 * i:nk], in_=negvis[:, 0:wv]),
                  reads=[negvis_r], writes=[acc_r])
            nchunk = (nk + 511) // 512
            ri = 0
            for hh in range(NH):
                for cch in range(nchunk):
                    c0 = cch * 512
                    cw = min(512, nk - c0)
                    pt, pr = next_psc()
                    po_ = (hh % 2) * 64
                    P.add("pe", (lambda pt=pt, c0=c0, cw=cw, hh=hh, po_=po_: lambda E: E.matmul(
                        pt[:, 0:cw], qiT[b][po_:po_ + 64, hh // 2, :], kiT[po_:po_ + 64, c0:c0 + cw],
                        start=True, stop=True))(),
                        reads=[qiT_r[b]] + kiT_r[0:jmax + 1], writes=[pr])
                    rb, rr = rbuf[ri % 2], rbuf_r[ri % 2]
                    ri += 1
                    P.add("act", (lambda pt=pt, cw=cw, rb=rb: lambda E: E.activation(
                        out=rb[:, 0:cw], in_=pt[:, 0:cw], func=AF.Relu))(), reads=[pr], writes=[rr])
                    P.add("dve", (lambda c0=c0, cw=cw, rb=rb, hh=hh: lambda E: E.scalar_tensor_tensor(
                        out=acc[:, c0:c0 + cw], in0=rb[:, 0:cw], scalar=wst[b][:, hh:hh + 1], in1=acc[:, c0:c0 + cw],
                        op0=ALU.mult, op1=ALU.add))(), reads=[rr, wst_r[b], acc_r], writes=[acc_r])
            if i < 2:
                src = acc
                src_r = acc_r
                for it in range(TOPK // 8):
                    P.add("dve", (lambda src=src: lambda E: E.max(out=m8[:], in_=src[:, 0:nk]))(),
                          reads=[src_r], writes=[m8_r])
                    if it < TOPK // 8 - 1:
                        P.add("dve", (lambda src=src: lambda E: E.match_replace(
                            out=work[:, 0:nk], in_to_replace=m8[:], in_values=src[:, 0:nk], imm_value=-3.0e38))(),
                            reads=[src_r, m8_r], writes=[work_r])
                        src = work
                        src_r = work_r
                thr_ap = m8[:, 7:8]
                thr_res = m8_r
            else:
                NB_IT = 12
                P.add("dve", lambda E: E.tensor_reduce(out=tb[:, 0:1], in_=acc[:, 0:nk], axis=AX.X, op=ALU.max),
                      reads=[acc_r], writes=[tb_r])
                P.add("dve", lambda E: E.tensor_reduce(out=tb[:, 1:2], in_=acc[:, 0:128 * i], axis=AX.X, op=ALU.min),
                      reads=[acc_r], writes=[tb_r])
                P.add("dve", lambda E: E.tensor_tensor(out=tb[:, 2:3], in0=tb[:, 0:1], in1=tb[:, 1:2], op=ALU.subtract),
                      reads=[tb_r], writes=[tb_r])
                for it in range(NB_IT):
                    cc = 0.5 ** (it + 1)
                    P.add("dve", (lambda cc=cc: lambda E: E.scalar_tensor_tensor(
                        out=tb[:, 3:4], in0=tb[:, 2:3], scalar=cc, in1=tb[:, 1:2], op0=ALU.mult, op1=ALU.add))(),
                        reads=[tb_r], writes=[tb_r])
                    P.add("dve", lambda E: E.tensor_scalar(
                        out=sel[:, 0:nk], in0=acc[:, 0:nk], scalar1=tb[:, 3:4], scalar2=None, op0=ALU.is_ge,
                        op1=ALU.add, accum_out=tb[:, 4:5]), reads=[acc_r, tb_r], writes=[work_r, tb_r])
                    P.add("dve", (lambda cc=cc: lambda E: E.tensor_scalar(
                        out=tb[:, 5:6], in0=tb[:, 4:5], scalar1=TOPK - 0.5, scalar2=cc, op0=ALU.is_ge, op1=ALU.mult))(),
                        reads=[tb_r], writes=[tb_r])
                    P.add("dve", lambda E: E.scalar_tensor_tensor(
                        out=tb[:, 1:2], in0=tb[:, 2:3], scalar=tb[:, 5:6], in1=tb[:, 1:2], op0=ALU.mult, op1=ALU.add),
                        reads=[tb_r], writes=[tb_r])
                P.add("dve", lambda E: E.scalar_tensor_tensor(
                    out=tb[:, 6:7], in0=tb[:, 2:3], scalar=0.5 ** NB_IT, in1=tb[:, 1:2], op0=ALU.mult, op1=ALU.add),
                    reads=[tb_r], writes=[tb_r])
                P.add("dve", lambda E: E.tensor_scalar(
                    out=sel[:, 0:nk], in0=acc[:, 0:nk], scalar1=tb[:, 6:7], scalar2=None, op0=ALU.is_ge,
                    op1=ALU.add, accum_out=tb[:, 7:8]), reads=[acc_r, tb_r], writes=[work_r, tb_r])
                P.add("dve", lambda E: E.tensor_scalar(
                    out=work[:, 0:nk], in0=acc[:, 0:nk], scalar1=tb[:, 6:7], scalar2=-1.0, op0=ALU.is_lt, op1=ALU.add),
                    reads=[acc_r, tb_r], writes=[work_r])
                P.add("dve", lambda E: E.scalar_tensor_tensor(
                    out=work[:, 0:nk], in0=work[:, 0:nk], scalar=3.0e38, in1=acc[:, 0:nk], op0=ALU.mult, op1=ALU.add),
                    reads=[acc_r, work_r], writes=[work_r])
                P.add("dve", lambda E: E.max(out=m8[:], in_=work[:, 0:nk]), reads=[work_r], writes=[m8_r])
                P.add("dve", lambda E: E.tensor_scalar(
                    out=tb[:, 8:9], in0=tb[:, 7:8], scalar1=-1.0, scalar2=float(TOPK - 1), op0=ALU.mult, op1=ALU.add),
                    reads=[tb_r], writes=[tb_r])
                P.add("dve", lambda E: E.tensor_scalar(
                    out=tb[:, 8:9], in0=tb[:, 8:9], scalar1=0.0, scalar2=7.0, op0=ALU.max, op1=ALU.min),
                    reads=[tb_r], writes=[tb_r])
                P.add("dve", lambda E: E.tensor_scalar(
                    out=tb[:, 16:24], in0=iota8[:], scalar1=tb[:, 8:9], scalar2=None, op0=ALU.is_equal),
                    reads=[tb_r, iota_r], writes=[tb_r])
                P.add("dve", lambda E: E.tensor_tensor(out=tb[:, 16:24], in0=tb[:, 16:24], in1=m8[:], op=ALU.mult),
                      reads=[tb_r, m8_r], writes=[tb_r])
                P.add("dve", lambda E: E.tensor_reduce(out=tb[:, 9:10], in_=tb[:, 16:24], axis=AX.X, op=ALU.add),
                      reads=[tb_r], writes=[tb_r])
                thr_ap = tb[:, 9:10]
                thr_res = tb_r
            P.add("dve", (lambda thr_ap=thr_ap: lambda E: E.tensor_scalar(
                out=sel[:, 0:nk], in0=acc[:, 0:nk], scalar1=thr_ap, scalar2=None, op0=ALU.is_ge))(),
                reads=[acc_r, thr_res], writes=[sel_r])
            for j0 in range(0, jmax + 1, 8):
                j1 = min(jmax + 1, j0 + 8)

                def trs(E, j0=j0, j1=j1):
                    ins = None
                    for j in range(j0, j1):
                        ins = E.transpose(out=ptr[:, (j - j0) * 128:(j - j0 + 1) * 128], in_=sel[:, j * 128:(j + 1) * 128],
                                          identity=ident[:])
                    return ins
                P.add("pe", trs, reads=[sel_r, ident_r], writes=[ptr_r])
                P.add("act", (lambda j0=j0, j1=j1: lambda E: E.copy(
                    out=selT[:, j0:j1, :], in_=ptr[:, 0:(j1 - j0) * 128].rearrange("p (k t) -> p k t", k=j1 - j0)))(),
                    reads=[ptr_r], writes=[selT_r])
            steps = [(g, j) for g in range(2) for j in range(jmax + 1)]
            pstate = {"pi": 0}

            def emit_qk(k):
                g, j = steps[k]
                pt, pr = next_psc()
                off = j - i
                near = off >= -1

                def mm(E, pt=pt, j=j, g=g, off=off, near=near):
                    ins = E.matmul(pt[:].rearrange("p (k t) -> p k t", k=4), kT[:, g, j * 128:(j + 1) * 128],
                                   qT[b][:, g * 4:(g + 1) * 4, :], start=True, stop=not near)
                    if near:
                        ins = E.matmul(pt[:].rearrange("p (k t) -> p k t", k=4), ident[:],
                                       addm[:, off + 1, g * 4:(g + 1) * 4, :], start=False, stop=True)
                    return ins
                P.add("pe", mm, reads=[kT_r[j], qT_r[b], ident_r, addm_ready], writes=[pr])
                return pt, pr

            def emit_rest(k, pt, pr):
                g, j = steps[k]
                pb, pbr = pT[pstate["pi"] % 3], pT_r[pstate["pi"] % 3]
                pstate["pi"] += 1
                P.add("act", (lambda pt=pt, pb=pb: lambda E: E.activation(out=pb[:], in_=pt[:], func=AF.Exp))(),
                      reads=[pr], writes=[pbr])
                P.add("dve", (lambda pb=pb, j=j: lambda E: E.tensor_tensor(
                    out=pb[:].rearrange("p (k t) -> p k t", k=4), in0=pb[:].rearrange("p (k t) -> p k t", k=4),
                    in1=selT[:, j:j + 1, :].to_broadcast([128, 4, 128]), op=ALU.mult))(),
                    reads=[pbr, selT_r], writes=[pbr])

                def pv(E, pb=pb, j=j, g=g):
                    ins = None
                    for hh in range(4):
                        ins = E.matmul(po[hh // 2][:, (hh % 2) * 256:(hh % 2) * 256 + 129],
                                       pb[:, hh * 128:(hh + 1) * 128], vaug[:, j, g, :],
                                       start=(j == 0 and hh % 2 == 0), stop=(j == jmax),
                                       skip_group_check=True)
                    return ins
                P.add("pe", pv, reads=[pbr, v_r[j]], writes=po_r)
                if j == jmax:
                    for hb in range(2):
                        P.add("dve", (lambda hb=hb, g=g: lambda E: E.reciprocal(
                            out=rz[:, g * 4 + hb * 2:g * 4 + hb * 2 + 2],
                            in_=po[hb][:].rearrange("p (k c) -> p k c", k=2)[:, :, 128]))(),
                            reads=[po_r[hb]], writes=[rz_r])
                    for hh in range(4):
                        P.add("dve", (lambda hh=hh, g=g: lambda E: E.tensor_scalar(
                            out=otok[:, (g * 4 + hh) * 128:(g * 4 + hh + 1) * 128],
                            in0=po[hh // 2][:, (hh % 2) * 256:(hh % 2) * 256 + 128],
                            scalar1=rz[:, g * 4 + hh:g * 4 + hh + 1], scalar2=None, op0=ALU.mult))(),
                            reads=[po_r[hh // 2], rz_r], writes=[otok_r])

            cur = emit_qk(0)
            for k in range(len(steps)):
                nxt = emit_qk(k + 1) if k + 1 < len(steps) else None
                emit_rest(k, *cur)
                cur = nxt
            def tro(E):
                ins = None
                for kc in range(KC):
                    ins = E.transpose(out=ptr[:, kc * 128:(kc + 1) * 128], in_=otok[:, kc * 128:(kc + 1) * 128],
                                      identity=ident[:])
                return ins
            P.add("pe", tro, reads=[otok_r, ident_r], writes=[ptr_r])
            P.add("act", lambda E: E.copy(out=oT[:], in_=ptr[:].rearrange("p (k t) -> p k t", k=KC)),
                  reads=[ptr_r], writes=[oT_r])
            for nh_ in range(2):
                def mmo(E, nh_=nh_):
                    ins = None
                    for kc in range(KC):
                        ins = E.matmul(py[:], oT[:, kc, :], w_o[:, kc, nh_ * 512:(nh_ + 1) * 512],
                                       start=(kc == 0), stop=(kc == KC - 1))
                    return ins
                P.add("pe", mmo, reads=[oT_r, w_o_r], writes=[py_r])
                P.add("dve", (lambda nh_=nh_: lambda E: E.tensor_tensor(
                    out=h[:, i, nh_ * 512:(nh_ + 1) * 512], in0=py[:], in1=h[:, i, nh_ * 512:(nh_ + 1) * 512],
                    op=ALU.add))(), reads=[py_r, h_res[i]], writes=[h_res[i]])

        proj_tile(0)
        for i in range(dbg_tiles):
            if i + 1 < NT:
                proj_tile(i + 1)
            attn_tile(i)
        P.barrier()
        sc.close()

    def conv_ffn(l):
        sc = contextlib.ExitStack()
        sbl = lambda scope, name, shape, dt: sb(scope, "L%d_%s" % (l, name), shape, dt)
        G = 6
        groups = []
        c = 0
        while c < NFC:
            groups.append(list(range(c, min(NFC, c + G))))
            c += G
        xnT = sbl(sc, "f_xnT", (128, KC, TP), BF16)
        xnT_r = [Res() for i in range(NT)]
        xn_tok = sbl(sc, "f_xn_tok", (128, D), BF16)
        xn_tok_r = Res()
        scr = sbl(sc, "f_scr", (128, D), BF16)
        scr_r = Res()
        st = sbl(sc, "f_st", (128, 64), F32)
        st_r = Res()
        NWU = 3
        wup = [sbl(sc, "wup%d" % k, (128, KC, 256), BF16) for k in range(NWU)]
        wup_r = [Res() for k in range(NWU)]
        NGT = G + 2
        gT = [sbl(sc, "gT%d" % k, (128, TP), BF16) for k in range(NGT)]
        gT_r = [Res() for k in range(NGT)]
        wdn = [sbl(sc, "wdn%d" % k, (128, D), BF16) for k in range(NGT)]
        wdn_r = [Res() for k in range(NGT)]
        ub = [sbl(sc, "ub%d" % k, (128, TP + 2), F32) for k in range(2)]
        TCH = [(0, 512), (512, 512), (1024, 512), (1536, 512), (2048, 128)]
        ub_r = [[Res() for t in TCH] for k in range(2)]
        halo_r = [Res() for k in range(2)]
        cb = [[sbl(sc, "cb%d_%d" % (k, q), (128, 512), F32) for q in range(2)] for k in range(2)]
        cb_r = [[Res() for q in range(2)] for k in range(2)]
        cw_sb = sbl(sc, "cw_sb", (128, 2 * NFC * 3), F32)
        cb_sb = sbl(sc, "cb_sb", (128, 2 * NFC), F32)
        cws_r = Res()

        load_gain(1 if l == 0 else 4)
        P.add("sp", lambda E: [E.dma_start(out=cw_sb[:], in_=convw_d[l]), E.dma_start(out=cb_sb[:], in_=convb_d[l])],
              writes=[cws_r], dma=1)
        for k in range(2):
            P.add("pool", (lambda k: lambda E: E.memset(ub[k][:, 0:2], 0.0))(k), writes=[halo_r[k]])
        for i in range(NT):
            rms_to_xnT(i, xn_tok, xn_tok_r, (lambda i=i: xnT[:, :, i * 128:(i + 1) * 128]), xnT_r[i], scr, scr_r, st, st_r)

        slot = 0
        qq = 0
        for grp in groups:
            gslots = []
            for c in grp:
                wu, wur = wup[c % NWU], wup_r[c % NWU]
                sl = slot % NGT
                slot += 1
                gslots.append(sl)
                P.add("pool", (lambda c=c, wu=wu: lambda E: [
                    E.dma_start(out=wu[:, :, 0:128],
                                in_=w_up_d[l, :, c * 128:(c + 1) * 128].rearrange("(kc k) n -> k kc n", k=128)),
                    E.dma_start(out=wu[:, :, 128:256],
                                in_=w_up_d[l, :, DFF + c * 128:DFF + (c + 1) * 128].rearrange("(kc k) n -> k kc n", k=128)),
                ])(), writes=[wur], dma=1)
                P.add("pool", (lambda c=c, sl=sl: lambda E: E.dma_start(
                    out=wdn[sl][:], in_=w_down_d[l, c * 128:(c + 1) * 128, :]))(), writes=[wdn_r[sl]], dma=1)
                for ti, (t0, tw) in enumerate(TCH):
                    for k in range(2):
                        pt, pr = next_pp()

                        def mm(E, pt=pt, k=k, t0=t0, tw=tw, wu=wu):
                            ins = None
                            for kc in range(KC):
                                ins = E.matmul(pt[:, 0:tw], wu[:, kc, k * 128:(k + 1) * 128], xnT[:, kc, t0:t0 + tw],
                                               start=(kc == 0), stop=(kc == KC - 1))
                            return ins
                        P.add("pe", mm, reads=[wur] + xnT_r[t0 // 128:(t0 + tw) // 128], writes=[pr])
                        ci = k * NFC + c
                        cbuf, cbr = cb[k][qq % 2], cb_r[k][qq % 2]
                        P.add("act", (lambda pt=pt, k=k, t0=t0, tw=tw: lambda E: E.copy(
                            out=ub[k][:, 2 + t0:2 + t0 + tw], in_=pt[:, 0:tw]))(), reads=[pr], writes=[ub_r[k][ti]])
                        P.add("act", (lambda pt=pt, tw=tw, ci=ci, cbuf=cbuf: lambda E: E.activation(
                            out=cbuf[:, 0:tw], in_=pt[:, 0:tw], func=AF.Identity,
                            scale=cw_sb[:, ci * 3 + 2:ci * 3 + 3], bias=cb_sb[:, ci:ci + 1]))(),
                            reads=[pr, cws_r], writes=[cbr])
                        prev = [ub_r[k][ti - 1]] if ti > 0 else [halo_r[k]]
                        P.add("dve", (lambda k=k, t0=t0, tw=tw, ci=ci, cbuf=cbuf: lambda E: E.scalar_tensor_tensor(
                            out=cbuf[:, 0:tw], in0=ub[k][:, 1 + t0:1 + t0 + tw], scalar=cw_sb[:, ci * 3 + 1:ci * 3 + 2],
                            in1=cbuf[:, 0:tw], op0=ALU.mult, op1=ALU.add))(),
                            reads=[ub_r[k][ti], cbr, cws_r] + prev, writes=[cbr])
                        P.add("dve", (lambda k=k, t0=t0, tw=tw, ci=ci, cbuf=cbuf: lambda E: E.scalar_tensor_tensor(
                            out=cbuf[:, 0:tw], in0=ub[k][:, t0:t0 + tw], scalar=cw_sb[:, ci * 3:ci * 3 + 1],
                            in1=cbuf[:, 0:tw], op0=ALU.mult, op1=ALU.add))(),
                            reads=[ub_r[k][ti], cbr, cws_r] + prev, writes=[cbr])
                    cg, cgr = cb[0][qq % 2], cb_r[0][qq % 2]
                    cv, cvr = cb[1][qq % 2], cb_r[1][qq % 2]
                    qq += 1
                    P.add("act", (lambda cg=cg, tw=tw: lambda E: E.activation(out=cg[:, 0:tw], in_=cg[:, 0:tw], func=AF.Silu))(),
                          reads=[cgr], writes=[cgr])
                    P.add("dve", (lambda cg=cg, cv=cv, t0=t0, tw=tw, sl=sl: lambda E: E.tensor_tensor(
                        out=gT[sl][:, t0:t0 + tw], in0=cg[:, 0:tw], in1=cv[:, 0:tw], op=ALU.mult))(),
                        reads=[cgr, cvr], writes=[gT_r[sl]])
            for i in range(NT):
                for nh_ in range(2):
                    def mmd(E, i=i, nh_=nh_, gslots=gslots):
                        ins = None
                        for q, sl in enumerate(gslots):
                            ins = E.matmul(py[:], gT[sl][:, i * 128:(i + 1) * 128], wdn[sl][:, nh_ * 512:(nh_ + 1) * 512],
                                           start=(q == 0), stop=(q == len(gslots) - 1))
                        return ins
                    P.add("pe", mmd, reads=[gT_r[s_] for s_ in gslots] + [wdn_r[s_] for s_ in gslots], writes=[py_r])
                    P.add("dve", (lambda i=i, nh_=nh_: lambda E: E.tensor_tensor(
                        out=h[:, i, nh_ * 512:(nh_ + 1) * 512], in0=py[:], in1=h[:, i, nh_ * 512:(nh_ + 1) * 512],
                        op=ALU.add))(), reads=[py_r, h_res[i]], writes=[h_res[i]])
        P.barrier()
        sc.close()

    def layer1_attention():
        sc = contextlib.ExitStack()
        kT12 = sb(sc, "kT12", (128, NH, TP), BF16)
        kT12_r = [Res() for i in range(NT)]
        vb = sb(sc, "vb", (128, NT, NH, 129), BF16)
        vb_r = [Res() for i in range(NT)]
        gk = sb(sc, "gk1", (128, 4), F32)
        gk_r = Res()
        lamb = sb(sc, "lamb", (128, 4, 64), F32)
        lamt = sb(sc, "lamt", (128, 8), F32)
        lam_r = Res()
        xn_tok = sb(sc, "b_xn_tok", (128, D), BF16)
        xn_tok_r = Res()
        xnT = sb(sc, "b_xnT", (128, KC, 128), BF16)
        xnT_r = Res()
        scr = sb(sc, "b_scr", (128, D), BF16)
        scr_r = Res()
        st = sb(sc, "b_st", (128, 64), F32)
        st_r = Res()
        ks12 = sb(sc, "ks12", (128, NH, 128), BF16)
        ks12_r = Res()

        P.add("sp", lambda E: E.dma_start(out=gk[:, 0:3], in_=vecs_d[2:5, :].rearrange("v p -> p v"),
                                          allow_slow_non_contiguous=True), writes=[gk_r], dma=1)
        P.add("dve", lambda E: E.tensor_scalar(out=gk[:, 0:1], in0=gk[:, 0:1], scalar1=64.0 ** -0.5, scalar2=None,
                                               op0=ALU.mult), reads=[gk_r], writes=[gk_r])
        P.add("dve", lambda E: E.tensor_scalar(out=gk[:, 2:3], in0=gk[:, 2:3], scalar1=1.0 - lam_init, scalar2=None,
                                               op0=ALU.mult), reads=[gk_r], writes=[gk_r])
        P.add("sp", lambda E: E.dma_start(out=lamb[:].rearrange("p a d -> p (a d)"),
                                          in_=lam_d.rearrange("a d -> (a d)").partition_broadcast(128)),
              writes=[lam_r], dma=1)
        P.add("dve", lambda E: E.tensor_tensor(out=lamb[:, 0, :], in0=lamb[:, 0, :], in1=lamb[:, 1, :], op=ALU.mult),
              reads=[lam_r], writes=[lam_r])
        P.add("dve", lambda E: E.tensor_tensor(out=lamb[:, 2, :], in0=lamb[:, 2, :], in1=lamb[:, 3, :], op=ALU.mult),
              reads=[lam_r], writes=[lam_r])
        P.add("dve", lambda E: E.tensor_reduce(out=lamt[:, 0:1], in_=lamb[:, 0, :], axis=AX.X, op=ALU.add),
              reads=[lam_r], writes=[lam_r])
        P.add("dve", lambda E: E.tensor_reduce(out=lamt[:, 1:2], in_=lamb[:, 2, :], axis=AX.X, op=ALU.add),
              reads=[lam_r], writes=[lam_r])
        P.add("act", lambda E: E.activation(out=lamt[:, 2:4], in_=lamt[:, 0:2], func=AF.Exp), reads=[lam_r], writes=[lam_r])
        P.add("dve", lambda E: E.tensor_tensor(out=lamt[:, 4:5], in0=lamt[:, 3:4], in1=lamt[:, 2:3], op=ALU.subtract),
              reads=[lam_r], writes=[lam_r])
        P.add("dve", lambda E: E.tensor_scalar(out=lamt[:, 5:6], in0=lamt[:, 4:5], scalar1=-lam_init, scalar2=None,
                                               op0=ALU.add), reads=[lam_r], writes=[lam_r])
        P.add("pool", lambda E: E.memset(vb[:, :, :, 128:129], 1.0), writes=vb_r)

        kv_sc = contextlib.ExitStack()
        w_kv = sb(kv_sc, "w_kv", (128, KC, 2048), BF16)
        w_kv_r = [Res() for k in range(KC)]
        for kc in range(KC):
            P.add("pool", (lambda kc: lambda E: E.dma_start(out=w_kv[:, kc, :], in_=w_kv_d[kc * 128:(kc + 1) * 128, :]))(kc),
                  writes=[w_kv_r[kc]], dma=1)
        load_gain(2)

        def kv_tile(i):
            rms_to_xnT(i, xn_tok, xn_tok_r, lambda: xnT[:], xnT_r, scr, scr_r, st, st_r)
            for c in range(4):
                pt, pr = next_pp()

                def mm(E, c=c, pt=pt):
                    ins = None
                    for kc in range(KC):
                        ins = E.matmul(pt[:], xnT[:, kc, :], w_kv[:, kc, c * 512:(c + 1) * 512],
                                       start=(kc == 0), stop=(kc == KC - 1))
                    return ins
                P.add("pe", mm, reads=[xnT_r] + w_kv_r, writes=[pr])
                if c < 2:
                    so = 8 + c * 8
                    for hh in range(NH):
                        P.add("act", (lambda hh, pt=pt, so=so: lambda E: E.activation(
                            out=scr[:, hh * 64:(hh + 1) * 64], in_=pt[:, hh * 64:(hh + 1) * 64], func=AF.Square,
                            accum_out=st[:, so + hh:so + hh + 1]))(hh), reads=[pr], writes=[scr_r, st_r])
                    P.add("dve", (lambda so=so: lambda E: E.tensor_scalar(
                        out=st[:, 24 + so:32 + so], in0=st[:, so:so + 8], scalar1=1.0 / 64, scalar2=EPS,
                        op0=ALU.mult, op1=ALU.add))(), reads=[st_r], writes=[st_r])
                    P.add("pool", (lambda so=so: lambda E: E.tensor_tensor(
                        out=st[:, 24 + so:32 + so], in0=st[:, 24 + so:32 + so], in1=nhalf[:, 0:8], op=ALU.pow))(),
                        reads=[st_r, nhalf_r], writes=[st_r])
                    for hh in range(NH):
                        P.add("dve", (lambda hh, pt=pt, so=so, c=c: lambda E: E.tensor_scalar(
                            out=ks12[:, hh, c * 64:(c + 1) * 64], in0=pt[:, hh * 64:(hh + 1) * 64],
                            scalar1=st[:, 24 + so + hh:25 + so + hh], scalar2=None, op0=ALU.mult))(hh),
                            reads=[pr, st_r], writes=[ks12_r])
                else:
                    P.add("act", (lambda pt=pt, c=c: lambda E: E.copy(
                        out=vb[:, i, (c - 2) * 4:(c - 1) * 4, 0:128], in_=pt[:].rearrange("p (g d) -> p g d", g=4)))(),
                        reads=[pr], writes=[vb_r[i]])

            def trk(E):
                ins = None
                for hh in range(NH):
                    ins = E.transpose(out=ptr[:, hh * 128:(hh + 1) * 128], in_=ks12[:, hh, :], identity=ident[:])
                return ins
            P.add("pe", trk, reads=[ks12_r, ident_r], writes=[ptr_r])
            P.add("act", (lambda i=i: lambda E: E.activation(
                out=kT12[:, :, i * 128:(i + 1) * 128], in_=ptr[:].rearrange("p (k t) -> p k t", k=NH),
                func=AF.Copy, scale=gk[:, 1:2]))(), reads=[ptr_r, gk_r], writes=[kT12_r[i]])
        for i_ in range(NT):
            kv_tile(i_)
        P.barrier()
        kv_sc.close()

        w_q = sb(sc, "w_q", (128, KC, D), BF16)
        w_q_r = Res()
        w_o = sb(sc, "w_ob", (128, KC, D), BF16)
        w_o_r = Res()
        qT12 = sb(sc, "qT12", (128, NH, 2, 128), BF16)
        qT12_r = Res()
        P.add("pool", lambda E: E.memset(qT12[:], 0.0), writes=[qT12_r])
        pT = [sb(sc, "b_pT%d" % b, (128, 512), BF16) for b in range(3)]
        pT_r = [Res() for b in range(3)]
        otok = sb(sc, "b_otok", (128, D), BF16)
        otok_r = Res()
        oT = sb(sc, "b_oT", (128, KC, 128), BF16)
        oT_r = Res()
        rz = sb(sc, "b_rz", (128, 8), F32)
        rz_r = Res()
        otmp = [sb(sc, "otmp%d" % b, (128, 128), F32) for b in range(2)]
        otmp_r = [Res() for b in range(2)]
        ost = sb(sc, "ost", (128, 32), F32)
        ost_r = Res()
        P.add("pool", lambda E: [E.dma_start(out=w_q[:, kc, :], in_=w_q_d[kc * 128:(kc + 1) * 128, :]) for kc in range(KC)],
              writes=[w_q_r], dma=1)
        P.add("pool", lambda E: [E.dma_start(out=w_o[:, kc, :], in_=w_o_b_d[kc * 128:(kc + 1) * 128, :]) for kc in range(KC)],
              writes=[w_o_r], dma=1)
        load_gain(3)

        def b_tile(i):
            jmax = min(i + 1, NT - 1)
            rms_to_xnT(i, xn_tok, xn_tok_r, lambda: xnT[:], xnT_r, scr, scr_r, st, st_r)
            for c in range(2):
                pt, pr = next_pp()

                def mm(E, c=c, pt=pt):
                    ins = None
                    for kc in range(KC):
                        ins = E.matmul(pt[:], xnT[:, kc, :], w_q[:, kc, c * 512:(c + 1) * 512],
                                       start=(kc == 0), stop=(kc == KC - 1))
                    return ins
                P.add("pe", mm, reads=[xnT_r, w_q_r], writes=[pr])
                so = 8 + c * 8
                for hh in range(NH):
                    P.add("act", (lambda hh, pt=pt, so=so: lambda E: E.activation(
                        out=scr[:, hh * 64:(hh + 1) * 64], in_=pt[:, hh * 64:(hh + 1) * 64], func=AF.Square,
                        accum_out=st[:, so + hh:so + hh + 1]))(hh), reads=[pr], writes=[scr_r, st_r])
                P.add("dve", (lambda so=so: lambda E: E.tensor_scalar(
                    out=st[:, 24 + so:32 + so], in0=st[:, so:so + 8], scalar1=1.0 / 64, scalar2=EPS,
                    op0=ALU.mult, op1=ALU.add))(), reads=[st_r], writes=[st_r])
                P.add("pool", (lambda so=so: lambda E: E.tensor_tensor(
                    out=st[:, 24 + so:32 + so], in0=st[:, 24 + so:32 + so], in1=nhalf[:, 0:8], op=ALU.pow))(),
                    reads=[st_r, nhalf_r], writes=[st_r])
                for hh in range(NH):
                    P.add("dve", (lambda hh, pt=pt, so=so, c=c: lambda E: E.tensor_scalar(
                        out=ks12[:, hh, c * 64:(c + 1) * 64], in0=pt[:, hh * 64:(hh + 1) * 64],
                        scalar1=st[:, 24 + so + hh:25 + so + hh], scalar2=None, op0=ALU.mult))(hh),
                        reads=[pr, st_r], writes=[ks12_r])

            def trq(E):
                ins = None
                for hh in range(NH):
                    ins = E.transpose(out=ptr[:, hh * 128:(hh + 1) * 128], in_=ks12[:, hh, :], identity=ident[:])
                return ins
            P.add("pe", trq, reads=[ks12_r, ident_r], writes=[ptr_r])
            P.add("act", lambda E: E.activation(out=qT12[0:64, :, 0, :],
                                                in_=ptr[0:64, :].rearrange("p (k t) -> p k t", k=NH),
                                                func=AF.Copy, scale=gk[0:64, 0:1]),
                  reads=[ptr_r, gk_r], writes=[qT12_r])
            P.add("act", lambda E: E.activation(out=qT12[64:128, :, 1, :],
                                                in_=ptr[64:128, :].rearrange("p (k t) -> p k t", k=NH),
                                                func=AF.Copy, scale=gk[64:128, 0:1]),
                  reads=[ptr_r, gk_r], writes=[qT12_r])
            steps = [(hp, j) for hp in range(4) for j in range(jmax + 1)]
            pstate = {"pi": 0}

            def emit_qk(k):
                hp, j = steps[k]
                pt, pr = next_psc()
                off = j - i
                near = off >= -1

                def mm(E, pt=pt, j=j, hp=hp, off=off, near=near):
                    ins = None
                    for hh in range(2):
                        h_ = hp * 2 + hh
                        ins = E.matmul(pt[:, hh * 256:(hh + 1) * 256].rearrange("p (b t) -> p b t", b=2),
                                       kT12[:, h_, j * 128:(j + 1) * 128], qT12[:, h_, :, :],
                                       start=(hh == 0), stop=not near, skip_group_check=True)
                    if near:
                        for hh in range(2):
                            h_ = hp * 2 + hh
                            for br in range(2):
                                o_ = (hh * 2 + br) * 128
                                ins = E.matmul(pt[:, o_:o_ + 128], ident[:], addm[:, off + 1, h_, :],
                                               start=False, stop=True, skip_group_check=True)
                    return ins
                P.add("pe", mm, reads=[kT12_r[j], qT12_r, ident_r, addm_ready], writes=[pr])
                return pt, pr

            def emit_rest(k, pt, pr):
                hp, j = steps[k]
                pb, pbr = pT[pstate["pi"] % 3], pT_r[pstate["pi"] % 3]
                pstate["pi"] += 1
                P.add("act", (lambda pt=pt, pb=pb: lambda E: E.activation(out=pb[:], in_=pt[:], func=AF.Exp))(),
                      reads=[pr], writes=[pbr])

                def pv(E, pb=pb, j=j, hp=hp):
                    ins = None
                    for q in range(4):
                        h_ = hp * 2 + q // 2
                        ins = E.matmul(po[q // 2][:, (q % 2) * 256:(q % 2) * 256 + 129],
                                       pb[:, q * 128:(q + 1) * 128], vb[:, j, h_, :],
                                       start=(j == 0 and q % 2 == 0), stop=(j == jmax), skip_group_check=True)
                    return ins
                P.add("pe", pv, reads=[pbr, vb_r[j]], writes=po_r)
                if j != jmax:
                    return
                for hh in range(2):
                    h_ = hp * 2 + hh
                    P.add("dve", (lambda hh=hh: lambda E: E.reciprocal(
                        out=rz[:, hh * 2:hh * 2 + 2], in_=po[hh][:].rearrange("p (k c) -> p k c", k=2)[:, :, 128]))(),
                        reads=[po_r[hh]], writes=[rz_r])
                    P.add("dve", (lambda hh=hh: lambda E: E.tensor_scalar(
                        out=rz[:, hh * 2 + 1:hh * 2 + 2], in0=rz[:, hh * 2 + 1:hh * 2 + 2], scalar1=lamt[:, 5:6],
                        scalar2=None, op0=ALU.mult))(), reads=[rz_r, lam_r], writes=[rz_r])
                    ot, otr = otmp[hh], otmp_r[hh]
                    P.add("dve", (lambda hh=hh, ot=ot: lambda E: E.tensor_scalar(
                        out=ot[:], in0=po[hh][:, 0:128], scalar1=rz[:, hh * 2:hh * 2 + 1], scalar2=None, op0=ALU.mult))(),
                        reads=[po_r[hh], rz_r], writes=[otr])
                    P.add("dve", (lambda hh=hh, ot=ot: lambda E: E.scalar_tensor_tensor(
                        out=ot[:], in0=po[hh][:, 256:384], scalar=rz[:, hh * 2 + 1:hh * 2 + 2], in1=ot[:],
                        op0=ALU.mult, op1=ALU.add))(), reads=[po_r[hh], rz_r, otr], writes=[otr])
                    P.add("act", (lambda h_=h_, ot=ot: lambda E: E.activation(
                        out=scr[:, 0:128], in_=ot[:], func=AF.Square, accum_out=ost[:, h_:h_ + 1]))(),
                        reads=[otr], writes=[scr_r, ost_r])
                    P.add("dve", (lambda h_=h_: lambda E: E.tensor_scalar(
                        out=ost[:, 8 + h_:9 + h_], in0=ost[:, h_:h_ + 1], scalar1=1.0 / 128, scalar2=EPS,
                        op0=ALU.mult, op1=ALU.add))(), reads=[ost_r], writes=[ost_r])
                    P.add("pool", (lambda h_=h_: lambda E: E.tensor_tensor(
                        out=ost[:, 24 + h_:25 + h_], in0=ost[:, 8 + h_:9 + h_], in1=nhalf[:, 0:1], op=ALU.pow))(),
                        reads=[ost_r, nhalf_r], writes=[ost_r])
                    P.add("dve", (lambda h_=h_, ot=ot: lambda E: E.tensor_scalar(
                        out=otok[:, h_ * 128:(h_ + 1) * 128], in0=ot[:], scalar1=ost[:, 24 + h_:25 + h_], scalar2=None,
                        op0=ALU.mult))(), reads=[otr, ost_r], writes=[otok_r])

            cur = emit_qk(0)
            for k in range(len(steps)):
                nxt = emit_qk(k + 1) if k + 1 < len(steps) else None
                emit_rest(k, *cur)
                cur = nxt

            def tro(E):
                ins = None
                for kc in range(KC):
                    ins = E.transpose(out=ptr[:, kc * 128:(kc + 1) * 128], in_=otok[:, kc * 128:(kc + 1) * 128],
                                      identity=ident[:])
                return ins
            P.add("pe", tro, reads=[otok_r, ident_r], writes=[ptr_r])
            P.add("act", lambda E: E.activation(out=oT[:], in_=ptr[:].rearrange("p (k t) -> p k t", k=KC),
                                                func=AF.Copy, scale=gk[:, 2:3]),
                  reads=[ptr_r, gk_r], writes=[oT_r])
            for nh_ in range(2):
                def mmo(E, nh_=nh_):
                    ins = None
                    for kc in range(KC):
                        ins = E.matmul(py[:], oT[:, kc, :], w_o[:, kc, nh_ * 512:(nh_ + 1) * 512],
                                       start=(kc == 0), stop=(kc == KC - 1))
                    return ins
                P.add("pe", mmo, reads=[oT_r, w_o_r], writes=[py_r])
                P.add("dve", (lambda nh_=nh_, i=i: lambda E: E.tensor_tensor(
                    out=h[:, i, nh_ * 512:(nh_ + 1) * 512], in0=py[:], in1=h[:, i, nh_ * 512:(nh_ + 1) * 512],
                    op=ALU.add))(), reads=[py_r, h_res[i]], writes=[h_res[i]])
        for i_ in range(dbg_tiles):
            b_tile(i_)
        P.barrier()
        sc.close()

    stage = dbg_stage if dbg_stage is not None else 99
    import os
    skip01 = os.environ.get("KSKIP01") == "1"
    if not skip01:
        layer0_attention()
    if stage >= 2 and not skip01:
        conv_ffn(0)
    if stage >= 3:
        layer1_attention()
    if stage >= 4:
        conv_ffn(1)

    out_r = Res("out")
    P.add("sp", lambda E: E.dma_start(out=out_d[0:112, :], in_=h[16:128, 0, :]), reads=[h_res[0]], writes=[out_r], dma=1)
    for i in range(1, 16):
        P.add("sp", (lambda i: lambda E: E.dma_start(out=out_d[128 * i - 16:128 * i + 112, :], in_=h[:, i, :]))(i),
              reads=[h_res[i]], writes=[Res()], dma=1)
    P.add("sp", lambda E: E.dma_start(out=out_d[2032:2048, :], in_=h[0:16, 16, :]), reads=[h_res[16]], writes=[Res()], dma=1)
    P.barrier()
    if dbg_stage is not None:
        print("n_ops", len(P.ops))
    P.emit(es)
    es.close()
    return nc


def _host_inputs(inputs):
    f = lambda a: np.ascontiguousarray(np.asarray(a, dtype=np.float32))
    idx, negvis = _static_tables()
    rel = f(inputs["rel_table"])
    table_ext = np.concatenate([rel, np.full((1, NH), NEG, np.float32)], axis=0)
    am = table_ext[idx]
    am = np.ascontiguousarray(am.transpose(1, 0, 3, 2)).reshape(128, 3 * NH * 128)
    gnorm = np.stack([f(inputs["ln_attn_g"])[0], f(inputs["ln_ffn_g"])[0], f(inputs["kv_norm_g"]),
                      f(inputs["ln_attn_g"])[1], f(inputs["ln_ffn_g"])[1]], axis=0)
    vecs = np.zeros((8, 128), np.float32)
    vecs[0] = f(inputs["qn_a"])[0]
    vecs[1] = f(inputs["kn_a"])[0]
    vecs[2] = np.concatenate([f(inputs["qn_b"])[0]] * 2)
    vecs[3] = np.concatenate([f(inputs["kn_b"])] * 2)
    vecs[4] = f(inputs["subln_b"])[0]
    lamv = np.stack([f(inputs["lam_q1"])[0], f(inputs["lam_k1"])[0], f(inputs["lam_q2"])[0], f(inputs["lam_k2"])[0]], 0)
    cw = f(inputs["conv_w"])
    convwT = np.ascontiguousarray(cw.reshape(2, 3, 2 * NFC, 128).transpose(0, 3, 2, 1)).reshape(2, 128, 2 * NFC * 3)
    cbias = f(inputs["conv_b"])
    convbT = np.ascontiguousarray(cbias.reshape(2, 2 * NFC, 128).transpose(0, 2, 1))
    shared = {
        "meta_tokens": f(inputs["meta_tokens"]),
        "gnorm": gnorm,
        "w_in_a": f(inputs["w_in_a"])[0],
        "w_o_a": f(inputs["w_o_a"])[0],
        "w_kv_b": f(inputs["w_kv_b"]),
        "w_q_b": f(inputs["w_q_b"])[0],
        "w_o_b": f(inputs["w_o_b"])[0],
        "w_up": f(inputs["w_up"]),
        "w_down": f(inputs["w_down"]),
        "conv_wT": convwT,
        "conv_bT": convbT,
        "vecs": vecs,
        "lamv": lamv,
        "addmask": am,
        "cfar": np.ascontiguousarray(rel[15:16, :]),
        "negvis": negvis,
        "ident": np.eye(128, dtype=np.float32),
    }
    return shared


def kernel(**inputs):
    shared = _host_inputs(inputs)
    x = np.asarray(inputs["x"], dtype=np.float32)
    nc = build_nc()
    in_maps = []
    for b in range(8):
        m = dict(shared)
        m["x"] = np.ascontiguousarray(x[b])
        in_maps.append(m)
    res = run_bass_kernel_spmd(nc, in_maps, core_ids=list(range(8)))
    out = np.stack([np.asarray(r["out"], dtype=np.float32) for r in res.results], axis=0)
    return out
```

```python
import math
import contextlib
import numpy as np
import concourse.bass as bass
import concourse.mybir as mybir
from concourse.bass_utils import run_bass_kernel_spmd

F32 = mybir.dt.float32
BF16 = mybir.dt.bfloat16
AF = mybir.ActivationFunctionType
ALU = mybir.AluOpType
AX = mybir.AxisListType

D = 1024
S = 2048
NMETA = 16
T = S + NMETA
NT = 17
TP = NT * 128
KC = 8
NH = 8
A_IN = 2120
DFF = 2816
NFC = DFF // 128
EPS = 1e-6
NEG = -30000.0
NEGBIG = -1.0e30
TOPK = 256


class Res:
    __slots__ = ("name", "writers", "readers", "excl")

    def __init__(self, name="", excl=False):
        self.name = name
        self.writers = {}
        self.readers = {}
        self.excl = excl


class Op:
    __slots__ = ("eng", "fn", "dma", "deps", "signal", "sem", "idx")


class Prog:
    ENGS = ("pe", "act", "dve", "pool", "sp")
    NDSEM = 8

    def __init__(self, nc):
        self.nc = nc
        self.ops = []
        self.pending = {e: [] for e in self.ENGS}
        self.last_op = {e: None for e in self.ENGS}
        self.open_dma = []
        import os
        self.cut = int(os.environ.get("KCUT", "100000000"))

    def add(self, eng, fn, reads=(), writes=(), dma=0):
        if len(self.ops) >= self.cut:
            return None
        op = Op()
        op.eng = eng
        op.fn = fn
        op.dma = dma
        op.signal = bool(dma)
        op.sem = None
        op.idx = len(self.ops)
        key = ("dma", op.idx) if dma else eng
        deps = set(self.pending[eng])
        self.pending[eng] = []
        for r in reads:
            for k, w in r.writers.items():
                if k == key and eng == "pe":
                    continue
                deps.add(w)
            if r.excl:
                for k, w in r.readers.items():
                    if k != key:
                        deps.add(w)
        for r in writes:
            for k, w in r.writers.items():
                if k == key:
                    continue
                deps.add(w)
            for k, w in r.readers.items():
                if k == key:
                    continue
                deps.add(w)
        for r in reads:
            r.readers[key] = op.idx
        for r in writes:
            r.writers = {key: op.idx}
            r.readers = {}
        op.deps = sorted(deps)
        for d in op.deps:
            self.ops[d].signal = True
        self.ops.append(op)
        if dma:
            self.open_dma.append(op.idx)
        else:
            self.last_op[eng] = op.idx
        return op

    def barrier(self):
        deps = [v for v in self.last_op.values() if v is not None] + list(self.open_dma)
        for d in deps:
            self.ops[d].signal = True
        for e in self.ENGS:
            self.pending[e] = sorted(set(self.pending[e]) | set(deps))
        self.open_dma = []

    def emit(self, es):
        nc = self.nc
        engs = {"pe": nc.tensor, "act": nc.scalar, "dve": nc.vector, "pool": nc.gpsimd, "sp": nc.sync}
        esem = {e: es.enter_context(nc.semaphore("s_" + e)) for e in self.ENGS}
        ecnt = {e: 0 for e in self.ENGS}
        dsem = {e: [es.enter_context(nc.semaphore("d_%s%d" % (e, i))) for i in range(self.NDSEM)]
                for e in ("sp", "pool", "act")}
        dval = {e: [0] * self.NDSEM for e in dsem}
        dcnt = {e: 0 for e in dsem}
        seen = {e: {} for e in self.ENGS}
        semid = {}

        def wait(e, sem, val):
            k = id(sem)
            semid[k] = sem
            if seen[e].get(k, 0) < val:
                engs[e].wait_ge(sem, val)
                seen[e][k] = val

        for op in self.ops:
            e = op.eng
            E = engs[e]
            need = {}
            for d in op.deps:
                sem, val = self.ops[d].sem
                k = id(sem)
                if k not in need or need[k][1] < val:
                    need[k] = (sem, val)
            for sem, val in need.values():
                wait(e, sem, val)
            if op.dma:
                n = dcnt[e]
                dcnt[e] += 1
                slot = n % self.NDSEM
                sem = dsem[e][slot]
                wait(e, sem, dval[e][slot])
                inss = op.fn(E)
                if not isinstance(inss, (list, tuple)):
                    inss = [inss]
                for ins in inss:
                    ins.then_inc(sem, 16)
                dval[e][slot] += 16 * len(inss)
                op.sem = (sem, dval[e][slot])
            else:
                ins = op.fn(E)
                if op.signal:
                    ecnt[e] += 1
                    ins.then_inc(esem[e], 1)
                    op.sem = (esem[e], ecnt[e])
        for d in self.pending["sp"]:
            sem, val = self.ops[d].sem
            wait("sp", sem, val)


def _rel_bucket_np(rel):
    nb = 16
    max_exact = 8
    n = np.abs(rel)
    nf = np.maximum(n, 1).astype(np.float32)
    large = max_exact + (np.log(nf / np.float32(max_exact)) / np.float32(math.log(128 / max_exact))
                         * np.float32(nb - max_exact)).astype(np.int32)
    large = np.minimum(large, nb - 1)
    return np.where(rel > 0, nb, 0) + np.where(n < max_exact, n, large)


def _static_tables():
    s_l = np.arange(128)[:, None]
    t_l = np.arange(128)[None, :]
    lim = np.where(t_l < 16, 16, np.where(t_l < 80, 80, 128))
    idx = np.zeros((3, 128, 128), np.int64)
    for o, off in enumerate((-1, 0, 1)):
        rel = 128 * off + s_l - t_l
        b = _rel_bucket_np(rel)
        if off == -1:
            vis = np.ones((128, 128), bool)
        elif off == 0:
            vis = s_l < lim
        else:
            vis = (t_l >= 80) & (s_l < 16)
        idx[o] = np.where(vis, b, 32)
    tt = np.arange(128)[:, None]
    ss = np.arange(256)[None, :]
    limt = np.where(tt < 16, 16, np.where(tt < 80, 80, 144))
    negvis = np.where(ss < limt, 0.0, NEGBIG).astype(np.float32)
    return idx, negvis


def build_nc(dbg_stage=None, dbg_tiles=NT):
    nc = bass.Bass("TRN2", target_bir_lowering=False)

    def din(name, shape):
        return nc.dram_tensor(name, list(shape), F32, kind="ExternalInput").ap()

    x_d = din("x", (S, D))
    meta_d = din("meta_tokens", (NMETA, D))
    gnorm_d = din("gnorm", (5, D))
    w_in_d = din("w_in_a", (D, A_IN))
    w_o_a_d = din("w_o_a", (D, D))
    w_kv_d = din("w_kv_b", (D, 2048))
    w_q_d = din("w_q_b", (D, D))
    w_o_b_d = din("w_o_b", (D, D))
    w_up_d = din("w_up", (2, D, 2 * DFF))
    w_down_d = din("w_down", (2, DFF, D))
    convw_d = din("conv_wT", (2, 128, 2 * NFC * 3))
    convb_d = din("conv_bT", (2, 128, 2 * NFC))
    vecs_d = din("vecs", (8, 128))
    lam_d = din("lamv", (4, 64))
    addm_d = din("addmask", (128, 3 * NH * 128))
    cfar_d = din("cfar", (1, NH))
    negvis_d = din("negvis", (128, 256))
    ident_d = din("ident", (128, 128))
    out_d = nc.dram_tensor("out", [S, D], F32, kind="ExternalOutput").ap()

    es = contextlib.ExitStack()
    P = Prog(nc)
    lam_init = 0.8 - 0.6 * math.exp(-0.3 * 1)

    def sb(scope, name, shape, dt):
        return scope.enter_context(nc.sbuf_tensor("sb_" + name, list(shape), dt))

    h = sb(es, "h", (128, NT, D), F32)
    h_res = [Res("h%d" % i) for i in range(NT)]
    ident = sb(es, "ident", (128, 128), BF16)
    ident_r = Res("ident")
    gb = sb(es, "gb", (128, D), F32)
    gb_r = Res("gb")
    vecs = sb(es, "vecs", (128, 8), F32)
    vecs_r = Res("vecs")
    addm = sb(es, "addm", (128, 3, NH, 128), BF16)
    addm_r = Res("addm")
    cfar = sb(es, "cfar", (128, NH), F32)
    cfar_r = Res("cfar")
    negvis = sb(es, "negvis", (128, 256), F32)
    negvis_r = Res("negvis")
    small = sb(es, "small", (128, 64), F32)
    nhalf = sb(es, "nhalf", (128, 8), F32)
    nhalf_r = Res("nhalf")
    init_sc = contextlib.ExitStack()
    addm_f = sb(init_sc, "addm_f", (128, 3, NH, 128), F32)
    big = [es.enter_context(nc.psum_tensor("big%d" % i, [128, 1024], F32)) for i in range(2)]
    pp = [big[0][:, 0:512], big[0][:, 512:1024]]
    pp_r = [Res("pp%d" % i, True) for i in range(2)]
    psc = [big[1][:, 0:512], big[1][:, 512:1024]]
    psc_r = [Res("psc%d" % i, True) for i in range(2)]
    po = [es.enter_context(nc.psum_tensor("po%d" % i, [128, 512], F32)) for i in range(2)]
    po_r = [Res("po%d" % i, True) for i in range(2)]
    ptr = es.enter_context(nc.psum_tensor("ptr", [128, 1024], BF16))
    ptr_r = Res("ptr", True)
    py = es.enter_context(nc.psum_tensor("py", [128, 512], F32))
    py_r = Res("py", True)

    cnt = {"pp": 0, "psc": 0}

    def next_pp():
        i = cnt["pp"] % 2
        cnt["pp"] += 1
        return pp[i], pp_r[i]

    def next_psc():
        i = cnt["psc"] % 2
        cnt["psc"] += 1
        return psc[i], psc_r[i]

    P.add("sp", lambda E: [
        E.dma_start(out=h[0:16, 0, :], in_=meta_d),
        E.dma_start(out=h[16:128, 0, :], in_=x_d[0:112, :]),
    ], writes=[h_res[0]], dma=1)
    for i in range(1, 16):
        P.add("sp", (lambda i: lambda E: E.dma_start(out=h[:, i, :], in_=x_d[128 * i - 16:128 * i + 112, :]))(i),
              writes=[h_res[i]], dma=1)
    P.add("dve", lambda E: E.memset(h[:, 16, :], 0.0), writes=[h_res[16]])
    P.add("pool", lambda E: E.memset(nhalf[:], -0.5), writes=[nhalf_r])
    P.add("sp", lambda E: E.dma_start(out=h[0:16, 16, :], in_=x_d[2032:2048, :]), writes=[h_res[16]], dma=1)
    P.add("pool", lambda E: E.dma_start(out=ident[:], in_=ident_d), writes=[ident_r], dma=1)
    P.add("sp", lambda E: E.dma_start(out=negvis[:], in_=negvis_d), writes=[negvis_r], dma=1)
    P.add("sp", lambda E: E.dma_start(out=addm_f[:].rearrange("p a h t -> p (a h t)"), in_=addm_d),
          writes=[addm_r], dma=1)
    P.add("sp", lambda E: E.dma_start(out=cfar[:], in_=cfar_d.partition_broadcast(128)), writes=[cfar_r], dma=1)
    for hh in range(NH):
        P.add("dve", (lambda hh: lambda E: E.tensor_scalar(
            out=addm[:, :, hh, :], in0=addm_f[:, :, hh, :], scalar1=cfar[:, hh:hh + 1], scalar2=None,
            op0=ALU.subtract))(hh), reads=[addm_r, cfar_r], writes=[Res()])
    addm_ready = Res("addm_ready")
    P.add("dve", lambda E: E.memset(small[:, 0:1], 0.0), reads=[], writes=[addm_ready])
    P.barrier()
    init_sc.close()

    def load_gain(idx):
        P.add("sp", lambda E: E.dma_start(out=gb[:], in_=gnorm_d[idx:idx + 1, :].partition_broadcast(128)),
              writes=[gb_r], dma=1)

    def rms_to_xnT(i, xn_tok, xn_tok_r, dst_fn, dst_res, scr, scr_r, st, st_r):
        P.add("act", lambda E: E.activation(out=scr[:], in_=h[:, i, :], func=AF.Square, accum_out=st[:, 0:1]),
              reads=[h_res[i]], writes=[scr_r, st_r])
        P.add("dve", lambda E: E.tensor_scalar(out=st[:, 1:2], in0=st[:, 0:1], scalar1=1.0 / D, scalar2=EPS,
                                               op0=ALU.mult, op1=ALU.add), reads=[st_r], writes=[st_r])
        P.add("pool", lambda E: E.tensor_tensor(out=st[:, 3:4], in0=st[:, 1:2], in1=nhalf[:, 0:1], op=ALU.pow),
              reads=[st_r, nhalf_r], writes=[st_r])
        P.add("dve", lambda E: E.scalar_tensor_tensor(out=xn_tok[:], in0=h[:, i, :], scalar=st[:, 3:4], in1=gb[:],
                                                      op0=ALU.mult, op1=ALU.mult),
              reads=[h_res[i], st_r, gb_r], writes=[xn_tok_r])

        def tr(E):
            ins = None
            for kc in range(KC):
                ins = E.transpose(out=ptr[:, kc * 128:(kc + 1) * 128], in_=xn_tok[:, kc * 128:(kc + 1) * 128],
                                  identity=ident[:])
            return ins
        P.add("pe", tr, reads=[xn_tok_r, ident_r], writes=[ptr_r])
        P.add("act", lambda E: E.copy(out=dst_fn(), in_=ptr[:].rearrange("p (k t) -> p k t", k=KC)),
              reads=[ptr_r], writes=[dst_res])

    def layer0_attention():
        sc = contextlib.ExitStack()
        w_in = sb(sc, "w_in", (128, KC, A_IN), BF16)
        w_in_r = [Res("w_in%d" % k) for k in range(KC)]
        w_o = sb(sc, "w_o", (128, KC, D), BF16)
        w_o_r = Res("w_o")
        kT = sb(sc, "kT", (128, 2, TP), BF16)
        kT_r = [Res("kT%d" % i) for i in range(NT)]
        vaug = sb(sc, "vaug", (128, NT, 2, 129), BF16)
        v_r = [Res("v%d" % i) for i in range(NT)]
        kiT = sb(sc, "kiT", (128, TP), BF16)
        kiT_r = [Res("kiT%d" % i) for i in range(NT)]
        NB = 2
        xn_tok = [sb(sc, "xn_tok%d" % b, (128, D), BF16) for b in range(1)] * NB
        xn_tok_r = [Res() for b in range(1)] * NB
        xnT = [sb(sc, "xnT%d" % b, (128, KC, 128), BF16) for b in range(1)] * NB
        xnT_r = [Res() for b in range(1)] * NB
        scr = sb(sc, "scr", (128, D), BF16)
        scr_r = Res("scr")
        st = [sb(sc, "st%d" % b, (128, 64), F32) for b in range(NB)]
        st_r = [Res() for b in range(NB)]
        qs = [sb(sc, "qs%d" % b, (128, NH, 128), BF16) for b in range(1)] * NB
        qs_r = [Res() for b in range(1)] * NB
        ks = [sb(sc, "ks%d" % b, (128, 2, 128), BF16) for b in range(1)] * NB
        ks_r = [Res() for b in range(1)] * NB
        qT = [sb(sc, "qT%d" % b, (128, NH, 128), BF16) for b in range(NB)]
        qT_r = [Res() for b in range(NB)]
        qis = [sb(sc, "qis%d" % b, (128, NH * 64), BF16) for b in range(1)] * NB
        qis_r = [Res() for b in range(1)] * NB
        qiT = [sb(sc, "qiT%d" % b, (128, 4, 128), BF16) for b in range(NB)]
        qiT_r = [Res() for b in range(NB)]
        kis = [sb(sc, "kis%d" % b, (128, 128), BF16) for b in range(1)] * NB
        kis_r = [Res() for b in range(1)] * NB
        wst = [sb(sc, "wst%d" % b, (128, 32), F32) for b in range(NB)]
        wst_r = [Res() for b in range(NB)]
        acc = sb(sc, "acc", (128, TP), F32)
        acc_r = Res("acc")
        work = sb(sc, "work", (128, TP), F32)
        work_r = Res("work")
        rbuf = [sb(sc, "rbuf%d" % b, (128, 512), F32) for b in range(2)]
        rbuf_r = [Res() for b in range(2)]
        m8 = sb(sc, "m8", (128, 8), F32)
        m8_r = Res("m8")
        tb = sb(sc, "tb", (128, 32), F32)
        tb_r = Res("tb")
        iota8 = sb(sc, "iota8", (128, 8), F32)
        iota_r = Res("iota8")
        for q8 in range(8):
            P.add("pool", (lambda q8=q8: lambda E: E.memset(iota8[:, q8:q8 + 1], float(q8)))(), writes=[iota_r])
        sel = work[:].bitcast(BF16)
        sel_r = work_r
        selT = sb(sc, "selT", (128, NT, 128), BF16)
        selT_r = Res("selT")
        pT = [sb(sc, "pT%d" % b, (128, 512), BF16) for b in range(3)]
        pT_r = [Res() for b in range(3)]
        otok = sb(sc, "otok", (128, D), BF16)
        otok_r = Res("otok")
        oT = sb(sc, "oT", (128, KC, 128), BF16)
        oT_r = Res("oT")
        rz = sb(sc, "rz", (128, 8), F32)
        rz_r = Res("rz")
        gq = sb(sc, "gq", (128, 2), F32)
        gq_r = Res("gq")

        for kc in range(KC):
            P.add("pool", (lambda kc: lambda E: E.dma_start(out=w_in[:, kc, :], in_=w_in_d[kc * 128:(kc + 1) * 128, :]))(kc),
                  writes=[w_in_r[kc]], dma=1)
        P.add("pool", lambda E: [E.dma_start(out=w_o[:, kc, :], in_=w_o_a_d[kc * 128:(kc + 1) * 128, :]) for kc in range(KC)],
              writes=[w_o_r], dma=1)
        load_gain(0)
        P.add("sp", lambda E: E.dma_start(out=gq[:], in_=vecs_d[0:2, :].rearrange("v p -> p v"),
                                          allow_slow_non_contiguous=True), writes=[gq_r], dma=1)
        P.add("dve", lambda E: E.tensor_scalar(out=gq[:, 0:1], in0=gq[:, 0:1], scalar1=128.0 ** -0.5, scalar2=None,
                                               op0=ALU.mult), reads=[gq_r], writes=[gq_r])
        P.add("pool", lambda E: E.memset(vaug[:, :, :, 128:129], 1.0), writes=v_r)

        def proj_tile(i):
            b = i % NB
            rms_to_xnT(i, xn_tok[b], xn_tok_r[b], lambda: xnT[b][:], xnT_r[b], scr, scr_r, st[b], st_r[b])
            for c, (c0, cw) in enumerate(((0, 512), (512, 512), (1024, 512), (1536, 512), (2048, 72))):
                pt, pr = next_pp()

                def mm(E, c0=c0, cw=cw, pt=pt):
                    ins = None
                    for kc in range(KC):
                        ins = E.matmul(pt[:, 0:cw], xnT[b][:, kc, :], w_in[:, kc, c0:c0 + cw],
                                       start=(kc == 0), stop=(kc == KC - 1))
                    return ins
                P.add("pe", mm, reads=[xnT_r[b]] + w_in_r, writes=[pr])
                if c in (0, 1, 2):
                    nh = 4 if c < 2 else 2
                    so = 8 + c * 4
                    for hh in range(nh):
                        P.add("act", (lambda hh, pt=pt, so=so: lambda E: E.activation(
                            out=scr[:, hh * 128:(hh + 1) * 128], in_=pt[:, hh * 128:(hh + 1) * 128], func=AF.Square,
                            accum_out=st[b][:, so + hh:so + hh + 1]))(hh),
                            reads=[pr], writes=[scr_r, st_r[b]])
                    P.add("dve", (lambda so=so, nh=nh: lambda E: E.tensor_scalar(
                        out=st[b][:, 24 + so:24 + so + nh], in0=st[b][:, so:so + nh], scalar1=1.0 / 128, scalar2=EPS,
                        op0=ALU.mult, op1=ALU.add))(), reads=[st_r[b]], writes=[st_r[b]])
                    P.add("pool", (lambda so=so, nh=nh: lambda E: E.tensor_tensor(
                        out=st[b][:, 24 + so:24 + so + nh], in0=st[b][:, 24 + so:24 + so + nh], in1=nhalf[:, 0:nh],
                        op=ALU.pow))(), reads=[st_r[b], nhalf_r], writes=[st_r[b]])
                    for hh in range(nh):
                        if c < 2:
                            dst = qs[b][:, c * 4 + hh, :]
                            dr = qs_r[b]
                        else:
                            dst = ks[b][:, hh, :]
                            dr = ks_r[b]
                        P.add("dve", (lambda hh, dst=dst, pt=pt, so=so: lambda E: E.tensor_scalar(
                            out=dst, in0=pt[:, hh * 128:(hh + 1) * 128], scalar1=st[b][:, 24 + so + hh:24 + so + hh + 1],
                            scalar2=None, op0=ALU.mult))(hh), reads=[pr, st_r[b]], writes=[dr])
                    if c == 2:
                        P.add("act", (lambda pt=pt: lambda E: E.copy(
                            out=vaug[:, i, :, 0:128], in_=pt[:, 256:512].rearrange("p (g d) -> p g d", g=2)))(),
                            reads=[pr], writes=[v_r[i]])
                elif c == 3:
                    pass
                    qi_pt, qi_pr = pt, pr
                else:
                    P.add("dve", (lambda pt=pt: lambda E: E.tensor_scalar(
                        out=wst[b][:, 0:8], in0=pt[:, 64:72], scalar1=0.0, scalar2=2.0, op0=ALU.is_gt, op1=ALU.mult))(),
                        reads=[pr], writes=[wst_r[b]])
                    P.add("dve", lambda E: E.tensor_scalar(
                        out=wst[b][:, 0:8], in0=wst[b][:, 0:8], scalar1=-1.0, scalar2=None, op0=ALU.add),
                        reads=[wst_r[b]], writes=[wst_r[b]])
                    P.add("dve", (lambda pt=pt: lambda E: E.scalar_tensor_tensor(
                        out=wst[b][:, 8:16], in0=pt[:, 64:72], scalar=(8.0 ** -0.5) * (64.0 ** -0.5), in1=wst[b][:, 0:8],
                        op0=ALU.mult, op1=ALU.mult))(), reads=[pr, wst_r[b]], writes=[wst_r[b]])
                    P.add("act", (lambda pt=pt: lambda E: E.copy(out=kis[b][:, 0:64], in_=pt[:, 0:64]))(),
                          reads=[pr], writes=[kis_r[b]])
                    P.add("act", (lambda pt=pt: lambda E: E.copy(out=kis[b][:, 64:128], in_=pt[:, 0:64]))(),
                          reads=[pr], writes=[kis_r[b]])
                    for hh in range(NH):
                        P.add("dve", (lambda hh, qi_pt=qi_pt: lambda E: E.tensor_scalar(
                            out=qis[b][:, hh * 64:(hh + 1) * 64], in0=qi_pt[:, hh * 64:(hh + 1) * 64],
                            scalar1=wst[b][:, 8 + hh:9 + hh], scalar2=None, op0=ALU.mult))(hh),
                            reads=[qi_pr, wst_r[b]], writes=[qis_r[b]])
            def trq(E):
                ins = None
                for hh in range(NH):
                    ins = E.transpose(out=ptr[:, hh * 128:(hh + 1) * 128], in_=qs[b][:, hh, :], identity=ident[:])
                return ins
            P.add("pe", trq, reads=[qs_r[b], ident_r], writes=[ptr_r])
            P.add("act", lambda E: E.activation(out=qT[b][:], in_=ptr[:].rearrange("p (k t) -> p k t", k=NH),
                                                func=AF.Copy, scale=gq[:, 0:1]),
                  reads=[ptr_r, gq_r], writes=[qT_r[b]])

            def trk(E):
                ins = None
                for g in range(2):
                    ins = E.transpose(out=ptr[:, g * 128:(g + 1) * 128], in_=ks[b][:, g, :], identity=ident[:])
                for hp in range(4):
                    ins = E.transpose(out=ptr[:, 256 + hp * 128:256 + (hp + 1) * 128],
                                      in_=qis[b][:, hp * 128:(hp + 1) * 128], identity=ident[:])
                ins = E.transpose(out=ptr[:, 768:896], in_=kis[b][:], identity=ident[:])
                return ins
            P.add("pe", trk, reads=[ks_r[b], qis_r[b], kis_r[b], ident_r], writes=[ptr_r])
            P.add("act", lambda E: E.activation(out=kT[:, :, i * 128:(i + 1) * 128],
                                                in_=ptr[:, 0:256].rearrange("p (g t) -> p g t", g=2),
                                                func=AF.Copy, scale=gq[:, 1:2]),
                  reads=[ptr_r, gq_r], writes=[kT_r[i]])
            P.add("dve", lambda E: E.tensor_copy(out=qiT[b][:], in_=ptr[:, 256:768].rearrange("p (k t) -> p k t", k=4)),
                  reads=[ptr_r], writes=[qiT_r[b]])
            P.add("dve", lambda E: E.tensor_copy(out=kiT[:, i * 128:(i + 1) * 128], in_=ptr[:, 768:896]),
                  reads=[ptr_r], writes=[kiT_r[i]])

        def attn_tile(i):
            b = i % NB
            jmax = min(i + 1, NT - 1)
            nk = 128 * (jmax + 1)
            if i > 0:
                P.add("pool", lambda E: E.memset(acc[:, 0:128 * i], 0.0), writes=[acc_r])
            wv = nk - 128 * i
            P.add("pool", lambda E: E.tensor_copy(out=acc[:, 128 * i:nk], in_=negvis[:, 0:wv]),
                  reads=[negvis_r], writes=[acc_r])
            nchunk = (nk + 511) // 512
            ri = 0
            for hh in range(NH):
                for cch in range(nchunk):
                    c0 = cch * 512
                    cw = min(512, nk - c0)
                    pt, pr = next_psc()
                    po_ = (hh % 2) * 64
                    P.add("pe", (lambda pt=pt, c0=c0, cw=cw, hh=hh, po_=po_: lambda E: E.matmul(
                        pt[:, 0:cw], qiT[b][po_:po_ + 64, hh // 2, :], kiT[po_:po_ + 64, c0:c0 + cw],
                        start=True, stop=True))(),
                        reads=[qiT_r[b]] + kiT_r[0:jmax + 1], writes=[pr])
                    rb, rr = rbuf[ri % 2], rbuf_r[ri % 2]
                    ri += 1
                    P.add("act", (lambda pt=pt, cw=cw, rb=rb: lambda E: E.activation(
                        out=rb[:, 0:cw], in_=pt[:, 0:cw], func=AF.Relu))(), reads=[pr], writes=[rr])
                    P.add("dve", (lambda c0=c0, cw=cw, rb=rb, hh=hh: lambda E: E.scalar_tensor_tensor(
                        out=acc[:, c0:c0 + cw], in0=rb[:, 0:cw], scalar=wst[b][:, hh:hh + 1], in1=acc[:, c0:c0 + cw],
                        op0=ALU.mult, op1=ALU.add))(), reads=[rr, wst_r[b], acc_r], writes=[acc_r])
            if i < 2:
                src = acc
                src_r = acc_r
                for it in range(TOPK // 8):
                    P.add("dve", (lambda src=src: lambda E: E.max(out=m8[:], in_=src[:, 0:nk]))(),
                          reads=[src_r], writes=[m8_r])
                    if it < TOPK // 8 - 1:
                        P.add("dve", (lambda src=src: lambda E: E.match_replace(
                            out=work[:, 0:nk], in_to_replace=m8[:], in_values=src[:, 0:nk], imm_value=-3.0e38))(),
                            reads=[src_r, m8_r], writes=[work_r])
                        src = work
                        src_r = work_r
                thr_ap = m8[:, 7:8]
                thr_res = m8_r
            else:
                NB_IT = 12
                P.add("dve", lambda E: E.tensor_reduce(out=tb[:, 0:1], in_=acc[:, 0:nk], axis=AX.X, op=ALU.max),
                      reads=[acc_r], writes=[tb_r])
                P.add("dve", lambda E: E.tensor_reduce(out=tb[:, 1:2], in_=acc[:, 0:128 * i], axis=AX.X, op=ALU.min),
                      reads=[acc_r], writes=[tb_r])
                P.add("dve", lambda E: E.tensor_tensor(out=tb[:, 2:3], in0=tb[:, 0:1], in1=tb[:, 1:2], op=ALU.subtract),
                      reads=[tb_r], writes=[tb_r])
                for it in range(NB_IT):
                    cc = 0.5 ** (it + 1)
                    P.add("dve", (lambda cc=cc: lambda E: E.scalar_tensor_tensor(
                        out=tb[:, 3:4], in0=tb[:, 2:3], scalar=cc, in1=tb[:, 1:2], op0=ALU.mult, op1=ALU.add))(),
                        reads=[tb_r], writes=[tb_r])
                    P.add("dve", lambda E: E.tensor_scalar(
                        out=sel[:, 0:nk], in0=acc[:, 0:nk], scalar1=tb[:, 3:4], scalar2=None, op0=ALU.is_ge,
                        op1=ALU.add, accum_out=tb[:, 4:5]), reads=[acc_r, tb_r], writes=[work_r, tb_r])
                    P.add("dve", (lambda cc=cc: lambda E: E.tensor_scalar(
                        out=tb[:, 5:6], in0=tb[:, 4:5], scalar1=TOPK - 0.5, scalar2=cc, op0=ALU.is_ge, op1=ALU.mult))(),
                        reads=[tb_r], writes=[tb_r])
                    P.add("dve", lambda E: E.scalar_tensor_tensor(
                        out=tb[:, 1:2], in0=tb[:, 2:3], scalar=tb[:, 5:6], in1=tb[:, 1:2], op0=ALU.mult, op1=ALU.add),
                        reads=[tb_r], writes=[tb_r])
                P.add("dve", lambda E: E.scalar_tensor_tensor(
                    out=tb[:, 6:7], in0=tb[:, 2:3], scalar=0.5 ** NB_IT, in1=tb[:, 1:2], op0=ALU.mult, op1=ALU.add),
                    reads=[tb_r], writes=[tb_r])
                P.add("dve", lambda E: E.tensor_scalar(
                    out=sel[:, 0:nk], in0=acc[:, 0:nk], scalar1=tb[:, 6:7], scalar2=None, op0=ALU.is_ge,
                    op1=ALU.add, accum_out=tb[:, 7:8]), reads=[acc_r, tb_r], writes=[work_r, tb_r])
                P.add("dve", lambda E: E.tensor_scalar(
                    out=work[:, 0:nk], in0=acc[:, 0:nk], scalar1=tb[:, 6:7], scalar2=-1.0, op0=ALU.is_lt, op1=ALU.add),
                    reads=[acc_r, tb_r], writes=[work_r])
                P.add("dve", lambda E: E.scalar_tensor_tensor(
                    out=work[:, 0:nk], in0=work[:, 0:nk], scalar=3.0e38, in1=acc[:, 0:nk], op0=ALU.mult, op1=ALU.add),
                    reads=[acc_r, work_r], writes=[work_r])
                P.add("dve", lambda E: E.max(out=m8[:], in_=work[:, 0:nk]), reads=[work_r], writes=[m8_r])
                P.add("dve", lambda E: E.tensor_scalar(
                    out=tb[:, 8:9], in0=tb[:, 7:8], scalar1=-1.0, scalar2=float(TOPK - 1), op0=ALU.mult, op1=ALU.add),
                    reads=[tb_r], writes=[tb_r])
                P.add("dve", lambda E: E.tensor_scalar(
                    out=tb[:, 8:9], in0=tb[:, 8:9], scalar1=0.0, scalar2=7.0, op0=ALU.max, op1=ALU.min),
                    reads=[tb_r], writes=[tb_r])
                P.add("dve", lambda E: E.tensor_scalar(
                    out=tb[:, 16:24], in0=iota8[:], scalar1=tb[:, 8:9], scalar2=None, op0=ALU.is_equal),
                    reads=[tb_r, iota_r], writes=[tb_r])
                P.add("dve", lambda E: E.tensor_tensor(out=tb[:, 16:24], in0=tb[:, 16:24], in1=m8[:], op=ALU.mult),
                      reads=[tb_r, m8_r], writes=[tb_r])
                P.add("dve", lambda E: E.tensor_reduce(out=tb[:, 9:10], in_=tb[:, 16:24], axis=AX.X, op=ALU.add),
                      reads=[tb_r], writes=[tb_r])
                thr_ap = tb[:, 9:10]
                thr_res = tb_r
            P.add("dve", (lambda thr_ap=thr_ap: lambda E: E.tensor_scalar(
                out=sel[:, 0:nk], in0=acc[:, 0:nk], scalar1=thr_ap, scalar2=None, op0=ALU.is_ge))(),
                reads=[acc_r, thr_res], writes=[sel_r])
            for j0 in range(0, jmax + 1, 8):
                j1 = min(jmax + 1, j0 + 8)

                def trs(E, j0=j0, j1=j1):
                    ins = None
                    for j in range(j0, j1):
                        ins = E.transpose(out=ptr[:, (j - j0) * 128:(j - j0 + 1) * 128], in_=sel[:, j * 128:(j + 1) * 128],
                                          identity=ident[:])
                    return ins
                P.add("pe", trs, reads=[sel_r, ident_r], writes=[ptr_r])
                P.add("act", (lambda j0=j0, j1=j1: lambda E: E.copy(
                    out=selT[:, j0:j1, :], in_=ptr[:, 0:(j1 - j0) * 128].rearrange("p (k t) -> p k t", k=j1 - j0)))(),
                    reads=[ptr_r], writes=[selT_r])
            steps = [(g, j) for g in range(2) for j in range(jmax + 1)]
            pstate = {"pi": 0}

            def emit_qk(k):
                g, j = steps[k]
                pt, pr = next_psc()
                off = j - i
                near = off >= -1

                def mm(E, pt=pt, j=j, g=g, off=off, near=near):
                    ins = E.matmul(pt[:].rearrange("p (k t) -> p k t", k=4), kT[:, g, j * 128:(j + 1) * 128],
                                   qT[b][:, g * 4:(g + 1) * 4, :], start=True, stop=not near)
                    if near:
                        ins = E.matmul(pt[:].rearrange("p (k t) -> p k t", k=4), ident[:],
                                       addm[:, off + 1, g * 4:(g + 1) * 4, :], start=False, stop=True)
                    return ins
                P.add("pe", mm, reads=[kT_r[j], qT_r[b], ident_r, addm_ready], writes=[pr])
                return pt, pr

            def emit_rest(k, pt, pr):
                g, j = steps[k]
                pb, pbr = pT[pstate["pi"] % 3], pT_r[pstate["pi"] % 3]
                pstate["pi"] += 1
                P.add("act", (lambda pt=pt, pb=pb: lambda E: E.activation(out=pb[:], in_=pt[:], func=AF.Exp))(),
                      reads=[pr], writes=[pbr])
                P.add("dve", (lambda pb=pb, j=j: lambda E: E.tensor_tensor(
                    out=pb[:].rearrange("p (k t) -> p k t", k=4), in0=pb[:].rearrange("p (k t) -> p k t", k=4),
                    in1=selT[:, j:j + 1, :].to_broadcast([128, 4, 128]), op=ALU.mult))(),
                    reads=[pbr, selT_r], writes=[pbr])

                def pv(E, pb=pb, j=j, g=g):
                    ins = None
                    for hh in range(4):
                        ins = E.matmul(po[hh // 2][:, (hh % 2) * 256:(hh % 2) * 256 + 129],
                                       pb[:, hh * 128:(hh + 1) * 128], vaug[:, j, g, :],
                                       start=(j == 0 and hh % 2 == 0), stop=(j == jmax),
                                       skip_group_check=True)
                    return ins
                P.add("pe", pv, reads=[pbr, v_r[j]], writes=po_r)
                if j == jmax:
                    for hb in range(2):
                        P.add("dve", (lambda hb=hb, g=g: lambda E: E.reciprocal(
                            out=rz[:, g * 4 + hb * 2:g * 4 + hb * 2 + 2],
                            in_=po[hb][:].rearrange("p (k c) -> p k c", k=2)[:, :, 128]))(),
                            reads=[po_r[hb]], writes=[rz_r])
                    for hh in range(4):
                        P.add("dve", (lambda hh=hh, g=g: lambda E: E.tensor_scalar(
                            out=otok[:, (g * 4 + hh) * 128:(g * 4 + hh + 1) * 128],
                            in0=po[hh // 2][:, (hh % 2) * 256:(hh % 2) * 256 + 128],
                            scalar1=rz[:, g * 4 + hh:g * 4 + hh + 1], scalar2=None, op0=ALU.mult))(),
                            reads=[po_r[hh // 2], rz_r], writes=[otok_r])

            cur = emit_qk(0)
            for k in range(len(steps)):
                nxt = emit_qk(k + 1) if k + 1 < len(steps) else None
                emit_rest(k, *cur)
                cur = nxt
            def tro(E):
                ins = None
                for kc in range(KC):
                    ins = E.transpose(out=ptr[:, kc * 128:(kc + 1) * 128], in_=otok[:, kc * 128:(kc + 1) * 128],
                                      identity=ident[:])
                return ins
            P.add("pe", tro, reads=[otok_r, ident_r], writes=[ptr_r])
            P.add("act", lambda E: E.copy(out=oT[:], in_=ptr[:].rearrange("p (k t) -> p k t", k=KC)),
                  reads=[ptr_r], writes=[oT_r])
            for nh_ in range(2):
                def mmo(E, nh_=nh_):
                    ins = None
                    for kc in range(KC):
                        ins = E.matmul(py[:], oT[:, kc, :], w_o[:, kc, nh_ * 512:(nh_ + 1) * 512],
                                       start=(kc == 0), stop=(kc == KC - 1))
                    return ins
                P.add("pe", mmo, reads=[oT_r, w_o_r], writes=[py_r])
                P.add("dve", (lambda nh_=nh_: lambda E: E.tensor_tensor(
                    out=h[:, i, nh_ * 512:(nh_ + 1) * 512], in0=py[:], in1=h[:, i, nh_ * 512:(nh_ + 1) * 512],
                    op=ALU.add))(), reads=[py_r, h_res[i]], writes=[h_res[i]])

        proj_tile(0)
        for i in range(dbg_tiles):
            if i + 1 < NT:
                proj_tile(i + 1)
            attn_tile(i)
        P.barrier()
        sc.close()

    def conv_ffn(l):
        sc = contextlib.ExitStack()
        sbl = lambda scope, name, shape, dt: sb(scope, "L%d_%s" % (l, name), shape, dt)
        G = 6
        groups = []
        c = 0
        while c < NFC:
            groups.append(list(range(c, min(NFC, c + G))))
            c += G
        xnT = sbl(sc, "f_xnT", (128, KC, TP), BF16)
        xnT_r = [Res() for i in range(NT)]
        xn_tok = sbl(sc, "f_xn_tok", (128, D), BF16)
        xn_tok_r = Res()
        scr = sbl(sc, "f_scr", (128, D), BF16)
        scr_r = Res()
        st = sbl(sc, "f_st", (128, 64), F32)
        st_r = Res()
        NWU = 3
        wup = [sbl(sc, "wup%d" % k, (128, KC, 256), BF16) for k in range(NWU)]
        wup_r = [Res() for k in range(NWU)]
        NGT = G + 2
        gT = [sbl(sc, "gT%d" % k, (128, TP), BF16) for k in range(NGT)]
        gT_r = [Res() for k in range(NGT)]
        wdn = [sbl(sc, "wdn%d" % k, (128, D), BF16) for k in range(NGT)]
        wdn_r = [Res() for k in range(NGT)]
        ub = [sbl(sc, "ub%d" % k, (128, TP + 2), F32) for k in range(2)]
        TCH = [(0, 512), (512, 512), (1024, 512), (1536, 512), (2048, 128)]
        ub_r = [[Res() for t in TCH] for k in range(2)]
        halo_r = [Res() for k in range(2)]
        cb = [[sbl(sc, "cb%d_%d" % (k, q), (128, 512), F32) for q in range(2)] for k in range(2)]
        cb_r = [[Res() for q in range(2)] for k in range(2)]
        cw_sb = sbl(sc, "cw_sb", (128, 2 * NFC * 3), F32)
        cb_sb = sbl(sc, "cb_sb", (128, 2 * NFC), F32)
        cws_r = Res()

        load_gain(1 if l == 0 else 4)
        P.add("sp", lambda E: [E.dma_start(out=cw_sb[:], in_=convw_d[l]), E.dma_start(out=cb_sb[:], in_=convb_d[l])],
              writes=[cws_r], dma=1)
        for k in range(2):
            P.add("pool", (lambda k: lambda E: E.memset(ub[k][:, 0:2], 0.0))(k), writes=[halo_r[k]])
        for i in range(NT):
            rms_to_xnT(i, xn_tok, xn_tok_r, (lambda i=i: xnT[:, :, i * 128:(i + 1) * 128]), xnT_r[i], scr, scr_r, st, st_r)

        slot = 0
        qq = 0
        for grp in groups:
            gslots = []
            for c in grp:
                wu, wur = wup[c % NWU], wup_r[c % NWU]
                sl = slot % NGT
                slot += 1
                gslots.append(sl)
                P.add("pool", (lambda c=c, wu=wu: lambda E: [
                    E.dma_start(out=wu[:, :, 0:128],
                                in_=w_up_d[l, :, c * 128:(c + 1) * 128].rearrange("(kc k) n -> k kc n", k=128)),
                    E.dma_start(out=wu[:, :, 128:256],
                                in_=w_up_d[l, :, DFF + c * 128:DFF + (c + 1) * 128].rearrange("(kc k) n -> k kc n", k=128)),
                ])(), writes=[wur], dma=1)
                P.add("pool", (lambda c=c, sl=sl: lambda E: E.dma_start(
                    out=wdn[sl][:], in_=w_down_d[l, c * 128:(c + 1) * 128, :]))(), writes=[wdn_r[sl]], dma=1)
                for ti, (t0, tw) in enumerate(TCH):
                    for k in range(2):
                        pt, pr = next_pp()

                        def mm(E, pt=pt, k=k, t0=t0, tw=tw, wu=wu):
                            ins = None
                            for kc in range(KC):
                                ins = E.matmul(pt[:, 0:tw], wu[:, kc, k * 128:(k + 1) * 128], xnT[:, kc, t0:t0 + tw],
                                               start=(kc == 0), stop=(kc == KC - 1))
                            return ins
                        P.add("pe", mm, reads=[wur] + xnT_r[t0 // 128:(t0 + tw) // 128], writes=[pr])
                        ci = k * NFC + c
                        cbuf, cbr = cb[k][qq % 2], cb_r[k][qq % 2]
                        P.add("act", (lambda pt=pt, k=k, t0=t0, tw=tw: lambda E: E.copy(
                            out=ub[k][:, 2 + t0:2 + t0 + tw], in_=pt[:, 0:tw]))(), reads=[pr], writes=[ub_r[k][ti]])
                        P.add("act", (lambda pt=pt, tw=tw, ci=ci, cbuf=cbuf: lambda E: E.activation(
                            out=cbuf[:, 0:tw], in_=pt[:, 0:tw], func=AF.Identity,
                            scale=cw_sb[:, ci * 3 + 2:ci * 3 + 3], bias=cb_sb[:, ci:ci + 1]))(),
                            reads=[pr, cws_r], writes=[cbr])
                        prev = [ub_r[k][ti - 1]] if ti > 0 else [halo_r[k]]
                        P.add("dve", (lambda k=k, t0=t0, tw=tw, ci=ci, cbuf=cbuf: lambda E: E.scalar_tensor_tensor(
                            out=cbuf[:, 0:tw], in0=ub[k][:, 1 + t0:1 + t0 + tw], scalar=cw_sb[:, ci * 3 + 1:ci * 3 + 2],
                            in1=cbuf[:, 0:tw], op0=ALU.mult, op1=ALU.add))(),
                            reads=[ub_r[k][ti], cbr, cws_r] + prev, writes=[cbr])
                        P.add("dve", (lambda k=k, t0=t0, tw=tw, ci=ci, cbuf=cbuf: lambda E: E.scalar_tensor_tensor(
                            out=cbuf[:, 0:tw], in0=ub[k][:, t0:t0 + tw], scalar=cw_sb[:, ci * 3:ci * 3 + 1],
                            in1=cbuf[:, 0:tw], op0=ALU.mult, op1=ALU.add))(),
                            reads=[ub_r[k][ti], cbr, cws_r] + prev, writes=[cbr])
                    cg, cgr = cb[0][qq % 2], cb_r[0][qq % 2]
                    cv, cvr = cb[1][qq % 2], cb_r[1][qq % 2]
                    qq += 1
                    P.add("act", (lambda cg=cg, tw=tw: lambda E: E.activation(out=cg[:, 0:tw], in_=cg[:, 0:tw], func=AF.Silu))(),
                          reads=[cgr], writes=[cgr])
                    P.add("dve", (lambda cg=cg, cv=cv, t0=t0, tw=tw, sl=sl: lambda E: E.tensor_tensor(
                        out=gT[sl][:, t0:t0 + tw], in0=cg[:, 0:tw], in1=cv[:, 0:tw], op=ALU.mult))(),
                        reads=[cgr, cvr], writes=[gT_r[sl]])
            for i in range(NT):
                for nh_ in range(2):
                    def mmd(E, i=i, nh_=nh_, gslots=gslots):
                        ins = None
                        for q, sl in enumerate(gslots):
                            ins = E.matmul(py[:], gT[sl][:, i * 128:(i + 1) * 128], wdn[sl][:, nh_ * 512:(nh_ + 1) * 512],
                                           start=(q == 0), stop=(q == len(gslots) - 1))
                        return ins
                    P.add("pe", mmd, reads=[gT_r[s_] for s_ in gslots] + [wdn_r[s_] for s_ in gslots], writes=[py_r])
                    P.add("dve", (lambda i=i, nh_=nh_: lambda E: E.tensor_tensor(
                        out=h[:, i, nh_ * 512:(nh_ + 1) * 512], in0=py[:], in1=h[:, i, nh_ * 512:(nh_ + 1) * 512],
                        op=ALU.add))(), reads=[py_r, h_res[i]], writes=[h_res[i]])
        P.barrier()
        sc.close()

    def layer1_attention():
        sc = contextlib.ExitStack()
        kT12 = sb(sc, "kT12", (128, NH, TP), BF16)
        kT12_r = [Res() for i in range(NT)]
        vb = sb(sc, "vb", (128, NT, NH, 129), BF16)
        vb_r = [Res() for i in range(NT)]
        gk = sb(sc, "gk1", (128, 4), F32)
        gk_r = Res()
        lamb = sb(sc, "lamb", (128, 4, 64), F32)
        lamt = sb(sc, "lamt", (128, 8), F32)
        lam_r = Res()
        xn_tok = sb(sc, "b_xn_tok", (128, D), BF16)
        xn_tok_r = Res()
        xnT = sb(sc, "b_xnT", (128, KC, 128), BF16)
        xnT_r = Res()
        scr = sb(sc, "b_scr", (128, D), BF16)
        scr_r = Res()
        st = sb(sc, "b_st", (128, 64), F32)
        st_r = Res()
        ks12 = sb(sc, "ks12", (128, NH, 128), BF16)
        ks12_r = Res()

        P.add("sp", lambda E: E.dma_start(out=gk[:, 0:3], in_=vecs_d[2:5, :].rearrange("v p -> p v"),
                                          allow_slow_non_contiguous=True), writes=[gk_r], dma=1)
        P.add("dve", lambda E: E.tensor_scalar(out=gk[:, 0:1], in0=gk[:, 0:1], scalar1=64.0 ** -0.5, scalar2=None,
                                               op0=ALU.mult), reads=[gk_r], writes=[gk_r])
        P.add("dve", lambda E: E.tensor_scalar(out=gk[:, 2:3], in0=gk[:, 2:3], scalar1=1.0 - lam_init, scalar2=None,
                                               op0=ALU.mult), reads=[gk_r], writes=[gk_r])
        P.add("sp", lambda E: E.dma_start(out=lamb[:].rearrange("p a d -> p (a d)"),
                                          in_=lam_d.rearrange("a d -> (a d)").partition_broadcast(128)),
              writes=[lam_r], dma=1)
        P.add("dve", lambda E: E.tensor_tensor(out=lamb[:, 0, :], in0=lamb[:, 0, :], in1=lamb[:, 1, :], op=ALU.mult),
              reads=[lam_r], writes=[lam_r])
        P.add("dve", lambda E: E.tensor_tensor(out=lamb[:, 2, :], in0=lamb[:, 2, :], in1=lamb[:, 3, :], op=ALU.mult),
              reads=[lam_r], writes=[lam_r])
        P.add("dve", lambda E: E.tensor_reduce(out=lamt[:, 0:1], in_=lamb[:, 0, :], axis=AX.X, op=ALU.add),
              reads=[lam_r], writes=[lam_r])
        P.add("dve", lambda E: E.tensor_reduce(out=lamt[:, 1:2], in_=lamb[:, 2, :], axis=AX.X, op=ALU.add),
              reads=[lam_r], writes=[lam_r])
        P.add("act", lambda E: E.activation(out=lamt[:, 2:4], in_=lamt[:, 0:2], func=AF.Exp), reads=[lam_r], writes=[lam_r])
        P.add("dve", lambda E: E.tensor_tensor(out=lamt[:, 4:5], in0=lamt[:, 3:4], in1=lamt[:, 2:3], op=ALU.subtract),
              reads=[lam_r], writes=[lam_r])
        P.add("dve", lambda E: E.tensor_scalar(out=lamt[:, 5:6], in0=lamt[:, 4:5], scalar1=-lam_init, scalar2=None,
                                               op0=ALU.add), reads=[lam_r], writes=[lam_r])
        P.add("pool", lambda E: E.memset(vb[:, :, :, 128:129], 1.0), writes=vb_r)

        kv_sc = contextlib.ExitStack()
        w_kv = sb(kv_sc, "w_kv", (128, KC, 2048), BF16)
        w_kv_r = [Res() for k in range(KC)]
        for kc in range(KC):
            P.add("pool", (lambda kc: lambda E: E.dma_start(out=w_kv[:, kc, :], in_=w_kv_d[kc * 128:(kc + 1) * 128, :]))(kc),
                  writes=[w_kv_r[kc]], dma=1)
        load_gain(2)

        def kv_tile(i):
            rms_to_xnT(i, xn_tok, xn_tok_r, lambda: xnT[:], xnT_r, scr, scr_r, st, st_r)
            for c in range(4):
                pt, pr = next_pp()

                def mm(E, c=c, pt=pt):
                    ins = None
                    for kc in range(KC):
                        ins = E.matmul(pt[:], xnT[:, kc, :], w_kv[:, kc, c * 512:(c + 1) * 512],
                                       start=(kc == 0), stop=(kc == KC - 1))
                    return ins
                P.add("pe", mm, reads=[xnT_r] + w_kv_r, writes=[pr])
                if c < 2:
                    so = 8 + c * 8
                    for hh in range(NH):
                        P.add("act", (lambda hh, pt=pt, so=so: lambda E: E.activation(
                            out=scr[:, hh * 64:(hh + 1) * 64], in_=pt[:, hh * 64:(hh + 1) * 64], func=AF.Square,
                            accum_out=st[:, so + hh:so + hh + 1]))(hh), reads=[pr], writes=[scr_r, st_r])
                    P.add("dve", (lambda so=so: lambda E: E.tensor_scalar(
                        out=st[:, 24 + so:32 + so], in0=st[:, so:so + 8], scalar1=1.0 / 64, scalar2=EPS,
                        op0=ALU.mult, op1=ALU.add))(), reads=[st_r], writes=[st_r])
                    P.add("pool", (lambda so=so: lambda E: E.tensor_tensor(
                        out=st[:, 24 + so:32 + so], in0=st[:, 24 + so:32 + so], in1=nhalf[:, 0:8], op=ALU.pow))(),
                        reads=[st_r, nhalf_r], writes=[st_r])
                    for hh in range(NH):
                        P.add("dve", (lambda hh, pt=pt, so=so, c=c: lambda E: E.tensor_scalar(
                            out=ks12[:, hh, c * 64:(c + 1) * 64], in0=pt[:, hh * 64:(hh + 1) * 64],
                            scalar1=st[:, 24 + so + hh:25 + so + hh], scalar2=None, op0=ALU.mult))(hh),
                            reads=[pr, st_r], writes=[ks12_r])
                else:
                    P.add("act", (lambda pt=pt, c=c: lambda E: E.copy(
                        out=vb[:, i, (c - 2) * 4:(c - 1) * 4, 0:128], in_=pt[:].rearrange("p (g d) -> p g d", g=4)))(),
                        reads=[pr], writes=[vb_r[i]])

            def trk(E):
                ins = None
                for hh in range(NH):
                    ins = E.transpose(out=ptr[:, hh * 128:(hh + 1) * 128], in_=ks12[:, hh, :], identity=ident[:])
                return ins
            P.add("pe", trk, reads=[ks12_r, ident_r], writes=[ptr_r])
            P.add("act", (lambda i=i: lambda E: E.activation(
                out=kT12[:, :, i * 128:(i + 1) * 128], in_=ptr[:].rearrange("p (k t) -> p k t", k=NH),
                func=AF.Copy, scale=gk[:, 1:2]))(), reads=[ptr_r, gk_r], writes=[kT12_r[i]])
        for i_ in range(NT):
            kv_tile(i_)
        P.barrier()
        kv_sc.close()

        w_q = sb(sc, "w_q", (128, KC, D), BF16)
        w_q_r = Res()
        w_o = sb(sc, "w_ob", (128, KC, D), BF16)
        w_o_r = Res()
        qT12 = sb(sc, "qT12", (128, NH, 2, 128), BF16)
        qT12_r = Res()
        P.add("pool", lambda E: E.memset(qT12[:], 0.0), writes=[qT12_r])
        pT = [sb(sc, "b_pT%d" % b, (128, 1024), BF16) for b in range(3)]
        pT_r = [Res() for b in range(3)]
        otok = sb(sc, "b_otok", (128, D), BF16)
        otok_r = Res()
        oT = sb(sc, "b_oT", (128, KC, 128), BF16)
        oT_r = Res()
        rz = sb(sc, "b_rz", (128, 8), F32)
        rz_r = Res()
        otmp = [sb(sc, "otmp%d" % b, (128, 128), F32) for b in range(2)]
        otmp_r = [Res() for b in range(2)]
        ost = sb(sc, "ost", (128, 32), F32)
        ost_r = Res()
        P.add("pool", lambda E: [E.dma_start(out=w_q[:, kc, :], in_=w_q_d[kc * 128:(kc + 1) * 128, :]) for kc in range(KC)],
              writes=[w_q_r], dma=1)
        P.add("pool", lambda E: [E.dma_start(out=w_o[:, kc, :], in_=w_o_b_d[kc * 128:(kc + 1) * 128, :]) for kc in range(KC)],
              writes=[w_o_r], dma=1)
        load_gain(3)

        def b_tile(i):
            jmax = min(i + 1, NT - 1)
            rms_to_xnT(i, xn_tok, xn_tok_r, lambda: xnT[:], xnT_r, scr, scr_r, st, st_r)
            for c in range(2):
                pt, pr = py, py_r

                def mm(E, c=c, pt=pt):
                    ins = None
                    for kc in range(KC):
                        ins = E.matmul(pt[:], xnT[:, kc, :], w_q[:, kc, c * 512:(c + 1) * 512],
                                       start=(kc == 0), stop=(kc == KC - 1))
                    return ins
                P.add("pe", mm, reads=[xnT_r, w_q_r], writes=[pr])
                so = 8 + c * 8
                for hh in range(NH):
                    P.add("act", (lambda hh, pt=pt, so=so: lambda E: E.activation(
                        out=scr[:, hh * 64:(hh + 1) * 64], in_=pt[:, hh * 64:(hh + 1) * 64], func=AF.Square,
                        accum_out=st[:, so + hh:so + hh + 1]))(hh), reads=[pr], writes=[scr_r, st_r])
                P.add("dve", (lambda so=so: lambda E: E.tensor_scalar(
                    out=st[:, 24 + so:32 + so], in0=st[:, so:so + 8], scalar1=1.0 / 64, scalar2=EPS,
                    op0=ALU.mult, op1=ALU.add))(), reads=[st_r], writes=[st_r])
                P.add("pool", (lambda so=so: lambda E: E.tensor_tensor(
                    out=st[:, 24 + so:32 + so], in0=st[:, 24 + so:32 + so], in1=nhalf[:, 0:8], op=ALU.pow))(),
                    reads=[st_r, nhalf_r], writes=[st_r])
                for hh in range(NH):
                    P.add("dve", (lambda hh, pt=pt, so=so, c=c: lambda E: E.tensor_scalar(
                        out=ks12[:, hh, c * 64:(c + 1) * 64], in0=pt[:, hh * 64:(hh + 1) * 64],
                        scalar1=st[:, 24 + so + hh:25 + so + hh], scalar2=None, op0=ALU.mult))(hh),
                        reads=[pr, st_r], writes=[ks12_r])

            def trq(E):
                ins = None
                for hh in range(NH):
                    ins = E.transpose(out=ptr[:, hh * 128:(hh + 1) * 128], in_=ks12[:, hh, :], identity=ident[:])
                return ins
            P.add("pe", trq, reads=[ks12_r, ident_r], writes=[ptr_r])
            P.add("act", lambda E: E.activation(out=qT12[0:64, :, 0, :],
                                                in_=ptr[0:64, :].rearrange("p (k t) -> p k t", k=NH),
                                                func=AF.Copy, scale=gk[0:64, 0:1]),
                  reads=[ptr_r, gk_r], writes=[qT12_r])
            P.add("act", lambda E: E.activation(out=qT12[64:128, :, 1, :],
                                                in_=ptr[64:128, :].rearrange("p (k t) -> p k t", k=NH),
                                                func=AF.Copy, scale=gk[64:128, 0:1]),
                  reads=[ptr_r, gk_r], writes=[qT12_r])
            steps = [(hp, list(range(j0, min(j0 + 2, jmax + 1)))) for hp in range(4) for j0 in range(0, jmax + 1, 2)]
            pstate = {"pi": 0}
            Lbuf = [(big[0], [pp_r[0], pp_r[1]]), (big[1], [psc_r[0], psc_r[1]])]

            def emit_qk(k):
                hp, js = steps[k]
                pt, prs = Lbuf[k % 2]
                prs = prs[0:len(js)]

                def mm(E, pt=pt, js=js, hp=hp):
                    ins = None
                    for q, j in enumerate(js):
                        near = (j - i) >= -1
                        for hh in range(2):
                            h_ = hp * 2 + hh
                            c0 = q * 512 + hh * 256
                            ins = E.matmul(pt[:, c0:c0 + 256].rearrange("p (b t) -> p b t", b=2),
                                           kT12[:, h_, j * 128:(j + 1) * 128], qT12[:, h_, :, :],
                                           start=(hh == 0), stop=not near, skip_group_check=True)
                        if near:
                            for hh in range(2):
                                h_ = hp * 2 + hh
                                for br in range(2):
                                    o_ = q * 512 + (hh * 2 + br) * 128
                                    ins = E.matmul(pt[:, o_:o_ + 128], ident[:], addm[:, j - i + 1, h_, :],
                                                   start=False, stop=True, skip_group_check=True)
                    return ins
                P.add("pe", mm, reads=[kT12_r[j] for j in js] + [qT12_r, ident_r, addm_ready], writes=prs)
                return pt, prs

            def emit_rest(k, pt, prs):
                hp, js = steps[k]
                j = js[-1]
                w_ = 512 * len(js)
                pb, pbr = pT[pstate["pi"] % 3], pT_r[pstate["pi"] % 3]
                pstate["pi"] += 1
                P.add("act", (lambda pt=pt, pb=pb, w_=w_: lambda E: E.activation(
                    out=pb[:, 0:w_], in_=pt[:, 0:w_], func=AF.Exp))(), reads=prs, writes=[pbr])

                def pv(E, pb=pb, js=js, hp=hp):
                    ins = None
                    for q_, jj in enumerate(js):
                        for q in range(4):
                            h_ = hp * 2 + q // 2
                            ins = E.matmul(po[q // 2][:, (q % 2) * 256:(q % 2) * 256 + 129],
                                           pb[:, q_ * 512 + q * 128:q_ * 512 + (q + 1) * 128], vb[:, jj, h_, :],
                                           start=(jj == 0 and q % 2 == 0), stop=(jj == jmax), skip_group_check=True)
                    return ins
                P.add("pe", pv, reads=[pbr] + [vb_r[jj] for jj in js], writes=po_r)
                if j != jmax:
                    return
                for hh in range(2):
                    h_ = hp * 2 + hh
                    P.add("dve", (lambda hh=hh: lambda E: E.reciprocal(
                        out=rz[:, hh * 2:hh * 2 + 2], in_=po[hh][:].rearrange("p (k c) -> p k c", k=2)[:, :, 128]))(),
                        reads=[po_r[hh]], writes=[rz_r])
                    P.add("dve", (lambda hh=hh: lambda E: E.tensor_scalar(
                        out=rz[:, hh * 2 + 1:hh * 2 + 2], in0=rz[:, hh * 2 + 1:hh * 2 + 2], scalar1=lamt[:, 5:6],
                        scalar2=None, op0=ALU.mult))(), reads=[rz_r, lam_r], writes=[rz_r])
                    ot, otr = otmp[hh], otmp_r[hh]
                    P.add("dve", (lambda hh=hh, ot=ot: lambda E: E.tensor_scalar(
                        out=ot[:], in0=po[hh][:, 0:128], scalar1=rz[:, hh * 2:hh * 2 + 1], scalar2=None, op0=ALU.mult))(),
                        reads=[po_r[hh], rz_r], writes=[otr])
                    P.add("dve", (lambda hh=hh, ot=ot: lambda E: E.scalar_tensor_tensor(
                        out=ot[:], in0=po[hh][:, 256:384], scalar=rz[:, hh * 2 + 1:hh * 2 + 2], in1=ot[:],
                        op0=ALU.mult, op1=ALU.add))(), reads=[po_r[hh], rz_r, otr], writes=[otr])
                    P.add("act", (lambda h_=h_, ot=ot: lambda E: E.activation(
                        out=scr[:, 0:128], in_=ot[:], func=AF.Square, accum_out=ost[:, h_:h_ + 1]))(),
                        reads=[otr], writes=[scr_r, ost_r])
                    P.add("dve", (lambda h_=h_: lambda E: E.tensor_scalar(
                        out=ost[:, 8 + h_:9 + h_], in0=ost[:, h_:h_ + 1], scalar1=1.0 / 128, scalar2=EPS,
                        op0=ALU.mult, op1=ALU.add))(), reads=[ost_r], writes=[ost_r])
                    P.add("pool", (lambda h_=h_: lambda E: E.tensor_tensor(
                        out=ost[:, 24 + h_:25 + h_], in0=ost[:, 8 + h_:9 + h_], in1=nhalf[:, 0:1], op=ALU.pow))(),
                        reads=[ost_r, nhalf_r], writes=[ost_r])
                    P.add("dve", (lambda h_=h_, ot=ot: lambda E: E.tensor_scalar(
                        out=otok[:, h_ * 128:(h_ + 1) * 128], in0=ot[:], scalar1=ost[:, 24 + h_:25 + h_], scalar2=None,
                        op0=ALU.mult))(), reads=[otr, ost_r], writes=[otok_r])

            cur = emit_qk(0)
            for k in range(len(steps)):
                nxt = emit_qk(k + 1) if k + 1 < len(steps) else None
                emit_rest(k, *cur)
                cur = nxt

            def tro(E):
                ins = None
                for kc in range(KC):
                    ins = E.transpose(out=ptr[:, kc * 128:(kc + 1) * 128], in_=otok[:, kc * 128:(kc + 1) * 128],
                                      identity=ident[:])
                return ins
            P.add("pe", tro, reads=[otok_r, ident_r], writes=[ptr_r])
            P.add("act", lambda E: E.activation(out=oT[:], in_=ptr[:].rearrange("p (k t) -> p k t", k=KC),
                                                func=AF.Copy, scale=gk[:, 2:3]),
                  reads=[ptr_r, gk_r], writes=[oT_r])
            for nh_ in range(2):
                def mmo(E, nh_=nh_):
                    ins = None
                    for kc in range(KC):
                        ins = E.matmul(py[:], oT[:, kc, :], w_o[:, kc, nh_ * 512:(nh_ + 1) * 512],
                                       start=(kc == 0), stop=(kc == KC - 1))
                    return ins
                P.add("pe", mmo, reads=[oT_r, w_o_r], writes=[py_r])
                P.add("dve", (lambda nh_=nh_, i=i: lambda E: E.tensor_tensor(
                    out=h[:, i, nh_ * 512:(nh_ + 1) * 512], in0=py[:], in1=h[:, i, nh_ * 512:(nh_ + 1) * 512],
                    op=ALU.add))(), reads=[py_r, h_res[i]], writes=[h_res[i]])
        for i_ in range(dbg_tiles):
            b_tile(i_)
        P.barrier()
        sc.close()

    stage = dbg_stage if dbg_stage is not None else 99
    import os
    skip01 = os.environ.get("KSKIP01") == "1"
    if not skip01:
        layer0_attention()
    if stage >= 2 and not skip01:
        conv_ffn(0)
    if stage >= 3:
        layer1_attention()
    if stage >= 4:
        conv_ffn(1)

    out_r = Res("out")
    P.add("sp", lambda E: E.dma_start(out=out_d[0:112, :], in_=h[16:128, 0, :]), reads=[h_res[0]], writes=[out_r], dma=1)
    for i in range(1, 16):
        P.add("sp", (lambda i: lambda E: E.dma_start(out=out_d[128 * i - 16:128 * i + 112, :], in_=h[:, i, :]))(i),
              reads=[h_res[i]], writes=[Res()], dma=1)
    P.add("sp", lambda E: E.dma_start(out=out_d[2032:2048, :], in_=h[0:16, 16, :]), reads=[h_res[16]], writes=[Res()], dma=1)
    P.barrier()
    if dbg_stage is not None:
        print("n_ops", len(P.ops))
    P.emit(es)
    es.close()
    return nc


def _host_inputs(inputs):
    f = lambda a: np.ascontiguousarray(np.asarray(a, dtype=np.float32))
    idx, negvis = _static_tables()
    rel = f(inputs["rel_table"])
    table_ext = np.concatenate([rel, np.full((1, NH), NEG, np.float32)], axis=0)
    am = table_ext[idx]
    am = np.ascontiguousarray(am.transpose(1, 0, 3, 2)).reshape(128, 3 * NH * 128)
    gnorm = np.stack([f(inputs["ln_attn_g"])[0], f(inputs["ln_ffn_g"])[0], f(inputs["kv_norm_g"]),
                      f(inputs["ln_attn_g"])[1], f(inputs["ln_ffn_g"])[1]], axis=0)
    vecs = np.zeros((8, 128), np.float32)
    vecs[0] = f(inputs["qn_a"])[0]
    vecs[1] = f(inputs["kn_a"])[0]
    vecs[2] = np.concatenate([f(inputs["qn_b"])[0]] * 2)
    vecs[3] = np.concatenate([f(inputs["kn_b"])] * 2)
    vecs[4] = f(inputs["subln_b"])[0]
    lamv = np.stack([f(inputs["lam_q1"])[0], f(inputs["lam_k1"])[0], f(inputs["lam_q2"])[0], f(inputs["lam_k2"])[0]], 0)
    cw = f(inputs["conv_w"])
    convwT = np.ascontiguousarray(cw.reshape(2, 3, 2 * NFC, 128).transpose(0, 3, 2, 1)).reshape(2, 128, 2 * NFC * 3)
    cbias = f(inputs["conv_b"])
    convbT = np.ascontiguousarray(cbias.reshape(2, 2 * NFC, 128).transpose(0, 2, 1))
    shared = {
        "meta_tokens": f(inputs["meta_tokens"]),
        "gnorm": gnorm,
        "w_in_a": f(inputs["w_in_a"])[0],
        "w_o_a": f(inputs["w_o_a"])[0],
        "w_kv_b": f(inputs["w_kv_b"]),
        "w_q_b": f(inputs["w_q_b"])[0],
        "w_o_b": f(inputs["w_o_b"])[0],
        "w_up": f(inputs["w_up"]),
        "w_down": f(inputs["w_down"]),
        "conv_wT": convwT,
        "conv_bT": convbT,
        "vecs": vecs,
        "lamv": lamv,
        "addmask": am,
        "cfar": np.ascontiguousarray(rel[15:16, :]),
        "negvis": negvis,
        "ident": np.eye(128, dtype=np.float32),
    }
    return shared


def kernel(**inputs):
    shared = _host_inputs(inputs)
    x = np.asarray(inputs["x"], dtype=np.float32)
    nc = build_nc()
    in_maps = []
    for b in range(8):
        m = dict(shared)
        m["x"] = np.ascontiguousarray(x[b])
        in_maps.append(m)
    res = run_bass_kernel_spmd(nc, in_maps, core_ids=list(range(8)))
    out = np.stack([np.asarray(r["out"], dtype=np.float32) for r in res.results], axis=0)
    return out
```

```python
import math
import contextlib
import numpy as np
import concourse.bass as bass
import concourse.mybir as mybir
from concourse.bass_utils import run_bass_kernel_spmd

F32 = mybir.dt.float32
BF16 = mybir.dt.bfloat16
AF = mybir.ActivationFunctionType
ALU = mybir.AluOpType
AX = mybir.AxisListType

D = 1024
S = 2048
NMETA = 16
T = S + NMETA
NT = 17
TP = NT * 128
KC = 8
NH = 8
A_IN = 2120
DFF = 2816
NFC = DFF // 128
EPS = 1e-6
NEG = -30000.0
NEGBIG = -1.0e30
TOPK = 256


class Res:
    __slots__ = ("name", "writers", "readers", "excl")

    def __init__(self, name="", excl=False):
        self.name = name
        self.writers = {}
        self.readers = {}
        self.excl = excl


class Op:
    __slots__ = ("eng", "fn", "dma", "deps", "signal", "sem", "idx")


class Prog:
    ENGS = ("pe", "act", "dve", "pool", "sp")
    NDSEM = 8

    def __init__(self, nc):
        self.nc = nc
        self.ops = []
        self.pending = {e: [] for e in self.ENGS}
        self.last_op = {e: None for e in self.ENGS}
        self.open_dma = []
        import os
        self.cut = int(os.environ.get("KCUT", "100000000"))

    def add(self, eng, fn, reads=(), writes=(), dma=0):
        if len(self.ops) >= self.cut:
            return None
        op = Op()
        op.eng = eng
        op.fn = fn
        op.dma = dma
        op.signal = bool(dma)
        op.sem = None
        op.idx = len(self.ops)
        key = ("dma", op.idx) if dma else eng
        deps = set(self.pending[eng])
        self.pending[eng] = []
        for r in reads:
            for k, w in r.writers.items():
                if k == key and eng == "pe":
                    continue
                deps.add(w)
            if r.excl:
                for k, w in r.readers.items():
                    if k != key:
                        deps.add(w)
        for r in writes:
            for k, w in r.writers.items():
                if k == key:
                    continue
                deps.add(w)
            for k, w in r.readers.items():
                if k == key:
                    continue
                deps.add(w)
        for r in reads:
            r.readers[key] = op.idx
        for r in writes:
            r.writers = {key: op.idx}
            r.readers = {}
        op.deps = sorted(deps)
        for d in op.deps:
            self.ops[d].signal = True
        self.ops.append(op)
        if dma:
            self.open_dma.append(op.idx)
        else:
            self.last_op[eng] = op.idx
        return op

    def barrier(self):
        deps = [v for v in self.last_op.values() if v is not None] + list(self.open_dma)
        for d in deps:
            self.ops[d].signal = True
        for e in self.ENGS:
            self.pending[e] = sorted(set(self.pending[e]) | set(deps))
        self.open_dma = []

    def emit(self, es):
        nc = self.nc
        engs = {"pe": nc.tensor, "act": nc.scalar, "dve": nc.vector, "pool": nc.gpsimd, "sp": nc.sync}
        esem = {e: es.enter_context(nc.semaphore("s_" + e)) for e in self.ENGS}
        ecnt = {e: 0 for e in self.ENGS}
        dsem = {e: [es.enter_context(nc.semaphore("d_%s%d" % (e, i))) for i in range(self.NDSEM)]
                for e in ("sp", "pool", "act")}
        dval = {e: [0] * self.NDSEM for e in dsem}
        dcnt = {e: 0 for e in dsem}
        seen = {e: {} for e in self.ENGS}
        semid = {}

        def wait(e, sem, val):
            k = id(sem)
            semid[k] = sem
            if seen[e].get(k, 0) < val:
                engs[e].wait_ge(sem, val)
                seen[e][k] = val

        for op in self.ops:
            e = op.eng
            E = engs[e]
            need = {}
            for d in op.deps:
                sem, val = self.ops[d].sem
                k = id(sem)
                if k not in need or need[k][1] < val:
                    need[k] = (sem, val)
            for sem, val in need.values():
                wait(e, sem, val)
            if op.dma:
                n = dcnt[e]
                dcnt[e] += 1
                slot = n % self.NDSEM
                sem = dsem[e][slot]
                wait(e, sem, dval[e][slot])
                inss = op.fn(E)
                if not isinstance(inss, (list, tuple)):
                    inss = [inss]
                for ins in inss:
                    ins.then_inc(sem, 16)
                dval[e][slot] += 16 * len(inss)
                op.sem = (sem, dval[e][slot])
            else:
                ins = op.fn(E)
                if op.signal:
                    ecnt[e] += 1
                    ins.then_inc(esem[e], 1)
                    op.sem = (esem[e], ecnt[e])
        for d in self.pending["sp"]:
            sem, val = self.ops[d].sem
            wait("sp", sem, val)


def _rel_bucket_np(rel):
    nb = 16
    max_exact = 8
    n = np.abs(rel)
    nf = np.maximum(n, 1).astype(np.float32)
    large = max_exact + (np.log(nf / np.float32(max_exact)) / np.float32(math.log(128 / max_exact))
                         * np.float32(nb - max_exact)).astype(np.int32)
    large = np.minimum(large, nb - 1)
    return np.where(rel > 0, nb, 0) + np.where(n < max_exact, n, large)


def _static_tables():
    s_l = np.arange(128)[:, None]
    t_l = np.arange(128)[None, :]
    lim = np.where(t_l < 16, 16, np.where(t_l < 80, 80, 128))
    idx = np.zeros((3, 128, 128), np.int64)
    for o, off in enumerate((-1, 0, 1)):
        rel = 128 * off + s_l - t_l
        b = _rel_bucket_np(rel)
        if off == -1:
            vis = np.ones((128, 128), bool)
        elif off == 0:
            vis = s_l < lim
        else:
            vis = (t_l >= 80) & (s_l < 16)
        idx[o] = np.where(vis, b, 32)
    tt = np.arange(128)[:, None]
    ss = np.arange(256)[None, :]
    limt = np.where(tt < 16, 16, np.where(tt < 80, 80, 144))
    negvis = np.where(ss < limt, 0.0, NEGBIG).astype(np.float32)
    return idx, negvis


def build_nc(dbg_stage=None, dbg_tiles=NT):
    nc = bass.Bass("TRN2", target_bir_lowering=False)

    def din(name, shape):
        return nc.dram_tensor(name, list(shape), F32, kind="ExternalInput").ap()

    x_d = din("x", (S, D))
    meta_d = din("meta_tokens", (NMETA, D))
    gnorm_d = din("gnorm", (5, D))
    w_in_d = din("w_in_a", (D, A_IN))
    w_o_a_d = din("w_o_a", (D, D))
    w_kv_d = din("w_kv_b", (D, 2048))
    w_q_d = din("w_q_b", (D, D))
    w_o_b_d = din("w_o_b", (D, D))
    w_up_d = din("w_up", (2, D, 2 * DFF))
    w_down_d = din("w_down", (2, DFF, D))
    convw_d = din("conv_wT", (2, 128, 2 * NFC * 3))
    convb_d = din("conv_bT", (2, 128, 2 * NFC))
    vecs_d = din("vecs", (8, 128))
    lam_d = din("lamv", (4, 64))
    addm_d = din("addmask", (128, 3 * NH * 128))
    cfar_d = din("cfar", (1, NH))
    negvis_d = din("negvis", (128, 256))
    ident_d = din("ident", (128, 128))
    out_d = nc.dram_tensor("out", [S, D], F32, kind="ExternalOutput").ap()

    es = contextlib.ExitStack()
    P = Prog(nc)
    lam_init = 0.8 - 0.6 * math.exp(-0.3 * 1)

    def sb(scope, name, shape, dt):
        return scope.enter_context(nc.sbuf_tensor("sb_" + name, list(shape), dt))

    h = sb(es, "h", (128, NT, D), F32)
    h_res = [Res("h%d" % i) for i in range(NT)]
    ident = sb(es, "ident", (128, 128), BF16)
    ident_r = Res("ident")
    gb = sb(es, "gb", (128, D), F32)
    gb_r = Res("gb")
    vecs = sb(es, "vecs", (128, 8), F32)
    vecs_r = Res("vecs")
    addm = sb(es, "addm", (128, 3, NH, 128), BF16)
    addm_r = Res("addm")
    cfar = sb(es, "cfar", (128, NH), F32)
    cfar_r = Res("cfar")
    negvis = sb(es, "negvis", (128, 256), F32)
    negvis_r = Res("negvis")
    small = sb(es, "small", (128, 64), F32)
    nhalf = sb(es, "nhalf", (128, 8), F32)
    nhalf_r = Res("nhalf")
    init_sc = contextlib.ExitStack()
    addm_f = sb(init_sc, "addm_f", (128, 3, NH, 128), F32)
    big = [es.enter_context(nc.psum_tensor("big%d" % i, [128, 1024], F32)) for i in range(2)]
    pp = [big[0][:, 0:512], big[0][:, 512:1024]]
    pp_r = [Res("pp%d" % i, True) for i in range(2)]
    psc = [big[1][:, 0:512], big[1][:, 512:1024]]
    psc_r = [Res("psc%d" % i, True) for i in range(2)]
    po = [es.enter_context(nc.psum_tensor("po%d" % i, [128, 512], F32)) for i in range(2)]
    po_r = [Res("po%d" % i, True) for i in range(2)]
    ptr = es.enter_context(nc.psum_tensor("ptr", [128, 1024], BF16))
    ptr_r = Res("ptr", True)
    py = es.enter_context(nc.psum_tensor("py", [128, 512], F32))
    py_r = Res("py", True)

    cnt = {"pp": 0, "psc": 0}

    def next_pp():
        i = cnt["pp"] % 2
        cnt["pp"] += 1
        return pp[i], pp_r[i]

    def next_psc():
        i = cnt["psc"] % 2
        cnt["psc"] += 1
        return psc[i], psc_r[i]

    P.add("sp", lambda E: [
        E.dma_start(out=h[0:16, 0, :], in_=meta_d),
        E.dma_start(out=h[16:128, 0, :], in_=x_d[0:112, :]),
    ], writes=[h_res[0]], dma=1)
    for i in range(1, 16):
        P.add("sp", (lambda i: lambda E: E.dma_start(out=h[:, i, :], in_=x_d[128 * i - 16:128 * i + 112, :]))(i),
              writes=[h_res[i]], dma=1)
    P.add("dve", lambda E: E.memset(h[:, 16, :], 0.0), writes=[h_res[16]])
    P.add("pool", lambda E: E.memset(nhalf[:], -0.5), writes=[nhalf_r])
    P.add("sp", lambda E: E.dma_start(out=h[0:16, 16, :], in_=x_d[2032:2048, :]), writes=[h_res[16]], dma=1)
    P.add("pool", lambda E: E.dma_start(out=ident[:], in_=ident_d), writes=[ident_r], dma=1)
    P.add("sp", lambda E: E.dma_start(out=negvis[:], in_=negvis_d), writes=[negvis_r], dma=1)
    P.add("sp", lambda E: E.dma_start(out=addm_f[:].rearrange("p a h t -> p (a h t)"), in_=addm_d),
          writes=[addm_r], dma=1)
    P.add("sp", lambda E: E.dma_start(out=cfar[:], in_=cfar_d.partition_broadcast(128)), writes=[cfar_r], dma=1)
    for hh in range(NH):
        P.add("dve", (lambda hh: lambda E: E.tensor_scalar(
            out=addm[:, :, hh, :], in0=addm_f[:, :, hh, :], scalar1=cfar[:, hh:hh + 1], scalar2=None,
            op0=ALU.subtract))(hh), reads=[addm_r, cfar_r], writes=[Res()])
    addm_ready = Res("addm_ready")
    P.add("dve", lambda E: E.memset(small[:, 0:1], 0.0), reads=[], writes=[addm_ready])
    P.barrier()
    init_sc.close()

    def load_gain(idx):
        P.add("sp", lambda E: E.dma_start(out=gb[:], in_=gnorm_d[idx:idx + 1, :].partition_broadcast(128)),
              writes=[gb_r], dma=1)

    def rms_to_xnT(i, xn_tok, xn_tok_r, dst_fn, dst_res, scr, scr_r, st, st_r):
        P.add("act", lambda E: E.activation(out=scr[:], in_=h[:, i, :], func=AF.Square, accum_out=st[:, 0:1]),
              reads=[h_res[i]], writes=[scr_r, st_r])
        P.add("dve", lambda E: E.tensor_scalar(out=st[:, 1:2], in0=st[:, 0:1], scalar1=1.0 / D, scalar2=EPS,
                                               op0=ALU.mult, op1=ALU.add), reads=[st_r], writes=[st_r])
        P.add("pool", lambda E: E.tensor_tensor(out=st[:, 3:4], in0=st[:, 1:2], in1=nhalf[:, 0:1], op=ALU.pow),
              reads=[st_r, nhalf_r], writes=[st_r])
        P.add("dve", lambda E: E.scalar_tensor_tensor(out=xn_tok[:], in0=h[:, i, :], scalar=st[:, 3:4], in1=gb[:],
                                                      op0=ALU.mult, op1=ALU.mult),
              reads=[h_res[i], st_r, gb_r], writes=[xn_tok_r])

        def tr(E):
            ins = None
            for kc in range(KC):
                ins = E.transpose(out=ptr[:, kc * 128:(kc + 1) * 128], in_=xn_tok[:, kc * 128:(kc + 1) * 128],
                                  identity=ident[:])
            return ins
        P.add("pe", tr, reads=[xn_tok_r, ident_r], writes=[ptr_r])
        P.add("act", lambda E: E.copy(out=dst_fn(), in_=ptr[:].rearrange("p (k t) -> p k t", k=KC)),
              reads=[ptr_r], writes=[dst_res])

    def layer0_attention():
        sc = contextlib.ExitStack()
        w_in = sb(sc, "w_in", (128, KC, A_IN), BF16)
        w_in_r = [Res("w_in%d" % k) for k in range(KC)]
        w_o = sb(sc, "w_o", (128, KC, D), BF16)
        w_o_r = Res("w_o")
        kT = sb(sc, "kT", (128, 2, TP), BF16)
        kT_r = [Res("kT%d" % i) for i in range(NT)]
        vaug = sb(sc, "vaug", (128, NT, 2, 129), BF16)
        v_r = [Res("v%d" % i) for i in range(NT)]
        kiT = sb(sc, "kiT", (128, TP), BF16)
        kiT_r = [Res("kiT%d" % i) for i in range(NT)]
        NB = 2
        xn_tok = [sb(sc, "xn_tok%d" % b, (128, D), BF16) for b in range(1)] * NB
        xn_tok_r = [Res() for b in range(1)] * NB
        xnT = [sb(sc, "xnT%d" % b, (128, KC, 128), BF16) for b in range(1)] * NB
        xnT_r = [Res() for b in range(1)] * NB
        scr = sb(sc, "scr", (128, D), BF16)
        scr_r = Res("scr")
        st = [sb(sc, "st%d" % b, (128, 64), F32) for b in range(NB)]
        st_r = [Res() for b in range(NB)]
        qs = [sb(sc, "qs%d" % b, (128, NH, 128), BF16) for b in range(1)] * NB
        qs_r = [Res() for b in range(1)] * NB
        ks = [sb(sc, "ks%d" % b, (128, 2, 128), BF16) for b in range(1)] * NB
        ks_r = [Res() for b in range(1)] * NB
        qT = [sb(sc, "qT%d" % b, (128, NH, 128), BF16) for b in range(NB)]
        qT_r = [Res() for b in range(NB)]
        qis = [sb(sc, "qis%d" % b, (128, NH * 64), BF16) for b in range(1)] * NB
        qis_r = [Res() for b in range(1)] * NB
        qiT = [sb(sc, "qiT%d" % b, (128, 4, 128), BF16) for b in range(NB)]
        qiT_r = [Res() for b in range(NB)]
        kis = [sb(sc, "kis%d" % b, (128, 128), BF16) for b in range(1)] * NB
        kis_r = [Res() for b in range(1)] * NB
        wst = [sb(sc, "wst%d" % b, (128, 32), F32) for b in range(NB)]
        wst_r = [Res() for b in range(NB)]
        acc = sb(sc, "acc", (128, TP), F32)
        acc_r = Res("acc")
        work = sb(sc, "work", (128, TP), F32)
        work_r = Res("work")
        rbuf = [sb(sc, "rbuf%d" % b, (128, 512), F32) for b in range(2)]
        rbuf_r = [Res() for b in range(2)]
        m8 = sb(sc, "m8", (128, 8), F32)
        m8_r = Res("m8")
        tb = sb(sc, "tb", (128, 32), F32)
        tb_r = Res("tb")
        iota8 = sb(sc, "iota8", (128, 8), F32)
        iota_r = Res("iota8")
        for q8 in range(8):
            P.add("pool", (lambda q8=q8: lambda E: E.memset(iota8[:, q8:q8 + 1], float(q8)))(), writes=[iota_r])
        sel = work[:].bitcast(BF16)
        sel_r = work_r
        selT = sb(sc, "selT", (128, NT, 128), BF16)
        selT_r = Res("selT")
        pT = [sb(sc, "pT%d" % b, (128, 512), BF16) for b in range(3)]
        pT_r = [Res() for b in range(3)]
        otok = sb(sc, "otok", (128, D), BF16)
        otok_r = Res("otok")
        oT = sb(sc, "oT", (128, KC, 128), BF16)
        oT_r = Res("oT")
        rz = sb(sc, "rz", (128, 8), F32)
        rz_r = Res("rz")
        gq = sb(sc, "gq", (128, 2), F32)
        gq_r = Res("gq")

        for kc in range(KC):
            P.add("pool", (lambda kc: lambda E: E.dma_start(out=w_in[:, kc, :], in_=w_in_d[kc * 128:(kc + 1) * 128, :]))(kc),
                  writes=[w_in_r[kc]], dma=1)
        P.add("pool", lambda E: [E.dma_start(out=w_o[:, kc, :], in_=w_o_a_d[kc * 128:(kc + 1) * 128, :]) for kc in range(KC)],
              writes=[w_o_r], dma=1)
        load_gain(0)
        P.add("sp", lambda E: E.dma_start(out=gq[:], in_=vecs_d[0:2, :].rearrange("v p -> p v"),
                                          allow_slow_non_contiguous=True), writes=[gq_r], dma=1)
        P.add("dve", lambda E: E.tensor_scalar(out=gq[:, 0:1], in0=gq[:, 0:1], scalar1=128.0 ** -0.5, scalar2=None,
                                               op0=ALU.mult), reads=[gq_r], writes=[gq_r])
        P.add("pool", lambda E: E.memset(vaug[:, :, :, 128:129], 1.0), writes=v_r)

        def proj_tile(i):
            b = i % NB
            rms_to_xnT(i, xn_tok[b], xn_tok_r[b], lambda: xnT[b][:], xnT_r[b], scr, scr_r, st[b], st_r[b])
            for c, (c0, cw) in enumerate(((0, 512), (512, 512), (1024, 512), (1536, 512), (2048, 72))):
                pt, pr = next_pp()

                def mm(E, c0=c0, cw=cw, pt=pt):
                    ins = None
                    for kc in range(KC):
                        ins = E.matmul(pt[:, 0:cw], xnT[b][:, kc, :], w_in[:, kc, c0:c0 + cw],
                                       start=(kc == 0), stop=(kc == KC - 1))
                    return ins
                P.add("pe", mm, reads=[xnT_r[b]] + w_in_r, writes=[pr])
                if c in (0, 1, 2):
                    nh = 4 if c < 2 else 2
                    so = 8 + c * 4
                    for hh in range(nh):
                        P.add("act", (lambda hh, pt=pt, so=so: lambda E: E.activation(
                            out=scr[:, hh * 128:(hh + 1) * 128], in_=pt[:, hh * 128:(hh + 1) * 128], func=AF.Square,
                            accum_out=st[b][:, so + hh:so + hh + 1]))(hh),
                            reads=[pr], writes=[scr_r, st_r[b]])
                    P.add("dve", (lambda so=so, nh=nh: lambda E: E.tensor_scalar(
                        out=st[b][:, 24 + so:24 + so + nh], in0=st[b][:, so:so + nh], scalar1=1.0 / 128, scalar2=EPS,
                        op0=ALU.mult, op1=ALU.add))(), reads=[st_r[b]], writes=[st_r[b]])
                    P.add("pool", (lambda so=so, nh=nh: lambda E: E.tensor_tensor(
                        out=st[b][:, 24 + so:24 + so + nh], in0=st[b][:, 24 + so:24 + so + nh], in1=nhalf[:, 0:nh],
                        op=ALU.pow))(), reads=[st_r[b], nhalf_r], writes=[st_r[b]])
                    for hh in range(nh):
                        if c < 2:
                            dst = qs[b][:, c * 4 + hh, :]
                            dr = qs_r[b]
                        else:
                            dst = ks[b][:, hh, :]
                            dr = ks_r[b]
                        P.add("dve", (lambda hh, dst=dst, pt=pt, so=so: lambda E: E.tensor_scalar(
                            out=dst, in0=pt[:, hh * 128:(hh + 1) * 128], scalar1=st[b][:, 24 + so + hh:24 + so + hh + 1],
                            scalar2=None, op0=ALU.mult))(hh), reads=[pr, st_r[b]], writes=[dr])
                    if c == 2:
                        P.add("act", (lambda pt=pt: lambda E: E.copy(
                            out=vaug[:, i, :, 0:128], in_=pt[:, 256:512].rearrange("p (g d) -> p g d", g=2)))(),
                            reads=[pr], writes=[v_r[i]])
                elif c == 3:
                    pass
                    qi_pt, qi_pr = pt, pr
                else:
                    P.add("dve", (lambda pt=pt: lambda E: E.tensor_scalar(
                        out=wst[b][:, 0:8], in0=pt[:, 64:72], scalar1=0.0, scalar2=2.0, op0=ALU.is_gt, op1=ALU.mult))(),
                        reads=[pr], writes=[wst_r[b]])
                    P.add("dve", lambda E: E.tensor_scalar(
                        out=wst[b][:, 0:8], in0=wst[b][:, 0:8], scalar1=-1.0, scalar2=None, op0=ALU.add),
                        reads=[wst_r[b]], writes=[wst_r[b]])
                    P.add("dve", (lambda pt=pt: lambda E: E.scalar_tensor_tensor(
                        out=wst[b][:, 8:16], in0=pt[:, 64:72], scalar=(8.0 ** -0.5) * (64.0 ** -0.5), in1=wst[b][:, 0:8],
                        op0=ALU.mult, op1=ALU.mult))(), reads=[pr, wst_r[b]], writes=[wst_r[b]])
                    P.add("act", (lambda pt=pt: lambda E: E.copy(out=kis[b][:, 0:64], in_=pt[:, 0:64]))(),
                          reads=[pr], writes=[kis_r[b]])
                    P.add("act", (lambda pt=pt: lambda E: E.copy(out=kis[b][:, 64:128], in_=pt[:, 0:64]))(),
                          reads=[pr], writes=[kis_r[b]])
                    for hh in range(NH):
                        P.add("dve", (lambda hh, qi_pt=qi_pt: lambda E: E.tensor_scalar(
                            out=qis[b][:, hh * 64:(hh + 1) * 64], in0=qi_pt[:, hh * 64:(hh + 1) * 64],
                            scalar1=wst[b][:, 8 + hh:9 + hh], scalar2=None, op0=ALU.mult))(hh),
                            reads=[qi_pr, wst_r[b]], writes=[qis_r[b]])
            def trq(E):
                ins = None
                for hh in range(NH):
                    ins = E.transpose(out=ptr[:, hh * 128:(hh + 1) * 128], in_=qs[b][:, hh, :], identity=ident[:])
                return ins
            P.add("pe", trq, reads=[qs_r[b], ident_r], writes=[ptr_r])
            P.add("act", lambda E: E.activation(out=qT[b][:], in_=ptr[:].rearrange("p (k t) -> p k t", k=NH),
                                                func=AF.Copy, scale=gq[:, 0:1]),
                  reads=[ptr_r, gq_r], writes=[qT_r[b]])

            def trk(E):
                ins = None
                for g in range(2):
                    ins = E.transpose(out=ptr[:, g * 128:(g + 1) * 128], in_=ks[b][:, g, :], identity=ident[:])
                for hp in range(4):
                    ins = E.transpose(out=ptr[:, 256 + hp * 128:256 + (hp + 1) * 128],
                                      in_=qis[b][:, hp * 128:(hp + 1) * 128], identity=ident[:])
                ins = E.transpose(out=ptr[:, 768:896], in_=kis[b][:], identity=ident[:])
                return ins
            P.add("pe", trk, reads=[ks_r[b], qis_r[b], kis_r[b], ident_r], writes=[ptr_r])
            P.add("act", lambda E: E.activation(out=kT[:, :, i * 128:(i + 1) * 128],
                                                in_=ptr[:, 0:256].rearrange("p (g t) -> p g t", g=2),
                                                func=AF.Copy, scale=gq[:, 1:2]),
                  reads=[ptr_r, gq_r], writes=[kT_r[i]])
            P.add("dve", lambda E: E.tensor_copy(out=qiT[b][:], in_=ptr[:, 256:768].rearrange("p (k t) -> p k t", k=4)),
                  reads=[ptr_r], writes=[qiT_r[b]])
            P.add("dve", lambda E: E.tensor_copy(out=kiT[:, i * 128:(i + 1) * 128], in_=ptr[:, 768:896]),
                  reads=[ptr_r], writes=[kiT_r[i]])

        def attn_tile(i):
            b = i % NB
            jmax = min(i + 1, NT - 1)
            nk = 128 * (jmax + 1)
            if i > 0:
                P.add("pool", lambda E: E.memset(acc[:, 0:128 * i], 0.0), writes=[acc_r])
            wv = nk - 128 * i
            P.add("pool", lambda E: E.tensor_copy(out=acc[:, 128 * i:nk], in_=negvis[:, 0:wv]),
                  reads=[negvis_r], writes=[acc_r])
            nchunk = (nk + 511) // 512
            ri = 0
            for hh in range(NH):
                for cch in range(nchunk):
                    c0 = cch * 512
                    cw = min(512, nk - c0)
                    pt, pr = next_psc()
                    po_ = (hh % 2) * 64
                    P.add("pe", (lambda pt=pt, c0=c0, cw=cw, hh=hh, po_=po_: lambda E: E.matmul(
                        pt[:, 0:cw], qiT[b][po_:po_ + 64, hh // 2, :], kiT[po_:po_ + 64, c0:c0 + cw],
                        start=True, stop=True))(),
                        reads=[qiT_r[b]] + kiT_r[0:jmax + 1], writes=[pr])
                    rb, rr = rbuf[ri % 2], rbuf_r[ri % 2]
                    ri += 1
                    P.add("act", (lambda pt=pt, cw=cw, rb=rb: lambda E: E.activation(
                        out=rb[:, 0:cw], in_=pt[:, 0:cw], func=AF.Relu))(), reads=[pr], writes=[rr])
                    P.add("dve", (lambda c0=c0, cw=cw, rb=rb, hh=hh: lambda E: E.scalar_tensor_tensor(
                        out=acc[:, c0:c0 + cw], in0=rb[:, 0:cw], scalar=wst[b][:, hh:hh + 1], in1=acc[:, c0:c0 + cw],
                        op0=ALU.mult, op1=ALU.add))(), reads=[rr, wst_r[b], acc_r], writes=[acc_r])
            if i < 2:
                src = acc
                src_r = acc_r
                for it in range(TOPK // 8):
                    P.add("dve", (lambda src=src: lambda E: E.max(out=m8[:], in_=src[:, 0:nk]))(),
                          reads=[src_r], writes=[m8_r])
                    if it < TOPK // 8 - 1:
                        P.add("dve", (lambda src=src: lambda E: E.match_replace(
                            out=work[:, 0:nk], in_to_replace=m8[:], in_values=src[:, 0:nk], imm_value=-3.0e38))(),
                            reads=[src_r, m8_r], writes=[work_r])
                        src = work
                        src_r = work_r
                thr_ap = m8[:, 7:8]
                thr_res = m8_r
            else:
                NB_IT = 12
                P.add("dve", lambda E: E.tensor_reduce(out=tb[:, 0:1], in_=acc[:, 0:nk], axis=AX.X, op=ALU.max),
                      reads=[acc_r], writes=[tb_r])
                P.add("dve", lambda E: E.tensor_reduce(out=tb[:, 1:2], in_=acc[:, 0:128 * i], axis=AX.X, op=ALU.min),
                      reads=[acc_r], writes=[tb_r])
                P.add("dve", lambda E: E.tensor_tensor(out=tb[:, 2:3], in0=tb[:, 0:1], in1=tb[:, 1:2], op=ALU.subtract),
                      reads=[tb_r], writes=[tb_r])
                for it in range(NB_IT):
                    cc = 0.5 ** (it + 1)
                    P.add("dve", (lambda cc=cc: lambda E: E.scalar_tensor_tensor(
                        out=tb[:, 3:4], in0=tb[:, 2:3], scalar=cc, in1=tb[:, 1:2], op0=ALU.mult, op1=ALU.add))(),
                        reads=[tb_r], writes=[tb_r])
                    P.add("dve", lambda E: E.tensor_scalar(
                        out=sel[:, 0:nk], in0=acc[:, 0:nk], scalar1=tb[:, 3:4], scalar2=None, op0=ALU.is_ge,
                        op1=ALU.add, accum_out=tb[:, 4:5]), reads=[acc_r, tb_r], writes=[work_r, tb_r])
                    P.add("dve", (lambda cc=cc: lambda E: E.tensor_scalar(
                        out=tb[:, 5:6], in0=tb[:, 4:5], scalar1=TOPK - 0.5, scalar2=cc, op0=ALU.is_ge, op1=ALU.mult))(),
                        reads=[tb_r], writes=[tb_r])
                    P.add("dve", lambda E: E.scalar_tensor_tensor(
                        out=tb[:, 1:2], in0=tb[:, 2:3], scalar=tb[:, 5:6], in1=tb[:, 1:2], op0=ALU.mult, op1=ALU.add),
                        reads=[tb_r], writes=[tb_r])
                P.add("dve", lambda E: E.scalar_tensor_tensor(
                    out=tb[:, 6:7], in0=tb[:, 2:3], scalar=0.5 ** NB_IT, in1=tb[:, 1:2], op0=ALU.mult, op1=ALU.add),
                    reads=[tb_r], writes=[tb_r])
                P.add("dve", lambda E: E.tensor_scalar(
                    out=sel[:, 0:nk], in0=acc[:, 0:nk], scalar1=tb[:, 6:7], scalar2=None, op0=ALU.is_ge,
                    op1=ALU.add, accum_out=tb[:, 7:8]), reads=[acc_r, tb_r], writes=[work_r, tb_r])
                P.add("dve", lambda E: E.tensor_scalar(
                    out=work[:, 0:nk], in0=acc[:, 0:nk], scalar1=tb[:, 6:7], scalar2=-1.0, op0=ALU.is_lt, op1=ALU.add),
                    reads=[acc_r, tb_r], writes=[work_r])
                P.add("dve", lambda E: E.scalar_tensor_tensor(
                    out=work[:, 0:nk], in0=work[:, 0:nk], scalar=3.0e38, in1=acc[:, 0:nk], op0=ALU.mult, op1=ALU.add),
                    reads=[acc_r, work_r], writes=[work_r])
                P.add("dve", lambda E: E.max(out=m8[:], in_=work[:, 0:nk]), reads=[work_r], writes=[m8_r])
                P.add("dve", lambda E: E.tensor_scalar(
                    out=tb[:, 8:9], in0=tb[:, 7:8], scalar1=-1.0, scalar2=float(TOPK - 1), op0=ALU.mult, op1=ALU.add),
                    reads=[tb_r], writes=[tb_r])
                P.add("dve", lambda E: E.tensor_scalar(
                    out=tb[:, 8:9], in0=tb[:, 8:9], scalar1=0.0, scalar2=7.0, op0=ALU.max, op1=ALU.min),
                    reads=[tb_r], writes=[tb_r])
                P.add("dve", lambda E: E.tensor_scalar(
                    out=tb[:, 16:24], in0=iota8[:], scalar1=tb[:, 8:9], scalar2=None, op0=ALU.is_equal),
                    reads=[tb_r, iota_r], writes=[tb_r])
                P.add("dve", lambda E: E.tensor_tensor(out=tb[:, 16:24], in0=tb[:, 16:24], in1=m8[:], op=ALU.mult),
                      reads=[tb_r, m8_r], writes=[tb_r])
                P.add("dve", lambda E: E.tensor_reduce(out=tb[:, 9:10], in_=tb[:, 16:24], axis=AX.X, op=ALU.add),
                      reads=[tb_r], writes=[tb_r])
                thr_ap = tb[:, 9:10]
                thr_res = tb_r
            P.add("dve", (lambda thr_ap=thr_ap: lambda E: E.tensor_scalar(
                out=sel[:, 0:nk], in0=acc[:, 0:nk], scalar1=thr_ap, scalar2=None, op0=ALU.is_ge))(),
                reads=[acc_r, thr_res], writes=[sel_r])
            for j0 in range(0, jmax + 1, 8):
                j1 = min(jmax + 1, j0 + 8)

                def trs(E, j0=j0, j1=j1):
                    ins = None
                    for j in range(j0, j1):
                        ins = E.transpose(out=ptr[:, (j - j0) * 128:(j - j0 + 1) * 128], in_=sel[:, j * 128:(j + 1) * 128],
                                          identity=ident[:])
                    return ins
                P.add("pe", trs, reads=[sel_r, ident_r], writes=[ptr_r])
                P.add("act", (lambda j0=j0, j1=j1: lambda E: E.copy(
                    out=selT[:, j0:j1, :], in_=ptr[:, 0:(j1 - j0) * 128].rearrange("p (k t) -> p k t", k=j1 - j0)))(),
                    reads=[ptr_r], writes=[selT_r])
            steps = [(g, j) for g in range(2) for j in range(jmax + 1)]
            pstate = {"pi": 0}

            def emit_qk(k):
                g, j = steps[k]
                pt, pr = next_psc()
                off = j - i
                near = off >= -1

                def mm(E, pt=pt, j=j, g=g, off=off, near=near):
                    ins = E.matmul(pt[:].rearrange("p (k t) -> p k t", k=4), kT[:, g, j * 128:(j + 1) * 128],
                                   qT[b][:, g * 4:(g + 1) * 4, :], start=True, stop=not near)
                    if near:
                        ins = E.matmul(pt[:].rearrange("p (k t) -> p k t", k=4), ident[:],
                                       addm[:, off + 1, g * 4:(g + 1) * 4, :], start=False, stop=True)
                    return ins
                P.add("pe", mm, reads=[kT_r[j], qT_r[b], ident_r, addm_ready], writes=[pr])
                return pt, pr

            def emit_rest(k, pt, pr):
                g, j = steps[k]
                pb, pbr = pT[pstate["pi"] % 3], pT_r[pstate["pi"] % 3]
                pstate["pi"] += 1
                P.add("act", (lambda pt=pt, pb=pb: lambda E: E.activation(out=pb[:], in_=pt[:], func=AF.Exp))(),
                      reads=[pr], writes=[pbr])
                P.add("dve", (lambda pb=pb, j=j: lambda E: E.tensor_tensor(
                    out=pb[:].rearrange("p (k t) -> p k t", k=4), in0=pb[:].rearrange("p (k t) -> p k t", k=4),
                    in1=selT[:, j:j + 1, :].to_broadcast([128, 4, 128]), op=ALU.mult))(),
                    reads=[pbr, selT_r], writes=[pbr])

                def pv(E, pb=pb, j=j, g=g):
                    ins = None
                    for hh in range(4):
                        ins = E.matmul(po[hh // 2][:, (hh % 2) * 256:(hh % 2) * 256 + 129],
                                       pb[:, hh * 128:(hh + 1) * 128], vaug[:, j, g, :],
                                       start=(j == 0 and hh % 2 == 0), stop=(j == jmax),
                                       skip_group_check=True)
                    return ins
                P.add("pe", pv, reads=[pbr, v_r[j]], writes=po_r)
                if j == jmax:
                    for hb in range(2):
                        P.add("dve", (lambda hb=hb, g=g: lambda E: E.reciprocal(
                            out=rz[:, g * 4 + hb * 2:g * 4 + hb * 2 + 2],
                            in_=po[hb][:].rearrange("p (k c) -> p k c", k=2)[:, :, 128]))(),
                            reads=[po_r[hb]], writes=[rz_r])
                    for hh in range(4):
                        P.add("dve", (lambda hh=hh, g=g: lambda E: E.tensor_scalar(
                            out=otok[:, (g * 4 + hh) * 128:(g * 4 + hh + 1) * 128],
                            in0=po[hh // 2][:, (hh % 2) * 256:(hh % 2) * 256 + 128],
                            scalar1=rz[:, g * 4 + hh:g * 4 + hh + 1], scalar2=None, op0=ALU.mult))(),
                            reads=[po_r[hh // 2], rz_r], writes=[otok_r])

            cur = emit_qk(0)
            for k in range(len(steps)):
                nxt = emit_qk(k + 1) if k + 1 < len(steps) else None
                emit_rest(k, *cur)
                cur = nxt
            def tro(E):
                ins = None
                for kc in range(KC):
                    ins = E.transpose(out=ptr[:, kc * 128:(kc + 1) * 128], in_=otok[:, kc * 128:(kc + 1) * 128],
                                      identity=ident[:])
                return ins
            P.add("pe", tro, reads=[otok_r, ident_r], writes=[ptr_r])
            P.add("act", lambda E: E.copy(out=oT[:], in_=ptr[:].rearrange("p (k t) -> p k t", k=KC)),
                  reads=[ptr_r], writes=[oT_r])
            for nh_ in range(2):
                def mmo(E, nh_=nh_):
                    ins = None
                    for kc in range(KC):
                        ins = E.matmul(py[:], oT[:, kc, :], w_o[:, kc, nh_ * 512:(nh_ + 1) * 512],
                                       start=(kc == 0), stop=(kc == KC - 1))
                    return ins
                P.add("pe", mmo, reads=[oT_r, w_o_r], writes=[py_r])
                P.add("dve", (lambda nh_=nh_: lambda E: E.tensor_tensor(
                    out=h[:, i, nh_ * 512:(nh_ + 1) * 512], in0=py[:], in1=h[:, i, nh_ * 512:(nh_ + 1) * 512],
                    op=ALU.add))(), reads=[py_r, h_res[i]], writes=[h_res[i]])

        proj_tile(0)
        for i in range(dbg_tiles):
            if i + 1 < NT:
                proj_tile(i + 1)
            attn_tile(i)
        P.barrier()
        sc.close()

    def conv_ffn(l):
        sc = contextlib.ExitStack()
        sbl = lambda scope, name, shape, dt: sb(scope, "L%d_%s" % (l, name), shape, dt)
        G = 6
        groups = []
        c = 0
        while c < NFC:
            groups.append(list(range(c, min(NFC, c + G))))
            c += G
        xnT = sbl(sc, "f_xnT", (128, KC, TP), BF16)
        xnT_r = [Res() for i in range(NT)]
        xn_tok = sbl(sc, "f_xn_tok", (128, D), BF16)
        xn_tok_r = Res()
        scr = sbl(sc, "f_scr", (128, D), BF16)
        scr_r = Res()
        st = sbl(sc, "f_st", (128, 64), F32)
        st_r = Res()
        NWU = 3
        wup = [sbl(sc, "wup%d" % k, (128, KC, 256), BF16) for k in range(NWU)]
        wup_r = [Res() for k in range(NWU)]
        NGT = G + 2
        gT = [sbl(sc, "gT%d" % k, (128, TP), BF16) for k in range(NGT)]
        gT_r = [Res() for k in range(NGT)]
        wdn = [sbl(sc, "wdn%d" % k, (128, D), BF16) for k in range(NGT)]
        wdn_r = [Res() for k in range(NGT)]
        ub = [sbl(sc, "ub%d" % k, (128, TP + 2), F32) for k in range(2)]
        TCH = [(0, 512), (512, 512), (1024, 512), (1536, 512), (2048, 128)]
        ub_r = [[Res() for t in TCH] for k in range(2)]
        halo_r = [Res() for k in range(2)]
        cb = [[sbl(sc, "cb%d_%d" % (k, q), (128, 512), F32) for q in range(2)] for k in range(2)]
        cb_r = [[Res() for q in range(2)] for k in range(2)]
        cw_sb = sbl(sc, "cw_sb", (128, 2 * NFC * 3), F32)
        cb_sb = sbl(sc, "cb_sb", (128, 2 * NFC), F32)
        cws_r = Res()

        load_gain(1 if l == 0 else 4)
        P.add("sp", lambda E: [E.dma_start(out=cw_sb[:], in_=convw_d[l]), E.dma_start(out=cb_sb[:], in_=convb_d[l])],
              writes=[cws_r], dma=1)
        for k in range(2):
            P.add("pool", (lambda k: lambda E: E.memset(ub[k][:, 0:2], 0.0))(k), writes=[halo_r[k]])
        for i in range(NT):
            rms_to_xnT(i, xn_tok, xn_tok_r, (lambda i=i: xnT[:, :, i * 128:(i + 1) * 128]), xnT_r[i], scr, scr_r, st, st_r)

        slot = 0
        qq = 0
        for grp in groups:
            gslots = []
            for c in grp:
                wu, wur = wup[c % NWU], wup_r[c % NWU]
                sl = slot % NGT
                slot += 1
                gslots.append(sl)
                P.add("pool", (lambda c=c, wu=wu: lambda E: [
                    E.dma_start(out=wu[:, :, 0:128],
                                in_=w_up_d[l, :, c * 128:(c + 1) * 128].rearrange("(kc k) n -> k kc n", k=128)),
                    E.dma_start(out=wu[:, :, 128:256],
                                in_=w_up_d[l, :, DFF + c * 128:DFF + (c + 1) * 128].rearrange("(kc k) n -> k kc n", k=128)),
                ])(), writes=[wur], dma=1)
                P.add("pool", (lambda c=c, sl=sl: lambda E: E.dma_start(
                    out=wdn[sl][:], in_=w_down_d[l, c * 128:(c + 1) * 128, :]))(), writes=[wdn_r[sl]], dma=1)
                for ti, (t0, tw) in enumerate(TCH):
                    for k in range(2):
                        pt, pr = next_pp()

                        def mm(E, pt=pt, k=k, t0=t0, tw=tw, wu=wu):
                            ins = None
                            for kc in range(KC):
                                ins = E.matmul(pt[:, 0:tw], wu[:, kc, k * 128:(k + 1) * 128], xnT[:, kc, t0:t0 + tw],
                                               start=(kc == 0), stop=(kc == KC - 1))
                            return ins
                        P.add("pe", mm, reads=[wur] + xnT_r[t0 // 128:(t0 + tw) // 128], writes=[pr])
                        ci = k * NFC + c
                        cbuf, cbr = cb[k][qq % 2], cb_r[k][qq % 2]
                        P.add("act", (lambda pt=pt, k=k, t0=t0, tw=tw: lambda E: E.copy(
                            out=ub[k][:, 2 + t0:2 + t0 + tw], in_=pt[:, 0:tw]))(), reads=[pr], writes=[ub_r[k][ti]])
                        P.add("act", (lambda pt=pt, tw=tw, ci=ci, cbuf=cbuf: lambda E: E.activation(
                            out=cbuf[:, 0:tw], in_=pt[:, 0:tw], func=AF.Identity,
                            scale=cw_sb[:, ci * 3 + 2:ci * 3 + 3], bias=cb_sb[:, ci:ci + 1]))(),
                            reads=[pr, cws_r], writes=[cbr])
                        prev = [ub_r[k][ti - 1]] if ti > 0 else [halo_r[k]]
                        P.add("dve", (lambda k=k, t0=t0, tw=tw, ci=ci, cbuf=cbuf: lambda E: E.scalar_tensor_tensor(
                            out=cbuf[:, 0:tw], in0=ub[k][:, 1 + t0:1 + t0 + tw], scalar=cw_sb[:, ci * 3 + 1:ci * 3 + 2],
                            in1=cbuf[:, 0:tw], op0=ALU.mult, op1=ALU.add))(),
                            reads=[ub_r[k][ti], cbr, cws_r] + prev, writes=[cbr])
                        P.add("dve", (lambda k=k, t0=t0, tw=tw, ci=ci, cbuf=cbuf: lambda E: E.scalar_tensor_tensor(
                            out=cbuf[:, 0:tw], in0=ub[k][:, t0:t0 + tw], scalar=cw_sb[:, ci * 3:ci * 3 + 1],
                            in1=cbuf[:, 0:tw], op0=ALU.mult, op1=ALU.add))(),
                            reads=[ub_r[k][ti], cbr, cws_r] + prev, writes=[cbr])
                    cg, cgr = cb[0][qq % 2], cb_r[0][qq % 2]
                    cv, cvr = cb[1][qq % 2], cb_r[1][qq % 2]
                    qq += 1
                    P.add("act", (lambda cg=cg, tw=tw: lambda E: E.activation(out=cg[:, 0:tw], in_=cg[:, 0:tw], func=AF.Silu))(),
                          reads=[cgr], writes=[cgr])
                    P.add("dve", (lambda cg=cg, cv=cv, t0=t0, tw=tw, sl=sl: lambda E: E.tensor_tensor(
                        out=gT[sl][:, t0:t0 + tw], in0=cg[:, 0:tw], in1=cv[:, 0:tw], op=ALU.mult))(),
                        reads=[cgr, cvr], writes=[gT_r[sl]])
            for i in range(NT):
                for nh_ in range(2):
                    def mmd(E, i=i, nh_=nh_, gslots=gslots):
                        ins = None
                        for q, sl in enumerate(gslots):
                            ins = E.matmul(py[:], gT[sl][:, i * 128:(i + 1) * 128], wdn[sl][:, nh_ * 512:(nh_ + 1) * 512],
                                           start=(q == 0), stop=(q == len(gslots) - 1))
                        return ins
                    P.add("pe", mmd, reads=[gT_r[s_] for s_ in gslots] + [wdn_r[s_] for s_ in gslots], writes=[py_r])
                    P.add("dve", (lambda i=i, nh_=nh_: lambda E: E.tensor_tensor(
                        out=h[:, i, nh_ * 512:(nh_ + 1) * 512], in0=py[:], in1=h[:, i, nh_ * 512:(nh_ + 1) * 512],
                        op=ALU.add))(), reads=[py_r, h_res[i]], writes=[h_res[i]])
        P.barrier()
        sc.close()

    def layer1_attention():
        sc = contextlib.ExitStack()
        kT12 = sb(sc, "kT12", (128, NH, TP), BF16)
        kT12_r = [Res() for i in range(NT)]
        vb = sb(sc, "vb", (128, NT, NH, 129), BF16)
        vb_r = [Res() for i in range(NT)]
        gk = sb(sc, "gk1", (128, 4), F32)
        gk_r = Res()
        lamb = sb(sc, "lamb", (128, 4, 64), F32)
        lamt = sb(sc, "lamt", (128, 8), F32)
        lam_r = Res()
        xn_tok = sb(sc, "b_xn_tok", (128, D), BF16)
        xn_tok_r = Res()
        xnT = sb(sc, "b_xnT", (128, KC, 128), BF16)
        xnT_r = Res()
        scr = sb(sc, "b_scr", (128, D), BF16)
        scr_r = Res()
        st = sb(sc, "b_st", (128, 64), F32)
        st_r = Res()
        ks12 = sb(sc, "ks12", (128, NH, 128), BF16)
        ks12_r = Res()

        P.add("sp", lambda E: E.dma_start(out=gk[:, 0:3], in_=vecs_d[2:5, :].rearrange("v p -> p v"),
                                          allow_slow_non_contiguous=True), writes=[gk_r], dma=1)
        P.add("dve", lambda E: E.tensor_scalar(out=gk[:, 0:1], in0=gk[:, 0:1], scalar1=64.0 ** -0.5, scalar2=None,
                                               op0=ALU.mult), reads=[gk_r], writes=[gk_r])
        P.add("dve", lambda E: E.tensor_scalar(out=gk[:, 2:3], in0=gk[:, 2:3], scalar1=1.0 - lam_init, scalar2=None,
                                               op0=ALU.mult), reads=[gk_r], writes=[gk_r])
        P.add("sp", lambda E: E.dma_start(out=lamb[:].rearrange("p a d -> p (a d)"),
                                          in_=lam_d.rearrange("a d -> (a d)").partition_broadcast(128)),
              writes=[lam_r], dma=1)
        P.add("dve", lambda E: E.tensor_tensor(out=lamb[:, 0, :], in0=lamb[:, 0, :], in1=lamb[:, 1, :], op=ALU.mult),
              reads=[lam_r], writes=[lam_r])
        P.add("dve", lambda E: E.tensor_tensor(out=lamb[:, 2, :], in0=lamb[:, 2, :], in1=lamb[:, 3, :], op=ALU.mult),
              reads=[lam_r], writes=[lam_r])
        P.add("dve", lambda E: E.tensor_reduce(out=lamt[:, 0:1], in_=lamb[:, 0, :], axis=AX.X, op=ALU.add),
              reads=[lam_r], writes=[lam_r])
        P.add("dve", lambda E: E.tensor_reduce(out=lamt[:, 1:2], in_=lamb[:, 2, :], axis=AX.X, op=ALU.add),
              reads=[lam_r], writes=[lam_r])
        P.add("act", lambda E: E.activation(out=lamt[:, 2:4], in_=lamt[:, 0:2], func=AF.Exp), reads=[lam_r], writes=[lam_r])
        P.add("dve", lambda E: E.tensor_tensor(out=lamt[:, 4:5], in0=lamt[:, 3:4], in1=lamt[:, 2:3], op=ALU.subtract),
              reads=[lam_r], writes=[lam_r])
        P.add("dve", lambda E: E.tensor_scalar(out=lamt[:, 5:6], in0=lamt[:, 4:5], scalar1=-lam_init, scalar2=None,
                                               op0=ALU.add), reads=[lam_r], writes=[lam_r])
        P.add("pool", lambda E: E.memset(vb[:, :, :, 128:129], 1.0), writes=vb_r)

        kv_sc = contextlib.ExitStack()
        w_kv = sb(kv_sc, "w_kv", (128, KC, 2048), BF16)
        w_kv_r = [Res() for k in range(KC)]
        for kc in range(KC):
            P.add("pool", (lambda kc: lambda E: E.dma_start(out=w_kv[:, kc, :], in_=w_kv_d[kc * 128:(kc + 1) * 128, :]))(kc),
                  writes=[w_kv_r[kc]], dma=1)
        load_gain(2)

        def kv_tile(i):
            rms_to_xnT(i, xn_tok, xn_tok_r, lambda: xnT[:], xnT_r, scr, scr_r, st, st_r)
            for c in range(4):
                pt, pr = next_pp()

                def mm(E, c=c, pt=pt):
                    ins = None
                    for kc in range(KC):
                        ins = E.matmul(pt[:], xnT[:, kc, :], w_kv[:, kc, c * 512:(c + 1) * 512],
                                       start=(kc == 0), stop=(kc == KC - 1))
                    return ins
                P.add("pe", mm, reads=[xnT_r] + w_kv_r, writes=[pr])
                if c < 2:
                    so = 8 + c * 8
                    for hh in range(NH):
                        P.add("act", (lambda hh, pt=pt, so=so: lambda E: E.activation(
                            out=scr[:, hh * 64:(hh + 1) * 64], in_=pt[:, hh * 64:(hh + 1) * 64], func=AF.Square,
                            accum_out=st[:, so + hh:so + hh + 1]))(hh), reads=[pr], writes=[scr_r, st_r])
                    P.add("dve", (lambda so=so: lambda E: E.tensor_scalar(
                        out=st[:, 24 + so:32 + so], in0=st[:, so:so + 8], scalar1=1.0 / 64, scalar2=EPS,
                        op0=ALU.mult, op1=ALU.add))(), reads=[st_r], writes=[st_r])
                    P.add("pool", (lambda so=so: lambda E: E.tensor_tensor(
                        out=st[:, 24 + so:32 + so], in0=st[:, 24 + so:32 + so], in1=nhalf[:, 0:8], op=ALU.pow))(),
                        reads=[st_r, nhalf_r], writes=[st_r])
                    for hh in range(NH):
                        P.add("dve", (lambda hh, pt=pt, so=so, c=c: lambda E: E.tensor_scalar(
                            out=ks12[:, hh, c * 64:(c + 1) * 64], in0=pt[:, hh * 64:(hh + 1) * 64],
                            scalar1=st[:, 24 + so + hh:25 + so + hh], scalar2=None, op0=ALU.mult))(hh),
                            reads=[pr, st_r], writes=[ks12_r])
                else:
                    P.add("act", (lambda pt=pt, c=c: lambda E: E.copy(
                        out=vb[:, i, (c - 2) * 4:(c - 1) * 4, 0:128], in_=pt[:].rearrange("p (g d) -> p g d", g=4)))(),
                        reads=[pr], writes=[vb_r[i]])

            def trk(E):
                ins = None
                for hh in range(NH):
                    ins = E.transpose(out=ptr[:, hh * 128:(hh + 1) * 128], in_=ks12[:, hh, :], identity=ident[:])
                return ins
            P.add("pe", trk, reads=[ks12_r, ident_r], writes=[ptr_r])
            P.add("act", (lambda i=i: lambda E: E.activation(
                out=kT12[:, :, i * 128:(i + 1) * 128], in_=ptr[:].rearrange("p (k t) -> p k t", k=NH),
                func=AF.Copy, scale=gk[:, 1:2]))(), reads=[ptr_r, gk_r], writes=[kT12_r[i]])
        for i_ in range(NT):
            kv_tile(i_)
        P.barrier()
        kv_sc.close()

        w_q = sb(sc, "w_q", (128, KC, D), BF16)
        w_q_r = Res()
        w_o = sb(sc, "w_ob", (128, KC, D), BF16)
        w_o_r = Res()
        qT12 = sb(sc, "qT12", (128, NH, 2, 128), BF16)
        qT12_r = Res()
        P.add("pool", lambda E: E.memset(qT12[:], 0.0), writes=[qT12_r])
        pT = [sb(sc, "b_pT%d" % b, (128, 1024), BF16) for b in range(3)]
        pT_r = [Res() for b in range(3)]
        otok = sb(sc, "b_otok", (128, D), BF16)
        otok_r = Res()
        oT = sb(sc, "b_oT", (128, KC, 128), BF16)
        oT_r = Res()
        rz = sb(sc, "b_rz", (128, 8), F32)
        rz_r = Res()
        otmp = [sb(sc, "otmp%d" % b, (128, 128), F32) for b in range(2)]
        otmp_r = [Res() for b in range(2)]
        ost = sb(sc, "ost", (128, 32), F32)
        ost_r = Res()
        osb = [sb(sc, "osb%d" % b, (128, 2, 2, 129), F32) for b in range(1)] * 2
        osb_r = [Res() for b in range(1)] * 2
        osq = sb(sc, "osq", (128, 128), F32)
        osq_r = Res()
        P.add("pool", lambda E: [E.dma_start(out=w_q[:, kc, :], in_=w_q_d[kc * 128:(kc + 1) * 128, :]) for kc in range(KC)],
              writes=[w_q_r], dma=1)
        P.add("pool", lambda E: [E.dma_start(out=w_o[:, kc, :], in_=w_o_b_d[kc * 128:(kc + 1) * 128, :]) for kc in range(KC)],
              writes=[w_o_r], dma=1)
        load_gain(3)

        def b_tile(i):
            jmax = min(i + 1, NT - 1)
            rms_to_xnT(i, xn_tok, xn_tok_r, lambda: xnT[:], xnT_r, scr, scr_r, st, st_r)
            for c in range(2):
                pt, pr = py, py_r

                def mm(E, c=c, pt=pt):
                    ins = None
                    for kc in range(KC):
                        ins = E.matmul(pt[:], xnT[:, kc, :], w_q[:, kc, c * 512:(c + 1) * 512],
                                       start=(kc == 0), stop=(kc == KC - 1))
                    return ins
                P.add("pe", mm, reads=[xnT_r, w_q_r], writes=[pr])
                so = 8 + c * 8
                for hh in range(NH):
                    P.add("act", (lambda hh, pt=pt, so=so: lambda E: E.activation(
                        out=scr[:, hh * 64:(hh + 1) * 64], in_=pt[:, hh * 64:(hh + 1) * 64], func=AF.Square,
                        accum_out=st[:, so + hh:so + hh + 1]))(hh), reads=[pr], writes=[scr_r, st_r])
                P.add("dve", (lambda so=so: lambda E: E.tensor_scalar(
                    out=st[:, 24 + so:32 + so], in0=st[:, so:so + 8], scalar1=1.0 / 64, scalar2=EPS,
                    op0=ALU.mult, op1=ALU.add))(), reads=[st_r], writes=[st_r])
                P.add("pool", (lambda so=so: lambda E: E.tensor_tensor(
                    out=st[:, 24 + so:32 + so], in0=st[:, 24 + so:32 + so], in1=nhalf[:, 0:8], op=ALU.pow))(),
                    reads=[st_r, nhalf_r], writes=[st_r])
                for hh in range(NH):
                    P.add("dve", (lambda hh, pt=pt, so=so, c=c: lambda E: E.tensor_scalar(
                        out=ks12[:, hh, c * 64:(c + 1) * 64], in0=pt[:, hh * 64:(hh + 1) * 64],
                        scalar1=st[:, 24 + so + hh:25 + so + hh], scalar2=None, op0=ALU.mult))(hh),
                        reads=[pr, st_r], writes=[ks12_r])

            def trq(E):
                ins = None
                for hh in range(NH):
                    ins = E.transpose(out=ptr[:, hh * 128:(hh + 1) * 128], in_=ks12[:, hh, :], identity=ident[:])
                return ins
            P.add("pe", trq, reads=[ks12_r, ident_r], writes=[ptr_r])
            P.add("act", lambda E: E.activation(out=qT12[0:64, :, 0, :],
                                                in_=ptr[0:64, :].rearrange("p (k t) -> p k t", k=NH),
                                                func=AF.Copy, scale=gk[0:64, 0:1]),
                  reads=[ptr_r, gk_r], writes=[qT12_r])
            P.add("act", lambda E: E.activation(out=qT12[64:128, :, 1, :],
                                                in_=ptr[64:128, :].rearrange("p (k t) -> p k t", k=NH),
                                                func=AF.Copy, scale=gk[64:128, 0:1]),
                  reads=[ptr_r, gk_r], writes=[qT12_r])
            steps = [(hp, list(range(j0, min(j0 + 2, jmax + 1)))) for hp in range(4) for j0 in range(0, jmax + 1, 2)]
            pstate = {"pi": 0}
            Lbuf = [(big[0], [pp_r[0], pp_r[1]]), (big[1], [psc_r[0], psc_r[1]])]

            def emit_qk(k):
                hp, js = steps[k]
                pt, prs = Lbuf[k % 2]
                prs = prs[0:len(js)]

                def mm(E, pt=pt, js=js, hp=hp):
                    ins = None
                    for q, j in enumerate(js):
                        near = (j - i) >= -1
                        for hh in range(2):
                            h_ = hp * 2 + hh
                            c0 = q * 512 + hh * 256
                            ins = E.matmul(pt[:, c0:c0 + 256].rearrange("p (b t) -> p b t", b=2),
                                           kT12[:, h_, j * 128:(j + 1) * 128], qT12[:, h_, :, :],
                                           start=(hh == 0), stop=not near, skip_group_check=True)
                        if near:
                            for hh in range(2):
                                h_ = hp * 2 + hh
                                for br in range(2):
                                    o_ = q * 512 + (hh * 2 + br) * 128
                                    ins = E.matmul(pt[:, o_:o_ + 128], ident[:], addm[:, j - i + 1, h_, :],
                                                   start=False, stop=True, skip_group_check=True)
                    return ins
                P.add("pe", mm, reads=[kT12_r[j] for j in js] + [qT12_r, ident_r, addm_ready], writes=prs)
                return pt, prs

            def emit_rest(k, pt, prs):
                hp, js = steps[k]
                j = js[-1]
                w_ = 512 * len(js)
                pb, pbr = pT[pstate["pi"] % 3], pT_r[pstate["pi"] % 3]
                pstate["pi"] += 1
                P.add("act", (lambda pt=pt, pb=pb, w_=w_: lambda E: E.activation(
                    out=pb[:, 0:w_], in_=pt[:, 0:w_], func=AF.Exp))(), reads=prs, writes=[pbr])

                def pv(E, pb=pb, js=js, hp=hp):
                    ins = None
                    for q_, jj in enumerate(js):
                        for q in range(4):
                            h_ = hp * 2 + q // 2
                            ins = E.matmul(po[q // 2][:, (q % 2) * 256:(q % 2) * 256 + 129],
                                           pb[:, q_ * 512 + q * 128:q_ * 512 + (q + 1) * 128], vb[:, jj, h_, :],
                                           start=(jj == 0 and q % 2 == 0), stop=(jj == jmax), skip_group_check=True)
                    return ins
                P.add("pe", pv, reads=[pbr] + [vb_r[jj] for jj in js], writes=po_r)
                if j != jmax:
                    return
                ob = osb[hp % 2]
                obr = osb_r[hp % 2]
                for hh in range(2):
                    P.add("act", (lambda hh=hh, ob=ob: lambda E: E.copy(
                        out=ob[:, hh, :, :], in_=po[hh][:].rearrange("p (k c) -> p k c", k=2)[:, :, 0:129]))(),
                        reads=[po_r[hh]], writes=[obr])
                P.add("dve", (lambda ob=ob: lambda E: E.reciprocal(
                    out=rz[:, 0:4], in_=ob[:].rearrange("p a b c -> p (a b) c")[:, :, 128]))(),
                    reads=[obr], writes=[rz_r])
                for hh in range(2):
                    h_ = hp * 2 + hh
                    P.add("dve", (lambda hh=hh: lambda E: E.tensor_scalar(
                        out=rz[:, hh * 2 + 1:hh * 2 + 2], in0=rz[:, hh * 2 + 1:hh * 2 + 2], scalar1=lamt[:, 5:6],
                        scalar2=None, op0=ALU.mult))(), reads=[rz_r, lam_r], writes=[rz_r])
                    ot, otr = otmp[hh], otmp_r[hh]
                    P.add("dve", (lambda hh=hh, ot=ot, ob=ob: lambda E: E.tensor_scalar(
                        out=ot[:], in0=ob[:, hh, 0, 0:128], scalar1=rz[:, hh * 2:hh * 2 + 1], scalar2=None, op0=ALU.mult))(),
                        reads=[obr, rz_r], writes=[otr])
                    P.add("dve", (lambda hh=hh, ot=ot, ob=ob, h_=h_: lambda E: E.scalar_tensor_tensor(
                        out=ot[:], in0=ob[:, hh, 1, 0:128], scalar=rz[:, hh * 2 + 1:hh * 2 + 2], in1=ot[:],
                        op0=ALU.mult, op1=ALU.add))(), reads=[obr, rz_r, otr], writes=[otr])
                    P.add("dve", (lambda h_=h_, ot=ot: lambda E: E.tensor_tensor(
                        out=osq[:], in0=ot[:], in1=ot[:], op=ALU.mult))(), reads=[otr], writes=[osq_r])
                    P.add("dve", (lambda h_=h_: lambda E: E.tensor_reduce(
                        out=ost[:, h_:h_ + 1], in_=osq[:], axis=AX.X, op=ALU.add))(), reads=[osq_r], writes=[ost_r])
                    P.add("dve", (lambda h_=h_: lambda E: E.tensor_scalar(
                        out=ost[:, 8 + h_:9 + h_], in0=ost[:, h_:h_ + 1], scalar1=1.0 / 128, scalar2=EPS,
                        op0=ALU.mult, op1=ALU.add))(), reads=[ost_r], writes=[ost_r])
                    P.add("pool", (lambda h_=h_: lambda E: E.tensor_tensor(
                        out=ost[:, 24 + h_:25 + h_], in0=ost[:, 8 + h_:9 + h_], in1=nhalf[:, 0:1], op=ALU.pow))(),
                        reads=[ost_r, nhalf_r], writes=[ost_r])
                    P.add("dve", (lambda h_=h_, ot=ot: lambda E: E.tensor_scalar(
                        out=otok[:, h_ * 128:(h_ + 1) * 128], in0=ot[:], scalar1=ost[:, 24 + h_:25 + h_], scalar2=None,
                        op0=ALU.mult))(), reads=[otr, ost_r], writes=[otok_r])

            cur = emit_qk(0)
            for k in range(len(steps)):
                nxt = emit_qk(k + 1) if k + 1 < len(steps) else None
                emit_rest(k, *cur)
                cur = nxt

            def tro(E):
                ins = None
                for kc in range(KC):
                    ins = E.transpose(out=ptr[:, kc * 128:(kc + 1) * 128], in_=otok[:, kc * 128:(kc + 1) * 128],
                                      identity=ident[:])
                return ins
            P.add("pe", tro, reads=[otok_r, ident_r], writes=[ptr_r])
            P.add("act", lambda E: E.activation(out=oT[:], in_=ptr[:].rearrange("p (k t) -> p k t", k=KC),
                                                func=AF.Copy, scale=gk[:, 2:3]),
                  reads=[ptr_r, gk_r], writes=[oT_r])
            for nh_ in range(2):
                def mmo(E, nh_=nh_):
                    ins = None
                    for kc in range(KC):
                        ins = E.matmul(py[:], oT[:, kc, :], w_o[:, kc, nh_ * 512:(nh_ + 1) * 512],
                                       start=(kc == 0), stop=(kc == KC - 1))
                    return ins
                P.add("pe", mmo, reads=[oT_r, w_o_r], writes=[py_r])
                P.add("dve", (lambda nh_=nh_, i=i: lambda E: E.tensor_tensor(
                    out=h[:, i, nh_ * 512:(nh_ + 1) * 512], in0=py[:], in1=h[:, i, nh_ * 512:(nh_ + 1) * 512],
                    op=ALU.add))(), reads=[py_r, h_res[i]], writes=[h_res[i]])
        for i_ in range(dbg_tiles):
            b_tile(i_)
        P.barrier()
        sc.close()

    stage = dbg_stage if dbg_stage is not None else 99
    import os
    skip01 = os.environ.get("KSKIP01") == "1"
    if not skip01:
        layer0_attention()
    if stage >= 2 and not skip01:
        conv_ffn(0)
    if stage >= 3:
        layer1_attention()
    if stage >= 4:
        conv_ffn(1)

    out_r = Res("out")
    P.add("sp", lambda E: E.dma_start(out=out_d[0:112, :], in_=h[16:128, 0, :]), reads=[h_res[0]], writes=[out_r], dma=1)
    for i in range(1, 16):
        P.add("sp", (lambda i: lambda E: E.dma_start(out=out_d[128 * i - 16:128 * i + 112, :], in_=h[:, i, :]))(i),
              reads=[h_res[i]], writes=[Res()], dma=1)
    P.add("sp", lambda E: E.dma_start(out=out_d[2032:2048, :], in_=h[0:16, 16, :]), reads=[h_res[16]], writes=[Res()], dma=1)
    P.barrier()
    if dbg_stage is not None:
        print("n_ops", len(P.ops))
    P.emit(es)
    es.close()
    return nc


def _host_inputs(inputs):
    f = lambda a: np.ascontiguousarray(np.asarray(a, dtype=np.float32))
    idx, negvis = _static_tables()
    rel = f(inputs["rel_table"])
    table_ext = np.concatenate([rel, np.full((1, NH), NEG, np.float32)], axis=0)
    am = table_ext[idx]
    am = np.ascontiguousarray(am.transpose(1, 0, 3, 2)).reshape(128, 3 * NH * 128)
    gnorm = np.stack([f(inputs["ln_attn_g"])[0], f(inputs["ln_ffn_g"])[0], f(inputs["kv_norm_g"]),
                      f(inputs["ln_attn_g"])[1], f(inputs["ln_ffn_g"])[1]], axis=0)
    vecs = np.zeros((8, 128), np.float32)
    vecs[0] = f(inputs["qn_a"])[0]
    vecs[1] = f(inputs["kn_a"])[0]
    vecs[2] = np.concatenate([f(inputs["qn_b"])[0]] * 2)
    vecs[3] = np.concatenate([f(inputs["kn_b"])] * 2)
    vecs[4] = f(inputs["subln_b"])[0]
    lamv = np.stack([f(inputs["lam_q1"])[0], f(inputs["lam_k1"])[0], f(inputs["lam_q2"])[0], f(inputs["lam_k2"])[0]], 0)
    cw = f(inputs["conv_w"])
    convwT = np.ascontiguousarray(cw.reshape(2, 3, 2 * NFC, 128).transpose(0, 3, 2, 1)).reshape(2, 128, 2 * NFC * 3)
    cbias = f(inputs["conv_b"])
    convbT = np.ascontiguousarray(cbias.reshape(2, 2 * NFC, 128).transpose(0, 2, 1))
    shared = {
        "meta_tokens": f(inputs["meta_tokens"]),
        "gnorm": gnorm,
        "w_in_a": f(inputs["w_in_a"])[0],
        "w_o_a": f(inputs["w_o_a"])[0],
        "w_kv_b": f(inputs["w_kv_b"]),
        "w_q_b": f(inputs["w_q_b"])[0],
        "w_o_b": f(inputs["w_o_b"])[0],
        "w_up": f(inputs["w_up"]),
        "w_down": f(inputs["w_down"]),
        "conv_wT": convwT,
        "conv_bT": convbT,
        "vecs": vecs,
        "lamv": lamv,
        "addmask": am,
        "cfar": np.ascontiguousarray(rel[15:16, :]),
        "negvis": negvis,
        "ident": np.eye(128, dtype=np.float32),
    }
    return shared


def kernel(**inputs):
    shared = _host_inputs(inputs)
    x = np.asarray(inputs["x"], dtype=np.float32)
    nc = build_nc()
    in_maps = []
    for b in range(8):
        m = dict(shared)
        m["x"] = np.ascontiguousarray(x[b])
        in_maps.append(m)
    res = run_bass_kernel_spmd(nc, in_maps, core_ids=list(range(8)))
    out = np.stack([np.asarray(r["out"], dtype=np.float32) for r in res.results], axis=0)
    return out
```
